# Optimizing a Trainium2 kernel written in Bass

```python
import numpy as np
import jax
import jax.numpy as jnp
from jax import lax

D_MODEL = 1024
BATCH = 4
SEQ = 4096
DEPTH = 2

MIX = D_MODEL // 2
GM_CHUNK = 128
GM_GROUPS = 4
GM_GW = MIX // GM_GROUPS
CONV_WIDTH = 4
LRU_BLOCKS = 8
LRU_BW = MIX // LRU_BLOCKS
LRU_C = 8.0
N_HEADS = 8
HEAD_DIM = MIX // N_HEADS
N_KV = 2
HPG = N_HEADS // N_KV
CMP_LEN = 32
CMP_STRIDE = 16
SLC_LEN = 64
SLC_TOPN = 8
WIN = 512
Q_BLOCK = 128
NSA_Q = N_HEADS * HEAD_DIM
NSA_KV = N_KV * HEAD_DIM
POOL_WINDOWS = (2, 4, 8, 16)
POOL_GROUPS = len(POOL_WINDOWS)
POOL_GW = MIX // POOL_GROUPS
N_BRANCH = 4
N_GROUPS = 4
EXPERTS_PER_GROUP = 4
N_EXPERTS = N_GROUPS * EXPERTS_PER_GROUP
EXPERT_TOPK = 2
D_EXPERT = D_MODEL // 2

EPS = 1e-6
NEG_INF = -1e30
FORCE_SCORE = 1e6

IN_SPLITS = (MIX, MIX, MIX, MIX, NSA_Q, 6 * NSA_KV, 3 * N_HEADS, MIX, N_BRANCH * D_MODEL)
N_IN = sum(IN_SPLITS)

kernel_name = 'hybrid_gated_mixers_hier_moe'


def rms_norm(x, g):
    xf = x.astype(jnp.float32)
    y = xf * lax.rsqrt(jnp.mean(xf * xf, axis=-1, keepdims=True) + EPS)
    return (y * g.astype(jnp.float32)).astype(x.dtype)


def gmlp_mixer(u, v, v_norm_g, ws, bs):
    B, S, _ = u.shape
    nc = S // GM_CHUNK
    u = jax.nn.gelu(u)
    v = rms_norm(jax.nn.gelu(v), v_norm_g).reshape(B, nc, GM_CHUNK, GM_GROUPS, GM_GW)
    causal = jnp.tril(jnp.ones((GM_CHUNK, GM_CHUNK), dtype=bool))
    w = jnp.where(causal[None], ws, 0.0)
    mixed = jnp.einsum('gts,bnsgc->bntgc', w, v) + bs.T[:, :, None]
    return u * mixed.reshape(B, S, MIX)


def rglru_mixer(gate_in, x_in, conv_w, conv_b, wa, ba, wx, bx, lam):
    B, S, C = x_in.shape
    f32 = jnp.float32
    xp = jnp.pad(x_in, ((0, 0), (CONV_WIDTH - 1, 0), (0, 0)))
    xc = conv_b + sum(xp[:, k:k + S] * conv_w[k] for k in range(CONV_WIDTH))
    xb = xc.reshape(B, S, LRU_BLOCKS, LRU_BW)
    r = jax.nn.sigmoid(jnp.einsum('bshi,hij->bshj', xb, wa).reshape(B, S, C) + ba)
    i = jax.nn.sigmoid(jnp.einsum('bshi,hij->bshj', xb, wx).reshape(B, S, C) + bx)
    log_a = -LRU_C * r.astype(f32) * jax.nn.softplus(-lam.astype(f32))
    a = jnp.exp(log_a)
    b = jnp.sqrt(-jnp.expm1(2.0 * log_a)) * (i * xc).astype(f32)

    def combine(c1, c2):
        a1, b1 = c1
        a2, b2 = c2
        return a1 * a2, a2 * b1 + b2

    _, h = lax.associative_scan(combine, (a, b), axis=1)
    return jax.nn.gelu(gate_in) * h.astype(x_in.dtype)


def _gather_blocks(blocks, idx):
    return blocks[idx].reshape(idx.shape[0], -1, blocks.shape[-1])


_gather_bg = jax.vmap(jax.vmap(_gather_blocks))


def nsa_mixer(q, kv, gate_logits, cmp_pe, cmp_w1, cmp_w2):
    B, S, _ = q.shape
    f32 = jnp.float32
    scale = HEAD_DIM ** -0.5
    t_pos = jnp.arange(S)
    qh = q.reshape(B, S, N_KV, HPG, HEAD_DIM).transpose(0, 2, 3, 1, 4)
    kv = kv.reshape(B, S, 6, N_KV, HEAD_DIM).transpose(2, 0, 3, 1, 4)

    n_sub = CMP_LEN // CMP_STRIDE
    n_chunk = S // CMP_STRIDE
    n_cmp = n_chunk - n_sub + 1
    chunks = kv[0:2].reshape(2, B, N_KV, n_chunk, CMP_STRIDE, HEAD_DIM)
    blocks = jnp.concatenate([chunks[:, :, :, j:j + n_cmp] for j in range(n_sub)], axis=4)
    blocks = blocks + cmp_pe[:, None, None, None]
    flat = blocks.reshape(2, B, N_KV, n_cmp, CMP_LEN * HEAD_DIM)
    hid = jax.nn.gelu(jnp.einsum('cbgnf,cgfe->cbgne', flat, cmp_w1))
    kvc = jnp.einsum('cbgne,cged->cbgnd', hid, cmp_w2)
    k_c, v_c = kvc[0], kvc[1]
    cmp_end = jnp.arange(n_cmp) * CMP_STRIDE + (CMP_LEN - 1)
    cmp_mask = cmp_end[None, :] <= t_pos[:, None]
    s_c = jnp.einsum('bgjsd,bgnd->bgjsn', qh, k_c).astype(f32) * scale
    p_c = jax.nn.softmax(jnp.where(cmp_mask, s_c, NEG_INF), axis=-1)
    p_c = jnp.where(jnp.any(cmp_mask, axis=-1)[:, None], p_c, 0.0)
    o_cmp = jnp.einsum('bgjsn,bgnd->bgjsd', p_c.astype(q.dtype), v_c)

    n_slc = S // SLC_LEN
    topn = min(SLC_TOPN, n_slc)
    c_start = np.arange(n_cmp) * CMP_STRIDE
    s_start = np.arange(n_slc) * SLC_LEN
    overlap = ((c_start[:, None] <= s_start[None, :] + SLC_LEN - 1)
               & (c_start[:, None] + CMP_LEN - 1 >= s_start[None, :])).astype(np.float32)
    imp = jnp.einsum('bgjsn,nm->bgsm', p_c, jnp.asarray(overlap))
    blk = jnp.arange(n_slc)
    cur = t_pos // SLC_LEN
    causal_blk = blk[None, :] * SLC_LEN <= t_pos[:, None]
    forced = (blk[None, :] == 0) | (blk[None, :] == cur[:, None]) | (blk[None, :] == cur[:, None] - 1)
    score = jnp.where(forced, FORCE_SCORE, jnp.where(causal_blk, imp, -1.0))
    top_val, top_idx = lax.top_k(score, topn)
    top_ok = top_val >= 0.0

    nq = S // Q_BLOCK
    ks_blk = kv[2].reshape(B, N_KV, n_slc, SLC_LEN, HEAD_DIM)
    vs_blk = kv[3].reshape(B, N_KV, n_slc, SLC_LEN, HEAD_DIM)

    def slc_block(args):
        q_b, idx_b, ok_b, t_b = args
        k_sel = _gather_bg(ks_blk, idx_b)
        v_sel = _gather_bg(vs_blk, idx_b)
        k_pos = idx_b[..., None] * SLC_LEN + jnp.arange(SLC_LEN)
        ok = (ok_b[..., None] & (k_pos <= t_b[:, None, None])).reshape(B, N_KV, Q_BLOCK, -1)
        s = jnp.einsum('bgjqd,bgqkd->bgjqk', q_b, k_sel).astype(f32) * scale
        p = jax.nn.softmax(jnp.where(ok[:, :, None], s, NEG_INF), axis=-1)
        return jnp.einsum('bgjqk,bgqkd->bgjqd', p.astype(q_b.dtype), v_sel)

    q_blocks = qh.reshape(B, N_KV, HPG, nq, Q_BLOCK, HEAD_DIM).transpose(3, 0, 1, 2, 4, 5)
    idx_blocks = top_idx.reshape(B, N_KV, nq, Q_BLOCK, topn).transpose(2, 0, 1, 3, 4)
    ok_blocks = top_ok.reshape(B, N_KV, nq, Q_BLOCK, topn).transpose(2, 0, 1, 3, 4)
    o_slc = lax.map(slc_block, (q_blocks, idx_blocks, ok_blocks, t_pos.reshape(nq, Q_BLOCK)))
    o_slc = o_slc.transpose(1, 2, 3, 0, 4, 5).reshape(B, N_KV, HPG, S, HEAD_DIM)

    n_pad = WIN // Q_BLOCK
    band_len = Q_BLOCK * (n_pad + 1)
    kw = jnp.pad(kv[4], ((0, 0), (0, 0), (WIN, 0), (0, 0))).reshape(B, N_KV, nq + n_pad, Q_BLOCK, HEAD_DIM)
    vw = jnp.pad(kv[5], ((0, 0), (0, 0), (WIN, 0), (0, 0))).reshape(B, N_KV, nq + n_pad, Q_BLOCK, HEAD_DIM)
    band_k = jnp.concatenate([kw[:, :, j:j + nq] for j in range(n_pad + 1)], axis=3)
    band_v = jnp.concatenate([vw[:, :, j:j + nq] for j in range(n_pad + 1)], axis=3)
    qb = qh.reshape(B, N_KV, HPG, nq, Q_BLOCK, HEAD_DIM)
    s_w = jnp.einsum('bgjiqd,bgikd->bgjiqk', qb, band_k).astype(f32) * scale
    q_idx = jnp.arange(Q_BLOCK)
    k_idx = jnp.arange(band_len)
    delta = WIN + q_idx[:, None] - k_idx[None, :]
    k_abs = (jnp.arange(nq)[:, None] - n_pad) * Q_BLOCK + k_idx[None, :]
    win_mask = ((delta >= 0) & (delta < WIN))[None] & (k_abs >= 0)[:, None, :]
    p_w = jax.nn.softmax(jnp.where(win_mask, s_w, NEG_INF), axis=-1)
    o_win = jnp.einsum('bgjiqk,bgikd->bgjiqd', p_w.astype(q.dtype), band_v).reshape(B, N_KV, HPG, S, HEAD_DIM)

    g = jax.nn.sigmoid(gate_logits.reshape(B, S, N_KV, HPG, 3).transpose(4, 0, 2, 3, 1))[..., None]
    o = g[0] * o_cmp + g[1] * o_slc + g[2] * o_win
    return o.transpose(0, 3, 1, 2, 4).reshape(B, S, NSA_Q)


def pool_mixer(xd, pool_w, pool_scale):
    B, S, C = xd.shape
    f32 = jnp.float32
    xf = xd.astype(f32)
    csum = jnp.pad(lax.cumsum(xf, axis=1), ((0, 0), (1, 0), (0, 0)))
    t = jnp.arange(S)
    outs = []
    for gi, w in enumerate(POOL_WINDOWS):
        c = csum[:, :, gi * POOL_GW:(gi + 1) * POOL_GW]
        lower = jnp.pad(c[:, :S + 1 - w], ((0, 0), (w - 1, 0), (0, 0)))
        cnt = jnp.minimum(t + 1, w).astype(f32)[None, :, None]
        outs.append((c[:, 1:] - lower) / cnt - xf[:, :, gi * POOL_GW:(gi + 1) * POOL_GW])
    pooled = jnp.stack(outs, axis=2).astype(xd.dtype)
    mixed = jnp.einsum('bsgi,gij->bsgj', pooled, pool_w).reshape(B, S, C)
    return mixed * pool_scale


def hybrid_mixer(h, w_in, gm_norm_g, gm_ws, gm_b, conv_w, conv_b, lru_wa, lru_ba, lru_wx,
                 lru_bx, lru_lambda, cmp_pe, cmp_w1, cmp_w2, pool_w, pool_scale, w_branch, w_out):
    B, S, _ = h.shape
    proj = h @ w_in
    cuts = np.cumsum(IN_SPLITS)[:-1].tolist()
    u, v, gate_b, rec_b, q, kv, nsa_g, xd, mg = jnp.split(proj, cuts, axis=-1)
    y_a = gmlp_mixer(u, v, gm_norm_g, gm_ws, gm_b)
    y_b = rglru_mixer(gate_b, rec_b, conv_w, conv_b, lru_wa, lru_ba, lru_wx, lru_bx, lru_lambda)
    y_c = nsa_mixer(q, kv, nsa_g, cmp_pe, cmp_w1, cmp_w2)
    y_d = pool_mixer(xd, pool_w, pool_scale)
    ys = jnp.stack([y_a, y_b, y_c, y_d])
    branch = jnp.einsum('nbsc,ncd->bsnd', ys, w_branch)
    gates = jax.nn.sigmoid(mg.reshape(B, S, N_BRANCH, D_MODEL))
    merged = jnp.sum(gates * branch, axis=2)
    return merged @ w_out


def hier_moe(h, wr_g, br_g, wr_e, br_e, w_gate, w_up, w_down):
    B, S, D = h.shape
    f32 = jnp.float32
    t = h.reshape(-1, D)
    T = t.shape[0]
    grp_p = jax.nn.softmax((t @ wr_g + br_g).astype(f32), axis=-1)
    grp_w, grp_idx = lax.top_k(grp_p, 1)
    exp_logits = (t @ wr_e + br_e).astype(f32).reshape(T, N_GROUPS, EXPERTS_PER_GROUP)
    in_grp = jnp.einsum('tge,tg->te', exp_logits, jax.nn.one_hot(grp_idx[:, 0], N_GROUPS, dtype=f32))
    top_l, top_e = lax.top_k(in_grp, EXPERT_TOPK)
    top_w = jax.nn.softmax(top_l, axis=-1) * grp_w
    expert_id = grp_idx * EXPERTS_PER_GROUP + top_e
    combine = jnp.sum(jax.nn.one_hot(expert_id, N_EXPERTS, dtype=f32) * top_w[..., None], axis=1)
    hid = jax.nn.silu(jnp.einsum('td,edf->etf', t, w_gate)) * jnp.einsum('td,edf->etf', t, w_up)
    hid = hid * combine.T[:, :, None].astype(hid.dtype)
    out = jnp.einsum('etf,efd->td', hid, w_down)
    return out.reshape(B, S, D)


def setup_inputs(seed: int = 0) -> dict:
    key = jax.random.key(seed)
    ks = iter(jax.random.split(key, 40))
    f32 = jnp.float32
    L = DEPTH

    def nrm(shape, scale):
        return jax.random.normal(next(ks), shape, f32) * scale

    def gain(shape):
        return 1.0 + 0.02 * jax.random.normal(next(ks), shape, f32)

    a_c = jax.random.uniform(next(ks), (L, MIX), f32, minval=0.9, maxval=0.999)
    a0 = a_c ** (1.0 / LRU_C)
    lru_lambda = jnp.log(a0) - jnp.log1p(-a0)
    return {
        'x': nrm((BATCH, SEQ, D_MODEL), 1.0),
        'norm1_g': gain((L, D_MODEL)),
        'w_in': nrm((L, D_MODEL, N_IN), D_MODEL ** -0.5),
        'gm_norm_g': gain((L, MIX)),
        'gm_ws': nrm((L, GM_GROUPS, GM_CHUNK, GM_CHUNK), GM_CHUNK ** -0.5),
        'gm_b': gain((L, GM_GROUPS, GM_CHUNK)),
        'conv_w': nrm((L, CONV_WIDTH, MIX), CONV_WIDTH ** -0.5),
        'conv_b': nrm((L, MIX), 0.02),
        'lru_wa': nrm((L, LRU_BLOCKS, LRU_BW, LRU_BW), LRU_BW ** -0.5),
        'lru_ba': nrm((L, MIX), 0.02),
        'lru_wx': nrm((L, LRU_BLOCKS, LRU_BW, LRU_BW), LRU_BW ** -0.5),
        'lru_bx': nrm((L, MIX), 0.02),
        'lru_lambda': lru_lambda,
        'cmp_pe': nrm((L, 2, CMP_LEN, HEAD_DIM), 0.02),
        'cmp_w1': nrm((L, 2, N_KV, CMP_LEN * HEAD_DIM, HEAD_DIM), (CMP_LEN * HEAD_DIM) ** -0.5),
        'cmp_w2': nrm((L, 2, N_KV, HEAD_DIM, HEAD_DIM), HEAD_DIM ** -0.5),
        'pool_w': nrm((L, POOL_GROUPS, POOL_GW, POOL_GW), POOL_GW ** -0.5),
        'pool_scale': gain((L, MIX)),
        'w_branch': nrm((L, N_BRANCH, MIX, D_MODEL), MIX ** -0.5),
        'w_out': nrm((L, D_MODEL, D_MODEL), D_MODEL ** -0.5),
        'norm2_g': gain((L, D_MODEL)),
        'router_w_group': nrm((L, D_MODEL, N_GROUPS), D_MODEL ** -0.5),
        'router_b_group': nrm((L, N_GROUPS), 0.01),
        'router_w_expert': nrm((L, D_MODEL, N_EXPERTS), D_MODEL ** -0.5),
        'router_b_expert': nrm((L, N_EXPERTS), 0.01),
        'moe_w_gate': nrm((L, N_EXPERTS, D_MODEL, D_EXPERT), D_MODEL ** -0.5),
        'moe_w_up': nrm((L, N_EXPERTS, D_MODEL, D_EXPERT), D_MODEL ** -0.5),
        'moe_w_down': nrm((L, N_EXPERTS, D_EXPERT, D_MODEL), D_EXPERT ** -0.5),
        'final_norm_g': gain((D_MODEL,)),
    }


def reference(x, norm1_g, w_in, gm_norm_g, gm_ws, gm_b, conv_w, conv_b, lru_wa, lru_ba, lru_wx,
              lru_bx, lru_lambda, cmp_pe, cmp_w1, cmp_w2, pool_w, pool_scale, w_branch, w_out,
              norm2_g, router_w_group, router_b_group, router_w_expert, router_b_expert,
              moe_w_gate, moe_w_up, moe_w_down, final_norm_g):
    h = x
    for l in range(DEPTH):
        h = h + hybrid_mixer(rms_norm(h, norm1_g[l]), w_in[l], gm_norm_g[l], gm_ws[l], gm_b[l],
                             conv_w[l], conv_b[l], lru_wa[l], lru_ba[l], lru_wx[l], lru_bx[l],
                             lru_lambda[l], cmp_pe[l], cmp_w1[l], cmp_w2[l], pool_w[l],
                             pool_scale[l], w_branch[l], w_out[l])
        h = h + hier_moe(rms_norm(h, norm2_g[l]), router_w_group[l], router_b_group[l],
                         router_w_expert[l], router_b_expert[l], moe_w_gate[l], moe_w_up[l],
                         moe_w_down[l])
    return rms_norm(h, final_norm_g)
```

```python
import numpy as np
import ml_dtypes
from contextlib import ExitStack
import concourse.bass as bass
import concourse.mybir as mybir
from concourse.bass_utils import run_bass_kernel_spmd

F32 = mybir.dt.float32
BF16 = mybir.dt.bfloat16
AF = mybir.ActivationFunctionType
ALU = mybir.AluOpType
AX = mybir.AxisListType

S = 4096
D = 1024
MIX = 512
NIN = 7960
DEPTH = 2
EPS = 1e-6
NQT = 32
OFF = dict(u=0, v=512, gb=1024, rb=1536, q=2048, kcmp=2560, vcmp=2688, kslc=2816, vslc=2944,
           kwin=3072, vwin=3200, ng=3328, xd=3352, mg=3864)
NV = 64
NR = 2068
BIG = 30000.0
SAME_ENG_WAIT = True
ENGS = ['sync', 'scalar', 'vector', 'gpsimd', 'tensor']


class PB:
    def __init__(self, nc, es):
        self.nc, self.es = nc, es
        self.q = {e: [] for e in ENGS}
        self.cnt = {e: 0 for e in ENGS}
        self.seen = {e: {} for e in ENGS}
        self.buf = {}
        self.sems = {}
        self.dcnt = {}

    def _need(self, eng, ev, waits):
        if ev is None:
            return
        sk, val = ev
        if sk == ('e', eng) and (eng == 'tensor' or not SAME_ENG_WAIT):
            return
        if self.seen[eng].get(sk, 0) >= val:
            return
        self.seen[eng][sk] = val
        waits[sk] = max(waits.get(sk, 0), val)

    def _deps(self, eng, reads, writes):
        waits = {}
        for k in reads:
            b = self.buf.get(k)
            if b:
                self._need(eng, b[0], waits)
        for k in writes:
            b = self.buf.get(k)
            if b:
                self._need(eng, b[0], waits)
                for sk, val in b[1].items():
                    self._need(eng, (sk, val), waits)
        return list(waits.items())

    def _commit(self, ev, reads, writes):
        for k in reads:
            b = self.buf.setdefault(k, [None, {}])
            b[1][ev[0]] = max(b[1].get(ev[0], 0), ev[1])
        for k in writes:
            self.buf[k] = [ev, {}]

    def op(self, eng, fn, reads=(), writes=()):
        waits = self._deps(eng, reads, writes)
        self.cnt[eng] += 1
        ev = (('e', eng), self.cnt[eng])
        self._commit(ev, reads, writes)
        self.q[eng].append((waits, fn, (('e', eng), 1)))

    def dma(self, eng, out, in_, dkey, reads=(), writes=()):
        waits = self._deps(eng, reads, writes)
        self.dcnt[dkey] = self.dcnt.get(dkey, 0) + 16
        ev = (('d', dkey), self.dcnt[dkey])
        self._commit(ev, reads, writes)
        self.q[eng].append((waits, (lambda e: e.dma_start(out=out, in_=in_)), (('d', dkey), 16)))

    def dma_batch(self, eng, pairs, dkey, reads=(), writes=()):
        waits = self._deps(eng, reads, writes)
        self.dcnt[dkey] = self.dcnt.get(dkey, 0) + 16 * len(pairs)
        ev = (('d', dkey), self.dcnt[dkey])
        self._commit(ev, reads, writes)
        for i, (out, in_) in enumerate(pairs):
            self.q[eng].append((waits if i == 0 else [], (lambda e, out=out, in_=in_: e.dma_start(out=out, in_=in_)),
                                (('d', dkey), 16)))

    def dma_split(self, eng, out, in_, dkey, n, reads=(), writes=()):
        A = out.shape[1]
        assert A % n == 0 and in_.shape[1] == A, (out.shape, in_.shape, n)
        c = A // n
        pairs = [(out[:, i * c:(i + 1) * c], in_[:, i * c:(i + 1) * c]) for i in range(n)]
        self.dma_batch(eng, pairs, dkey, reads=reads, writes=writes)

    def group_done(self, dkey, key):
        self.buf[key] = [(('d', dkey), self.dcnt[dkey]), {}]

    def wait_event(self, eng, ev):
        waits = {}
        self._need(eng, ev, waits)
        if waits:
            self.q[eng].append((list(waits.items()), None, None))

    def barrier(self):
        evs = [(('e', e), self.cnt[e]) for e in ENGS if self.cnt[e] > 0]
        evs += [(('d', k), v) for k, v in self.dcnt.items()]
        for eng in ENGS:
            waits = {}
            for ev in evs:
                self._need(eng, ev, waits)
            if waits:
                self.q[eng].append((list(waits.items()), None, None))

    def emit(self):
        allsk = set()
        for e in ENGS:
            for waits, fn, inc in self.q[e]:
                for sk, _ in waits:
                    allsk.add(sk)
                if inc is not None:
                    allsk.add(inc[0])
        for i, sk in enumerate(sorted(allsk, key=repr)):
            self.sems[sk] = self.es.enter_context(self.nc.semaphore("s%d" % i))
        block = self.es.enter_context(self.nc.Block())
        for e in ENGS:
            def body(engine, e=e):
                for waits, fn, inc in self.q[e]:
                    for sk, val in waits:
                        engine.wait_ge(self.sems[sk], val)
                    if fn is not None:
                        ins = fn(engine)
                        ins.then_inc(self.sems[inc[0]], inc[1])
            getattr(block, e)(body)


class Arena:
    def __init__(self, ap_f32, nwords):
        self.ap, self.n, self.off = ap_f32, nwords, 0

    def mark(self):
        return self.off

    def reset(self, off):
        self.off = off

    def alloc(self, free_shape, dtype):
        n = int(np.prod(free_shape))
        nw = n if dtype == F32 else (n + 1) // 2
        nw = (nw + 7) // 8 * 8
        w0 = self.off
        self.off += nw
        assert self.off <= self.n, ("SBUF arena overflow", self.off, self.n)
        v = self.ap[:, w0:w0 + nw]
        if dtype != F32:
            v = v.bitcast(dtype)
        v = v[:, 0:n]
        if len(free_shape) == 2:
            v = v.rearrange("p (a b) -> p a b", a=free_shape[0], b=free_shape[1])
        elif len(free_shape) == 3:
            v = v.rearrange("p (a b c) -> p a b c", a=free_shape[0], b=free_shape[1], c=free_shape[2])
        return v


def flat2d(ap, cols):
    nd = len(ap.shape)
    names = " ".join("a%d" % i for i in range(nd))
    f = ap.rearrange("%s -> (%s)" % (names, names))
    return f.rearrange("(r c) -> r c", c=cols)


def build(stop_after=None, debug=False, nlayers=DEPTH):
    nc = bass.Bass("TRN2", target_bir_lowering=False)
    with ExitStack() as es:
        _build(nc, es, stop_after, debug, nlayers)
    return nc


def _build(nc, es, stop_after, debug, nlayers):
    pb = PB(nc, es)
    dbg_kind = "ExternalOutput" if debug else "Internal"

    def din(name, shape, dt=F32):
        return nc.dram_tensor(name, list(shape), dt, kind="ExternalInput").ap()

    def dscr(name, shape, dt, kind=None):
        return nc.dram_tensor(name, list(shape), dt, kind=kind or dbg_kind).ap()

    x_in = din("x", [S, D])
    w_in = din("w_in", [DEPTH, D, NIN])
    w_branch = din("w_branch", [DEPTH, 4 * MIX, D])
    w_out = din("w_out", [DEPTH, D, D])
    moe_g = din("moe_g", [DEPTH, 16, D, 512])
    moe_u = din("moe_u", [DEPTH, 16, D, 512])
    moe_d = din("moe_d", [DEPTH, 16, 512, D])
    gm_wsT = din("gm_wsT", [DEPTH, 4, 128, 128])
    lru_wa = din("lru_wa", [DEPTH, 8, 64, 64])
    lru_wx = din("lru_wx", [DEPTH, 8, 64, 64])
    cmp_w1 = din("cmp_w1", [DEPTH, 2, 2, 2048, 64])
    cmp_w2 = din("cmp_w2", [DEPTH, 2, 2, 64, 64])
    cmp_peT = din("cmp_peT", [DEPTH, 2, 64, 32])
    pool_w = din("pool_w", [DEPTH, 4, 128, 128])
    router_w = din("router_w", [DEPTH, D, 20])
    vecs = din("vecs", [DEPTH, 128, NV])
    rows = din("rows", [DEPTH, 128, NR])
    c_ident = din("c_ident", [128, 128])
    c_tril4 = din("c_tril4", [128, 512], BF16)
    c_triu4 = din("c_triu4", [128, 512], BF16)
    c_cmpmask = din("c_cmpmask", [NQT, 128, 2, 512], BF16)
    c_ovc = din("c_ovc", [128, 2, 65], BF16)
    c_eblk = din("c_eblk", [64, S], BF16)
    c_selC = din("c_selC", [S, 64])
    c_selB = din("c_selB", [S, 64])
    c_pcorr = din("c_pcorr", [128, 4, 16])
    out_d = nc.dram_tensor("out", [S, D], F32, kind="ExternalOutput").ap()

    w_in_bf = dscr("w_in_bf", [DEPTH, D, NIN], BF16, "Internal")
    w_branch_bf = dscr("w_branch_bf", [DEPTH, 4 * MIX, D], BF16, "Internal")
    w_out_bf = dscr("w_out_bf", [DEPTH, D, D], BF16, "Internal")
    moe_g_bf = dscr("moe_g_bf", [DEPTH, 16, D, 512], BF16, "Internal")
    moe_u_bf = dscr("moe_u_bf", [DEPTH, 16, D, 512], BF16, "Internal")
    moe_d_bf = dscr("moe_d_bf", [DEPTH, 16, 512, D], BF16, "Internal")
    cmp_w1_bf = dscr("cmp_w1_bf", [DEPTH, 2, 2, 2048, 64], BF16, "Internal")
    uT = dscr("uT", [MIX, S], BF16)
    vn = dscr("vn", [S, MIX], BF16)
    gbT = dscr("gbT", [MIX, S], BF16)
    rbT = dscr("rbT", [MIX, S], BF16)
    qT = dscr("qT", [MIX, S], BF16)
    kvT = dscr("kvT", [4, 128, S], BF16)
    vtok = dscr("vtok", [S, 256], BF16)
    gC = dscr("gC", [S, 24], F32)
    xdT = dscr("xdT", [MIX, S], BF16)
    mgT = dscr("mgT", [4 * D, S], BF16)
    yT = dscr("yT", [4 * MIX, S], BF16)
    xres = dscr("xres", [S, D], F32)

    ARW = 48 * 1024
    arena_t = es.enter_context(nc.sbuf_tensor("arena", [128, ARW], F32))
    ar = Arena(arena_t, ARW)
    psb = [es.enter_context(nc.psum_tensor("ps%d" % i, [128, 512], F32)) for i in range(8)]

    def PS(i):
        return ('ps', i)

    identb = ar.alloc([128], BF16)
    identf = ar.alloc([128], F32)
    tril4 = ar.alloc([512], BF16)
    triu4 = ar.alloc([512], BF16)
    vec = ar.alloc([NV], F32)
    pb.dma('gpsimd', identb, c_ident[:, :], 'identb', writes=['identb'])
    pb.dma('sync', identf, c_ident[:, :], 'identf', writes=['identf'])
    pb.dma('sync', tril4, c_tril4[:, :], 'tril4', writes=['tril4'])
    pb.dma('sync', triu4, c_triu4[:, :], 'triu4', writes=['triu4'])
    PERSIST = ar.mark()

    import os
    STAGE = int(os.environ.get("KSTAGE", "9"))

    cv_n = [0]

    def cv_dma(dst, src, key):
        slot = cv_n[0] % 4
        cv_n[0] += 1
        dk = ('cv', slot)
        if dk in pb.dcnt:
            pb.wait_event('gpsimd', (('d', dk), pb.dcnt[dk]))
        pb.dma('gpsimd', dst, src, dk, writes=[key])

    wkeys = {}

    def conv_w_in(l):
        ks = []
        for kc in range(8):
            ks.append(('w_in_bf', l, kc))
            cv_dma(w_in_bf[l, kc * 128:(kc + 1) * 128, :], w_in[l, kc * 128:(kc + 1) * 128, :], ks[-1])
        wkeys[('w_in', l)] = ks

    def conv_flat(dst, src, key, cols=4096, rows_per=128):
        d2, s2 = flat2d(dst, cols), flat2d(src, cols)
        nr = d2.shape[0]
        ks = []
        for r0 in range(0, nr, rows_per):
            r1 = min(nr, r0 + rows_per)
            ks.append((key, r0))
            cv_dma(d2[r0:r1, :], s2[r0:r1, :], ks[-1])
        return ks

    def conv_rest(l):
        wkeys[('w1', l)] = conv_flat(cmp_w1_bf[l], cmp_w1[l], ('cmp_w1_bf', l))
        wkeys[('wb', l)] = conv_flat(w_branch_bf[l], w_branch[l], ('w_branch_bf', l))
        wkeys[('wo', l)] = conv_flat(w_out_bf[l], w_out[l], ('w_out_bf', l))
        if STAGE < 4:
            return
        for e in range(int(os.environ.get("KNE", "16"))):
            wkeys[('moe', l, e)] = (conv_flat(moe_g_bf[l, e], moe_g[l, e], ('moe_g_bf', l, e))
                                    + conv_flat(moe_u_bf[l, e], moe_u[l, e], ('moe_u_bf', l, e))
                                    + conv_flat(moe_d_bf[l, e], moe_d[l, e], ('moe_d_bf', l, e)))

    if STAGE >= 2:
        conv_w_in(0)
    if STAGE >= 3:
        conv_rest(0)

    def rmsnorm_rstd(xt_s, junk, ss, rs, nfeat, tag):
        pb.op('scalar', lambda e: e.activation(out=junk, in_=xt_s[0], func=AF.Square),
              reads=xt_s[1], writes=[tag + 'junk'])
        pb.op('vector', lambda e: e.reduce_sum(out=ss, in_=junk, axis=AX.X), reads=[tag + 'junk'], writes=[tag + 'ss'])
        pb.op('vector', lambda e: e.tensor_scalar(out=rs, in0=ss, scalar1=1.0 / nfeat, scalar2=EPS,
                                                  op0=ALU.mult, op1=ALU.add), reads=[tag + 'ss'], writes=[tag + 'rs'])
        pb.op('scalar', lambda e: e.activation(out=rs, in_=rs, func=AF.Sqrt), reads=[tag + 'rs'], writes=[tag + 'rs'])
        pb.op('vector', lambda e: e.reciprocal(out=rs, in_=rs), reads=[tag + 'rs'], writes=[tag + 'rs'])

    def phase_A(l):
        base = ar.mark()
        Wsb = ar.alloc([8, NIN], BF16)
        xt = ar.alloc([4, D], F32)
        xn = ar.alloc([4, D], BF16)
        nT = ar.alloc([8, 512], BF16)
        junk = ar.alloc([D], F32)
        stg = [ar.alloc([4, 512], BF16) for _ in range(2)]
        vg = ar.alloc([512], F32)
        vstg = ar.alloc([4, 512], BF16)
        kvstg = ar.alloc([4, 256], BF16)
        gstg = ar.alloc([4, 24], F32)
        rowg = ar.alloc([512], F32)
        ss = ar.alloc([8], F32)
        rs = ar.alloc([8], F32)
        pb.dma('sync', vec, vecs[l], 'vec', writes=['vec'])
        pb.dma('sync', rowg, rows[l, :, 0:512], 'rowg', writes=['rowg'])
        for kc in range(8):
            pb.dma('sync', Wsb[:, kc, :], w_in_bf[l, kc * 128:(kc + 1) * 128, :], ('Wsb', kc),
                   reads=[('w_in_bf', l, kc)], writes=[('Wsb', kc)])
        Wk = [('Wsb', kc) for kc in range(8)]
        xsrc = x_in if l == 0 else xres
        psi = [0]

        def nextps():
            psi[0] = (psi[0] + 1) % 6
            return psi[0]

        groups = []
        for c4 in range(1):
            groups.append((OFF['u'], 4, AF.Gelu_apprx_tanh, uT, 0))
            groups.append((OFF['gb'], 4, AF.Gelu_apprx_tanh, gbT, 0))
            groups.append((OFF['rb'], 4, None, rbT, 0))
            groups.append((OFF['q'], 4, None, qT, 0))
            groups.append((OFF['xd'], 4, None, xdT, 0))
        for b in range(8):
            groups.append((OFF['mg'] + b * 512, 4, AF.Sigmoid, mgT, b * 512))
        kvT2 = kvT.rearrange("k p t -> (k p) t")
        kv_cols = [OFF['kcmp'], OFF['vcmp'], OFF['kslc'], OFF['kwin']]

        KA = int(os.environ.get("KA", "9"))
        for jt in range(int(os.environ.get("KJT", "8"))):
            t0 = jt * 512
            pb.dma('sync', xt, xsrc[t0:t0 + 512, :].rearrange("(s p) d -> p s d", p=128), 'xtA',
                   reads=[('xres', jt)] if l > 0 else [], writes=['xtA'])
            if KA < 2:
                continue
            for s in range(4):
                rmsnorm_rstd((xt[:, s, :], ['xtA']), junk, ss[:, s:s + 1], rs[:, s:s + 1], D, 'A%d' % s)
                pb.op('vector', lambda e, s=s: e.tensor_scalar(out=xn[:, s, :], in0=xt[:, s, :], scalar1=rs[:, s:s + 1],
                                                              scalar2=None, op0=ALU.mult),
                      reads=['xtA', 'A%drs' % s], writes=[('xn', s)])
            if KA < 3:
                continue
            for kc in range(8):
                bank = 6 + (kc % 2)
                pst = psb[bank][:].bitcast(BF16)
                for s in range(4):
                    pb.op('tensor', lambda e, s=s, kc=kc, pst=pst: e.transpose(
                        out=pst[:, s * 128:(s + 1) * 128], in_=xn[:, s, kc * 128:(kc + 1) * 128], identity=identb),
                        reads=[('xn', s), 'identb'], writes=[PS(bank)])
                pb.op('vector', lambda e, kc=kc, pst=pst: e.tensor_scalar(
                    out=nT[:, kc, :], in0=pst[:, 0:512], scalar1=vec[:, kc:kc + 1], scalar2=None, op0=ALU.mult),
                    reads=[PS(bank), 'vec'], writes=[('nT', kc)])
            nTk = [('nT', kc) for kc in range(8)]
            gi = 0

            def fm_group(col0, nch, func, dest, row0, cols_list=None):
                nonlocal gi
                slot = gi % 2
                gi += 1
                st = stg[slot]
                for c in range(nch):
                    cc0 = cols_list[c] if cols_list else col0 + c * 128
                    bank = nextps()
                    for kc in range(8):
                        pb.op('tensor', lambda e, kc=kc, cc0=cc0, bank=bank: e.matmul(
                            psb[bank][:], lhsT=Wsb[:, kc, cc0:cc0 + 128], rhs=nT[:, kc, :],
                            start=(kc == 0), stop=(kc == 7)),
                            reads=[Wk[kc], nTk[kc]], writes=[PS(bank)])
                    if func is None:
                        pb.op('vector', lambda e, c=c, bank=bank, st=st: e.tensor_copy(out=st[:, c, :], in_=psb[bank][:]),
                              reads=[PS(bank)], writes=[('stg', slot, c)])
                    else:
                        pb.op('scalar', lambda e, c=c, bank=bank, st=st, func=func: e.activation(
                            out=st[:, c, :], in_=psb[bank][:], func=func),
                            reads=[PS(bank)], writes=[('stg', slot, c)])
                pb.dma('sync', dest[row0:row0 + nch * 128, t0:t0 + 512].rearrange("(c p) t -> p c t", p=128),
                       st[:, 0:nch, :], ('stg', slot), reads=[('stg', slot, c) for c in range(nch)],
                       writes=[(dest.name, row0, jt)])

            if KA < 4:
                continue
            for (col0, nch, func, dest, row0) in (groups if KA >= 5 else groups[:1]):
                fm_group(col0, nch, func, dest, row0)
            if KA < 6:
                continue
            fm_group(0, 4, None, kvT2, 0, cols_list=kv_cols)
            if KA < 7:
                continue
            for s in range(4):
                bank = nextps()
                for kc in range(8):
                    pb.op('tensor', lambda e, kc=kc, s=s, bank=bank: e.matmul(
                        psb[bank][:], lhsT=nT[:, kc, s * 128:(s + 1) * 128], rhs=Wsb[:, kc, OFF['v']:OFF['v'] + 512],
                        start=(kc == 0), stop=(kc == 7)), reads=[Wk[kc], nTk[kc]], writes=[PS(bank)])
                pb.op('scalar', lambda e, bank=bank: e.activation(out=vg, in_=psb[bank][:], func=AF.Gelu_apprx_tanh),
                      reads=[PS(bank)], writes=['vg'])
                rmsnorm_rstd((vg, ['vg']), junk[:, 0:512], ss[:, 4:5], rs[:, 4:5], MIX, 'Av')
                pb.op('vector', lambda e: e.tensor_scalar(out=vg, in0=vg, scalar1=rs[:, 4:5], scalar2=None, op0=ALU.mult),
                      reads=['vg', 'Avrs'], writes=['vg'])
                pb.op('vector', lambda e, s=s: e.tensor_tensor(out=vstg[:, s, :], in0=vg, in1=rowg, op=ALU.mult),
                      reads=['vg', 'rowg'], writes=[('vstg', s)])
                if KA < 8:
                    continue
                bank = nextps()
                for (c0, n, o0) in ((OFF['vslc'], 128, 0), (OFF['vwin'], 128, 128), (OFF['ng'], 24, 256)):
                    for kc in range(8):
                        pb.op('tensor', lambda e, kc=kc, s=s, bank=bank, c0=c0, n=n, o0=o0: e.matmul(
                            psb[bank][:, o0:o0 + n], lhsT=nT[:, kc, s * 128:(s + 1) * 128], rhs=Wsb[:, kc, c0:c0 + n],
                            start=(kc == 0), stop=(kc == 7)), reads=[Wk[kc], nTk[kc]], writes=[PS(bank)])
                pb.op('vector', lambda e, s=s, bank=bank: e.tensor_copy(out=kvstg[:, s, :], in_=psb[bank][:, 0:256]),
                      reads=[PS(bank)], writes=[('kvstg', s)])
                pb.op('scalar', lambda e, s=s, bank=bank: e.activation(out=gstg[:, s, :], in_=psb[bank][:, 256:280],
                                                                      func=AF.Sigmoid),
                      reads=[PS(bank)], writes=[('gstg', s)])
            if KA < 9:
                continue
            pb.dma('sync', vn[t0:t0 + 512, :].rearrange("(s p) c -> p s c", p=128), vstg, 'vstg',
                   reads=[('vstg', s) for s in range(4)], writes=[('vn', jt)])
            pb.dma('sync', vtok[t0:t0 + 512, :].rearrange("(s p) c -> p s c", p=128), kvstg, 'kvstg',
                   reads=[('kvstg', s) for s in range(4)], writes=[('vtok', jt)])
            pb.dma('sync', gC[t0:t0 + 512, :].rearrange("(s p) c -> p s c", p=128), gstg, 'gstg',
                   reads=[('gstg', s) for s in range(4)], writes=[('gC', jt)])
        pb.barrier()
        ar.reset(base)


    def mm(out, lhsT, rhs, start, stop, reads, bank):
        pb.op('tensor', lambda e: e.matmul(out, lhsT=lhsT, rhs=rhs, start=start, stop=stop),
              reads=reads, writes=[PS(bank)])

    def vop(fn, reads, writes, eng='vector'):
        pb.op(eng, fn, reads=reads, writes=writes)

    def phase_BA(l):
        base = ar.mark()
        vnsb = ar.alloc([32, 512], BF16)
        usb = ar.alloc([4, S], BF16)
        ya = ar.alloc([4, S], BF16)
        wTf = ar.alloc([4, 128], F32)
        wTm = ar.alloc([4, 128], BF16)
        rowb = ar.alloc([512], F32)
        bsb = ar.alloc([512], BF16)
        ones1 = ar.alloc([128], BF16)
        pb.dma_split('sync', vnsb, vn.rearrange("(ch s) c -> s ch c", s=128), 'vnsb', 8, writes=['vnsb'])
        pb.dma('sync', usb, uT.rearrange("(g c) t -> c g t", c=128), 'usb', writes=['usb'])
        pb.dma('sync', wTf, gm_wsT[l].rearrange("g s t -> s g t"), 'wTf', writes=['wTf'])
        pb.dma('sync', rowb[0:1, :], rows[l, 0:1, 512:1024], 'rowb', writes=['rowb'])
        for g in range(4):
            vop(lambda e, g=g: e.tensor_tensor(out=wTm[:, g, :], in0=wTf[:, g, :], in1=tril4[:, 0:128], op=ALU.mult),
                ['wTf', 'tril4'], [('wTm', g)])
        vop(lambda e: e.tensor_copy(out=bsb[0:1, :], in_=rowb[0:1, :]), ['rowb'], ['bsb'])
        vop(lambda e: e.memset(ones1[0:1, :], 1.0), [], ['ones1'])
        for g in range(4):
            for ch4 in range(8):
                bank = (g * 8 + ch4) % 4
                for cq in range(4):
                    ch = ch4 * 4 + cq
                    mm(psb[bank][:, cq * 128:(cq + 1) * 128], vnsb[:, ch, g * 128:(g + 1) * 128], wTm[:, g, :],
                       True, False, ['vnsb', ('wTm', g)], bank)
                    mm(psb[bank][:, cq * 128:(cq + 1) * 128], ones1[0:1, :], bsb[0:1, g * 128:(g + 1) * 128],
                       False, True, ['ones1', 'bsb'], bank)
                vop(lambda e, g=g, ch4=ch4, bank=bank: e.tensor_tensor(
                    out=ya[:, g, ch4 * 512:(ch4 + 1) * 512], in0=psb[bank][:], in1=usb[:, g, ch4 * 512:(ch4 + 1) * 512],
                    op=ALU.mult), [PS(bank), 'usb'], [('ya', g, ch4)])
        pb.dma('sync', yT[0:512, :].rearrange("(g c) t -> c g t", c=128), ya, 'ya_out',
               reads=[('ya', g, c) for g in range(4) for c in range(8)], writes=[])
        pb.barrier()
        ar.reset(base)

    def phase_BB(l):
        base = ar.mark()
        xpad = ar.alloc([S + 8], BF16)
        xc = ar.alloc([S], F32)
        xcb = ar.alloc([S], BF16)
        rr = ar.alloc([S], F32)
        ii = ar.alloc([S], F32)
        tt = ar.alloc([S], F32)
        gb = ar.alloc([S], BF16)
        yb = ar.alloc([S], BF16)
        BDf = ar.alloc([2, 128], F32)
        BDb = ar.alloc([2, 128], BF16)
        sm = ar.alloc([8, 4], F32)
        vop(lambda e: e.memset(xpad[:, 0:8], 0.0), [], ['xpad0'])
        lam = vec[:, 44:48]
        z, az, ez, mz, negc = sm[:, 0, :], sm[:, 1, :], sm[:, 2, :], sm[:, 3, :], sm[:, 4, :]
        vop(lambda e: e.tensor_scalar(out=z, in0=lam, scalar1=-1.0, scalar2=None, op0=ALU.mult), ['vec'], ['z'])
        vop(lambda e: e.tensor_tensor(out=az, in0=z, in1=lam, op=ALU.max), ['z', 'vec'], ['az'])
        pb.op('scalar', lambda e: e.activation(out=ez, in_=az, func=AF.Exp, scale=-1.0), ['az'], ['ez'])
        vop(lambda e: e.tensor_scalar(out=ez, in0=ez, scalar1=1.0, scalar2=None, op0=ALU.add), ['ez'], ['ez'])
        pb.op('scalar', lambda e: e.activation(out=ez, in_=ez, func=AF.Ln), ['ez'], ['ez'])
        vop(lambda e: e.tensor_scalar(out=mz, in0=z, scalar1=0.0, scalar2=None, op0=ALU.max), ['z'], ['mz'])
        vop(lambda e: e.tensor_tensor(out=mz, in0=mz, in1=ez, op=ALU.add), ['mz', 'ez'], ['mz'])
        vop(lambda e: e.tensor_scalar(out=negc, in0=mz, scalar1=-8.0, scalar2=None, op0=ALU.mult), ['mz'], ['negc'])
        for cc in range(4):
            r0 = cc * 128
            pb.dma('sync', xpad[:, 8:], rbT[r0:r0 + 128, :], 'xpad', writes=['xpad'])
            pb.dma('sync', gb, gbT[r0:r0 + 128, :], 'gbsb', writes=['gbsb'])
            vop(lambda e: e.memset(BDf, 0.0), [], ['BDf'])
            pairs = []
            for h in range(2):
                p0 = h * 64
                pairs.append((BDf[p0:p0 + 64, 0, p0:p0 + 64], lru_wa[l, 2 * cc + h]))
                pairs.append((BDf[p0:p0 + 64, 1, p0:p0 + 64], lru_wx[l, 2 * cc + h]))
            pb.dma_batch('sync', pairs, 'BDf', reads=['BDf'], writes=['BDfd'])
            vop(lambda e: e.tensor_copy(out=BDb, in_=BDf), ['BDf', 'BDfd'], ['BDb'])
            vop(lambda e, cc=cc: e.tensor_scalar(out=xc, in0=xpad[:, 5:5 + S], scalar1=vec[:, 16 + cc:17 + cc],
                                                 scalar2=vec[:, 32 + cc:33 + cc], op0=ALU.mult, op1=ALU.add),
                ['xpad', 'xpad0', 'vec'], ['xc'])
            for k in range(1, 4):
                vop(lambda e, cc=cc, k=k: e.tensor_scalar(out=tt, in0=xpad[:, 5 + k:5 + k + S],
                                                          scalar1=vec[:, 16 + 4 * k + cc:17 + 4 * k + cc], scalar2=None, op0=ALU.mult),
                    ['xpad', 'xpad0', 'vec'], ['tt'], eng='gpsimd')
                vop(lambda e: e.tensor_tensor(out=xc, in0=xc, in1=tt, op=ALU.add), ['xc', 'tt'], ['xc'])
            pb.op('scalar', lambda e: e.activation(out=xcb, in_=xc, func=AF.Copy), ['xc'], ['xcb'])
            for tq in range(8):
                ts_ = slice(tq * 512, (tq + 1) * 512)
                for k, (dst, bcol) in enumerate(((rr, 36 + cc), (ii, 40 + cc))):
                    bank = (tq * 2 + k) % 6
                    mm(psb[bank][:], BDb[:, k, :], xcb[:, ts_], True, True, ['BDb', 'xcb'], bank)
                    pb.op('scalar', lambda e, dst=dst, bcol=bcol, bank=bank, ts_=ts_: e.activation(
                        out=dst[:, ts_], in_=psb[bank][:], func=AF.Sigmoid, bias=vec[:, bcol:bcol + 1]),
                        [PS(bank), 'vec'], [('ri', k, tq)])
            allri = [('ri', k, tq) for k in range(2) for tq in range(8)]
            pb.op('scalar', lambda e, cc=cc: e.activation(out=rr, in_=rr, func=AF.Exp, scale=negc[:, cc:cc + 1]),
                  allri + ['negc'], ['rr'])
            vop(lambda e: e.tensor_tensor(out=tt, in0=rr, in1=rr, op=ALU.mult), ['rr'], ['tt'])
            vop(lambda e: e.tensor_scalar(out=tt, in0=tt, scalar1=-1.0, scalar2=1.0, op0=ALU.mult, op1=ALU.add), ['tt'], ['tt'])
            pb.op('scalar', lambda e: e.activation(out=tt, in_=tt, func=AF.Sqrt), ['tt'], ['tt'])
            vop(lambda e: e.tensor_tensor(out=ii, in0=ii, in1=xc, op=ALU.mult), allri + ['xc'], ['ii'])
            vop(lambda e: e.tensor_tensor(out=ii, in0=ii, in1=tt, op=ALU.mult), ['ii', 'tt'], ['ii'])
            vop(lambda e: e.tensor_tensor_scan(out=tt, data0=rr, data1=ii, initial=0.0, op0=ALU.mult, op1=ALU.add),
                ['rr', 'ii', 'tt'], ['tt'])
            vop(lambda e: e.tensor_tensor(out=yb, in0=tt, in1=gb, op=ALU.mult), ['tt', 'gbsb'], ['yb'])
            pb.dma('sync', yT[512 + r0:512 + r0 + 128, :], yb, 'yb_out', reads=['yb'], writes=[])
        pb.barrier()
        ar.reset(base)

    def phase_BD(l):
        base = ar.mark()
        xp = ar.alloc([S + 16], BF16)
        A = ar.alloc([S + 16], F32)
        B = ar.alloc([S + 16], F32)
        plb = ar.alloc([S], BF16)
        yd = ar.alloc([S], BF16)
        pw = ar.alloc([4, 128], BF16)
        pcor = ar.alloc([4, 16], F32)
        pb.dma('gpsimd', pw, pool_w[l].rearrange("g i j -> i g j"), 'pw', writes=['pw'])
        pb.dma('sync', pcor, c_pcorr[:, :, :], 'pcor', writes=['pcor'])
        vop(lambda e: e.memset(A[:, 0:16], 0.0), [], ['A0'])
        vop(lambda e: e.memset(B[:, 0:16], 0.0), [], ['B0'])
        for gi in range(4):
            r0 = gi * 128
            w = 2 ** (gi + 1)
            pb.dma('sync', xp[:, 16:], xdT[r0:r0 + 128, :], 'xp', writes=['xp'])
            vop(lambda e: e.tensor_copy(out=A[:, 16:], in_=xp[:, 16:]), ['xp'], ['A'])
            cur, nxt, ck, nk = A, B, 'A', 'B'
            for k in range(gi + 1):
                sh = 2 ** k
                vop(lambda e, cur=cur, nxt=nxt, sh=sh: e.tensor_tensor(
                    out=nxt[:, 16:], in0=cur[:, 16:], in1=cur[:, 16 - sh:16 - sh + S], op=ALU.add),
                    [ck, 'A0', 'B0'], [nk])
                cur, nxt, ck, nk = nxt, cur, nk, ck
            vop(lambda e, cur=cur, nxt=nxt, w=w: e.tensor_scalar(out=nxt[:, 16:], in0=cur[:, 16:], scalar1=1.0 / w,
                                                               scalar2=None, op0=ALU.mult), [ck], [nk])
            vop(lambda e, cur=cur, nxt=nxt, gi=gi: e.tensor_tensor(out=nxt[:, 16:32], in0=cur[:, 16:32], in1=pcor[:, gi, :],
                                                                 op=ALU.mult), [ck, nk, 'pcor'], [nk])
            vop(lambda e, nxt=nxt: e.tensor_tensor(out=plb, in0=nxt[:, 16:], in1=xp[:, 16:], op=ALU.subtract),
                [nk, 'xp'], ['plb'])
            vop(lambda e, nxt=nxt: e.memset(nxt[:, 0:16], 0.0), [nk], [nk, 'A0', 'B0'])
            for tq in range(8):
                ts_ = slice(tq * 512, (tq + 1) * 512)
                bank = tq % 6
                mm(psb[bank][:], pw[:, gi, :], plb[:, ts_], True, True, ['pw', 'plb'], bank)
                vop(lambda e, bank=bank, ts_=ts_, gi=gi: e.tensor_scalar(out=yd[:, ts_], in0=psb[bank][:],
                                                                        scalar1=vec[:, 48 + gi:49 + gi], scalar2=None, op0=ALU.mult),
                    [PS(bank), 'vec'], [('yd', tq)])
            pb.dma('sync', yT[1536 + r0:1536 + r0 + 128, :], yd, 'yd_out', reads=[('yd', tq) for tq in range(8)], writes=[])
        pb.barrier()
        ar.reset(base)


    def phase_BC(l):
        base = ar.mark()
        NQ = int(os.environ.get("KNQ", str(NQT)))
        pe_sb = ar.alloc([2, 32], BF16)
        gates = ar.alloc([NQT, 24], F32)
        XT = ar.alloc([S], BF16)
        w1sb = ar.alloc([32, 64], BF16)
        w2sb = ar.alloc([2, 64], BF16)
        hidT = ar.alloc([256], BF16)
        KcT = ar.alloc([256], BF16)
        Vca_full = ar.alloc([2, 130], BF16)
        Vca = Vca_full[:, :, 0:129]
        biasb = ar.alloc([8], F32)
        Qa = ar.alloc([4, S], BF16)
        Ka = ar.alloc([S], BF16)
        Kw = ar.alloc([S], BF16)
        Vs_full = ar.alloc([NQT, 66], BF16)
        Vw_full = ar.alloc([NQT, 66], BF16)
        Vs = Vs_full[:, :, 0:65]
        Vw = Vw_full[:, :, 0:65]
        ycT = ar.alloc([2, S], BF16)
        cmk = [ar.alloc([2, 512], BF16) for _ in range(2)]
        sCB = [ar.alloc([2, 64], F32) for _ in range(2)]
        pbuf = [ar.alloc([512], BF16) for _ in range(3)]
        sm = ar.alloc([16, 4], F32)
        imp = ar.alloc([64], F32)
        tmp64 = ar.alloc([64], F32)
        m8 = ar.alloc([8], F32)
        Mpad = ar.alloc([128], BF16)
        oacc = ar.alloc([256], F32)
        otmp = ar.alloc([256], F32)
        ycb = ar.alloc([256], BF16)
        pb.dma('gpsimd', pe_sb[0:64], cmp_peT[l].rearrange("c d l -> d c l"), 'pe_sb', writes=['pe_sb'])
        pb.dma_split('sync', gates, gC.rearrange("(qt q) c -> q qt c", q=128), 'gates', 8, writes=['gates'])
        vop(lambda e: e.memset(Mpad, 0.0), [], ['Mpad'])
        vop(lambda e: e.memset(hidT, 0.0), [], ['hidT'])
        XT16 = XT.rearrange("p (n s) -> p n s", s=16)
        for g in range(2):
            pb.dma('gpsimd', w2sb[0:64], cmp_w2[l, :, g].rearrange("c e d -> e c d"), 'w2sb', writes=['w2sb'])
            pb.dma('sync', Vca[:, :, 64:129], c_ovc[:, :, :], 'Vca_c', writes=['Vca_c', ('Vca', 0), ('Vca', 1)])
            for c in range(2):
                pb.dma('sync', XT[0:64, :], kvT[c, g * 64:(g + 1) * 64, :], 'XT', writes=['XT'])
                pb.dma_split('sync', w1sb[0:64], cmp_w1_bf[l, c, g].rearrange("(l d) e -> d l e", d=64), 'w1sb', 4,
                             reads=wkeys[('w1', l)], writes=['w1sb'])
                for li in range(32):
                    mm(psb[7][0:64, 0:1], w1sb[0:64, li, :], pe_sb[0:64, c, li:li + 1], li == 0, li == 31, ['w1sb', 'pe_sb'], 7)
                vop(lambda e: e.tensor_copy(out=biasb[0:64, 0:1], in_=psb[7][0:64, 0:1]), [PS(7)], ['biasb'])
                for li in range(32):
                    rhs = XT16[0:64, 0:255, li] if li < 16 else XT16[0:64, 1:256, li - 16]
                    mm(psb[6][0:64, 0:255], w1sb[0:64, li, :], rhs, li == 0, li == 31, ['w1sb', 'XT'], 6)
                pb.op('scalar', lambda e: e.activation(out=hidT[0:64, 0:255], in_=psb[6][0:64, 0:255],
                                                       func=AF.Gelu_apprx_tanh, bias=biasb[0:64, 0:1]),
                      [PS(6), 'biasb'], ['hidT'])
                if c == 0:
                    mm(psb[5][0:64, 0:256], w2sb[0:64, 0, :], hidT[0:64, 0:256], True, True, ['w2sb', 'hidT'], 5)
                    vop(lambda e: e.tensor_copy(out=KcT[0:64, :], in_=psb[5][0:64, 0:256]), [PS(5)], ['KcT'])
                else:
                    for kt in range(2):
                        mm(psb[5][:, kt * 64:(kt + 1) * 64], hidT[0:64, kt * 128:(kt + 1) * 128], w2sb[0:64, 1, :],
                           True, True, ['w2sb', 'hidT'], 5)
                    for kt in range(2):
                        vop(lambda e, kt=kt: e.tensor_copy(out=Vca[:, kt, 0:64], in_=psb[5][:, kt * 64:(kt + 1) * 64]),
                            [PS(5), 'Vca_c'], [('Vca', kt)])
            pb.dma('sync', Qa[0:64], qT[g * 256:(g + 1) * 256, :].rearrange("(j d) t -> d j t", d=64), 'Qa', writes=['Qa'])
            pb.dma_batch('sync', [(Ka[0:64, :], kvT[2, g * 64:(g + 1) * 64, :]), (Ka[64:128, :], c_eblk[:, :])], 'Ka', writes=['Ka'])
            pb.dma('sync', Kw[0:64, :], kvT[3, g * 64:(g + 1) * 64, :], 'Kw', writes=['Kw'])
            pb.dma_split('sync', Vs[:, :, 0:64], vtok[:, g * 64:(g + 1) * 64].rearrange("(kt k) d -> k kt d", k=128), 'Vs', 8, writes=['Vs', 'Vs1'])
            pb.dma_split('sync', Vw[:, :, 0:64], vtok[:, 128 + g * 64:128 + (g + 1) * 64].rearrange("(kt k) d -> k kt d", k=128),
                         'Vw', 8, writes=['Vw', 'Vw1'])
            vop(lambda e: e.memset(Vs_full[:, :, 64:66], 1.0), ['Vs'], ['Vs1'])
            vop(lambda e: e.memset(Vw_full[:, :, 64:66], 1.0), ['Vw'], ['Vw1'])
            if debug and g == 0 and l == 0 and os.environ.get("KDBGBC") == "1":
                for nm, t_, shp, rk in (('dbgKa', Ka, [128, S], ['Ka']), ('dbgKw', Kw, [128, S], ['Kw']),
                                        ('dbgVs', Vs_full, [128, NQT, 66], ['Vs', 'Vs1']), ('dbgVw', Vw_full, [128, NQT, 66], ['Vw', 'Vw1']),
                                        ('dbgKcT', KcT, [128, 256], ['KcT']), ('dbgVca', Vca_full, [128, 2, 130], [('Vca', 0), ('Vca', 1), 'Vca_c'])):
                    dd = dscr(nm, shp, BF16)
                    pb.dma('sync', dd, t_, nm, reads=rk, writes=[])
                dd = dscr('dbgQa', [64, 4, S], BF16)
                pb.dma('sync', dd, Qa[0:64], 'dbgQa', reads=['Qa'], writes=[])
            sci = [0]

            def nextsc():
                sci[0] = (sci[0] + 1) % 3
                return sci[0]
            pbi = [0]

            def nextpb():
                pbi[0] = (pbi[0] + 1) % 3
                return pbi[0]

            def poc(j):
                return psb[3 + j // 2][:, (j % 2) * 129:(j % 2) * 129 + 129]

            for i in range(NQ):
                qs = slice(i * 128, (i + 1) * 128)
                sl = i % 2
                pb.dma('sync', cmk[sl], c_cmpmask[i], ('cmk', sl), writes=[('cmk', sl)])
                pb.dma_batch('sync', [(sCB[sl][:, 0, :], c_selC[qs, :]), (sCB[sl][:, 1, :], c_selB[qs, :])], ('sCB', sl),
                             writes=[('sCB', sl)])
                gt = gates[:, i, g * 12:(g + 1) * 12].rearrange("p (j b) -> p j b", b=3)
                nkt = 2 if i >= 16 else 1
                for kt in range(nkt):
                    b = nextsc()
                    mm(psb[b][:], KcT[0:64, kt * 128:(kt + 1) * 128], Qa[0:64, :, qs], True, True, ['KcT', 'Qa'], b)
                    pi = nextpb()
                    P = pbuf[pi]
                    pb.op('scalar', lambda e, P=P, b=b: e.activation(out=P, in_=psb[b][:], func=AF.Exp, scale=0.125),
                          [PS(b)], [('P', pi)])
                    vop(lambda e, P=P, kt=kt, sl=sl: e.tensor_tensor(out=P, in0=P, in1=cmk[sl][:, kt, :], op=ALU.mult),
                        [('P', pi), ('cmk', sl)], [('P', pi)])
                    for j in range(4):
                        mm(poc(j), P[:, j * 128:(j + 1) * 128], Vca[:, kt, :], kt == 0 and j % 2 == 0,
                           kt == nkt - 1 and j % 2 == 1, [('P', pi), ('Vca', kt), 'Vca_c'], 3 + j // 2)
                brs = ((5, Ka, 128, Vs, 'Vs', 0), (6, Kw, 64, Vw, 'Vw', max(0, i - 4)))

                def stream(br):
                    bank, Kt, kparts, Vt, vkey, kt0 = brs[br]
                    kts = list(range(kt0, i + 1))
                    for kt in kts:
                        b = nextsc()
                        mm(psb[b][:], Kt[0:kparts, kt * 128:(kt + 1) * 128], Qa[0:kparts, :, qs], True, True,
                           ['Ka' if br == 0 else 'Kw', 'Qa'], b)
                        pi = nextpb()
                        P = pbuf[pi]
                        pb.op('scalar', lambda e, P=P, b=b: e.activation(out=P, in_=psb[b][:], func=AF.Exp, scale=0.125),
                              [PS(b)], [('P', pi)])
                        msk = None
                        if kt == i:
                            msk, mk = tril4, 'tril4'
                        elif br == 1 and kt == i - 4:
                            msk, mk = triu4, 'triu4'
                        if msk is not None:
                            vop(lambda e, P=P, msk=msk: e.tensor_tensor(out=P, in0=P, in1=msk, op=ALU.mult), [('P', pi), mk], [('P', pi)])
                        for j in range(4):
                            mm(psb[bank][:, j * 65:(j + 1) * 65], P[:, j * 128:(j + 1) * 128], Vt[:, kt, :],
                               kt == kts[0] and j == 0, kt == i and j == 3, [('P', pi), vkey, vkey + '1'], bank)

                stream(1)
                Zc, rZc, fC, rZs, fS = sm[:, 0, :], sm[:, 1, :], sm[:, 2, :], sm[:, 3, :], sm[:, 4, :]
                thr = sm[:, 5, 0:1]
                for h2 in range(2):
                    zv = psb[3 + h2][:, 0:258].rearrange("p (j c) -> p j c", c=129)[:, :, 128]
                    vop(lambda e, h2=h2, zv=zv: e.tensor_scalar(out=Zc[:, 2 * h2:2 * h2 + 2], in0=zv, scalar1=1e-30, scalar2=None,
                                                               op0=ALU.max), [PS(3 + h2)], [('Zc', h2)])
                vop(lambda e: e.reciprocal(out=rZc, in_=Zc), [('Zc', 0), ('Zc', 1)], ['rZc'])
                for j in range(4):
                    dst = imp if j == 0 else tmp64
                    vop(lambda e, j=j, dst=dst: e.tensor_scalar(out=dst, in0=poc(j)[:, 64:128], scalar1=rZc[:, j:j + 1],
                                                                scalar2=None, op0=ALU.mult),
                        [PS(3 + j // 2), 'rZc'], ['imp' if j == 0 else 'tmp64'])
                    if j > 0:
                        vop(lambda e: e.tensor_tensor(out=imp, in0=imp, in1=tmp64, op=ALU.add), ['imp', 'tmp64'], ['imp'])
                vop(lambda e, sl=sl: e.tensor_tensor(out=imp, in0=imp, in1=sCB[sl][:, 0, :], op=ALU.mult), ['imp', ('sCB', sl)], ['imp'])
                vop(lambda e, sl=sl: e.tensor_tensor(out=imp, in0=imp, in1=sCB[sl][:, 1, :], op=ALU.add), ['imp', ('sCB', sl)], ['imp'])
                vop(lambda e: e.max(out=m8, in_=imp), ['imp'], ['m8'])
                vop(lambda e: e.tensor_scalar(out=thr, in0=m8[:, 7:8], scalar1=0.0, scalar2=None, op0=ALU.max), ['m8'], ['thr'])
                vop(lambda e: e.tensor_scalar(out=Mpad[:, 64:128], in0=imp, scalar1=thr, scalar2=-BIG, op0=ALU.is_lt, op1=ALU.mult),
                    ['imp', 'thr'], ['Mpad'])
                pst = psb[7][:].bitcast(BF16)
                pb.op('tensor', lambda e, pst=pst: e.transpose(out=pst[:, 0:128], in_=Mpad, identity=identb),
                      ['Mpad', 'identb'], [PS(7)])
                for j in range(4):
                    if j % 2 == 0:
                        pb.op('scalar', lambda e, j=j, pst=pst, qs=qs: e.activation(out=Qa[64:128, j, qs], in_=pst[64:128, 0:128], func=AF.Copy),
                              [PS(7)], ['Qa'])
                    else:
                        vop(lambda e, j=j, pst=pst, qs=qs: e.tensor_copy(out=Qa[64:128, j, qs], in_=pst[64:128, 0:128]), [PS(7)], ['Qa'])
                vop(lambda e, gt=gt: e.tensor_tensor(out=fC, in0=rZc, in1=gt[:, :, 0], op=ALU.mult), ['rZc', 'gates'], ['fC'])
                for j in range(4):
                    pb.op('scalar', lambda e, j=j: e.activation(out=oacc[:, j * 64:(j + 1) * 64], in_=poc(j)[:, 0:64], func=AF.Copy,
                                                                scale=fC[:, j:j + 1]), [PS(3 + j // 2), 'fC'], [('oacc', j)])
                def combine(br):
                    bank = brs[br][0]
                    zv = psb[bank][:, 0:260].rearrange("p (j c) -> p j c", c=65)[:, :, 64]
                    vop(lambda e, zv=zv: e.reciprocal(out=rZs, in_=zv), [PS(bank)], ['rZs'])
                    vop(lambda e, gt=gt, br=br: e.tensor_tensor(out=fS, in0=rZs, in1=gt[:, :, 1 + br], op=ALU.mult), ['rZs', 'gates'], ['fS'])
                    for j in range(4):
                        pb.op('scalar', lambda e, j=j, bank=bank: e.activation(
                            out=otmp[:, j * 64:(j + 1) * 64], in_=psb[bank][:, j * 65:j * 65 + 64], func=AF.Copy, scale=fS[:, j:j + 1]),
                            [PS(bank), 'fS'], [('otmp', j)])
                    dst, dk = (oacc, 'oaccs') if br == 0 else (ycb, 'ycb')
                    vop(lambda e, dst=dst: e.tensor_tensor(out=dst, in0=oacc, in1=otmp, op=ALU.add),
                        [('oacc', j) for j in range(4)] + [('otmp', j) for j in range(4)] + ['oaccs'],
                        [dk] + ([('oacc', j) for j in range(4)] if br == 0 else []))

                stream(0)
                combine(0)
                combine(1)
                for c2 in range(2):
                    pb.op('tensor', lambda e, c2=c2, pst=pst: e.transpose(out=pst[:, 256 + c2 * 128:256 + (c2 + 1) * 128],
                                                                          in_=ycb[:, c2 * 128:(c2 + 1) * 128], identity=identb),
                          ['ycb', 'identb'], [PS(7)])
                vop(lambda e, pst=pst, qs=qs: e.tensor_copy(out=ycT[:, :, qs], in_=pst[:, 256:512].rearrange("p (c q) -> p c q", q=128)),
                    [PS(7)], ['ycT'])
            pb.dma('sync', yT[1024 + g * 256:1024 + (g + 1) * 256, :].rearrange("(c p) t -> p c t", p=128), ycT, 'yc_out',
                   reads=['ycT'], writes=[])
        pb.barrier()
        ar.reset(base)


    def phase_C(l):
        base = ar.mark()
        NJ = int(os.environ.get("KJC", "8"))
        Wb = ar.alloc([16, D], BF16)
        Wo = ar.alloc([8, D], BF16)
        ysb = ar.alloc([16, 512], BF16)
        gsb = ar.alloc([32, 512], BF16)
        xt = ar.alloc([4, D], F32)
        mT = ar.alloc([8, 512], BF16)
        macc = ar.alloc([512], F32)
        mtmp = ar.alloc([512], F32)
        pb.dma_split('sync', Wb, w_branch_bf[l].rearrange("(k p) d -> p k d", p=128), 'Wb', 4, reads=wkeys[('wb', l)], writes=['Wb'])
        pb.dma_split('sync', Wo, w_out_bf[l].rearrange("(k p) d -> p k d", p=128), 'Wo', 2, reads=wkeys[('wo', l)], writes=['Wo'])
        xsrc = x_in if l == 0 else xres
        for jt in range(NJ):
            t0 = jt * 512
            pb.dma_split('sync', ysb, yT[:, t0:t0 + 512].rearrange("(k p) t -> p k t", p=128), 'ysb', 4, writes=['ysb'])
            pb.dma_split('sync', gsb, mgT[:, t0:t0 + 512].rearrange("(k p) t -> p k t", p=128), 'gsb', 8, writes=['gsb'])
            pb.dma('sync', xt, xsrc[t0:t0 + 512, :].rearrange("(s p) d -> p s d", p=128), 'xtC', writes=['xtC'])
            for dmc in range(8):
                for b in range(4):
                    bank = b
                    for cc in range(4):
                        mm(psb[bank][:], Wb[:, b * 4 + cc, dmc * 128:(dmc + 1) * 128], ysb[:, b * 4 + cc, :], cc == 0, cc == 3,
                           ['Wb', 'ysb'], bank)
                    dst, dk = (macc, 'macc') if b == 0 else (mtmp, 'mtmp')
                    vop(lambda e, dst=dst, bank=bank, b=b, dmc=dmc: e.tensor_tensor(out=dst, in0=psb[bank][:], in1=gsb[:, b * 8 + dmc, :],
                                                                                    op=ALU.mult), [PS(bank), 'gsb'], [dk])
                    if b > 0:
                        o = mT[:, dmc, :] if b == 3 else macc
                        vop(lambda e, o=o: e.tensor_tensor(out=o, in0=macc, in1=mtmp, op=ALU.add), ['macc', 'mtmp'],
                            [('mT', dmc)] if b == 3 else ['macc'], eng='gpsimd')
            for s_ in range(4):
                for hf in range(2):
                    bank = 4 + (s_ * 2 + hf) % 4
                    for dmc in range(8):
                        mm(psb[bank][:], mT[:, dmc, s_ * 128:(s_ + 1) * 128], Wo[:, dmc, hf * 512:(hf + 1) * 512], dmc == 0, dmc == 7,
                           [('mT', dmc), 'Wo'], bank)
                    vop(lambda e, s_=s_, hf=hf, bank=bank: e.tensor_tensor(out=xt[:, s_, hf * 512:(hf + 1) * 512], in0=psb[bank][:],
                                                                          in1=xt[:, s_, hf * 512:(hf + 1) * 512], op=ALU.add),
                        [PS(bank), 'xtC'], [('xtCo', s_, hf)])
            pb.dma('sync', xres[t0:t0 + 512, :].rearrange("(s p) d -> p s d", p=128), xt, 'xtC_out',
                   reads=[('xtCo', s_, hf) for s_ in range(4) for hf in range(2)], writes=['xtC'])
        pb.barrier()
        ar.reset(base)

    def phase_D(l, last):
        base = ar.mark()
        NJ = int(os.environ.get("KJD", "8"))
        NE = int(os.environ.get("KNE", "16"))
        xt = ar.alloc([4, D], F32)
        xnf = ar.alloc([4, D], F32)
        n2b = ar.alloc([8, 512], BF16)
        xhi = ar.alloc([4, D], BF16)
        xlo = ar.alloc([4, D], BF16)
        hiT = ar.alloc([8, 512], BF16)
        loT = ar.alloc([8, 512], BF16)
        Wrh = ar.alloc([8, 20], BF16)
        Wrl = ar.alloc([8, 20], BF16)
        Wrt = ar.alloc([8, 20], F32)
        junk = ar.alloc([D], F32)
        Wr = ar.alloc([8, 20], F32)
        rbias = ar.alloc([20], F32)
        fng = ar.alloc([D], F32)
        Wg = [ar.alloc([8, 512], BF16) for _ in range(2)]
        Wu = [ar.alloc([8, 512], BF16) for _ in range(2)]
        Wd = [ar.alloc([4, D], BF16) for _ in range(2)]
        hid = [ar.alloc([4, 512], BF16) for _ in range(2)]
        sg = [ar.alloc([512], F32) for _ in range(2)]
        dtmp = [ar.alloc([512], F32) for _ in range(2)]
        ss = ar.alloc([8], F32)
        rs = ar.alloc([8], F32)
        lg = ar.alloc([4, 20], F32)
        comb = ar.alloc([4, 16], F32)
        sm = ar.alloc([16, 8], F32)
        m16 = ar.alloc([16], F32)
        e16 = ar.alloc([16], F32)
        mx8 = ar.alloc([8], F32)
        pb.dma_split('sync', Wr, router_w[l].rearrange("(k p) c -> p k c", p=128), 'Wr', 2, writes=['Wr'])
        for kc in range(8):
            vop(lambda e, kc=kc: e.tensor_scalar(out=Wr[:, kc, :], in0=Wr[:, kc, :], scalar1=vec[:, 8 + kc:9 + kc], scalar2=None,
                                                 op0=ALU.mult), ['Wr', 'vec'], ['Wr'])
        vop(lambda e: e.tensor_copy(out=Wrh, in_=Wr), ['Wr'], ['Wrh'])
        vop(lambda e: e.tensor_tensor(out=Wrt, in0=Wr, in1=Wrh, op=ALU.subtract), ['Wr', 'Wrh'], ['Wrt'])
        vop(lambda e: e.tensor_copy(out=Wrl, in_=Wrt), ['Wrt'], ['Wrl'])
        pb.dma('sync', rbias, rows[l, :, 1024:1044], 'rbias', writes=['rbias'])
        pb.dma('sync', fng, rows[l, :, 1044:2068], 'fng', writes=['fng'])
        for jt in range(NJ):
            t0 = jt * 512
            pb.dma('sync', xt, xres[t0:t0 + 512, :].rearrange("(s p) d -> p s d", p=128), 'xtD', writes=['xtD'])
            if int(os.environ.get("KD", "9")) <= 1:
                pb.dma('sync', out_d[t0:t0 + 512, :].rearrange("(s p) d -> p s d", p=128), xt, 'out_st', reads=['xtD'], writes=[])
                continue
            for s_ in range(4):
                rmsnorm_rstd((xt[:, s_, :], ['xtD']), junk, ss[:, s_:s_ + 1], rs[:, s_:s_ + 1], D, 'D%d' % s_)
                vop(lambda e, s_=s_: e.tensor_scalar(out=xnf[:, s_, :], in0=xt[:, s_, :], scalar1=rs[:, s_:s_ + 1], scalar2=None,
                                                     op0=ALU.mult), ['xtD', 'D%drs' % s_], [('xnf', s_)])
            KB = os.environ.get("KB", "z")
            if KB == "a":
                pb.dma('sync', out_d[t0:t0 + 512, :].rearrange("(s p) d -> p s d", p=128), xnf, 'out_st',
                       reads=[('xnf', s_) for s_ in range(4)], writes=[])
                continue
            for s_ in range(4):
                vop(lambda e, s_=s_: e.tensor_copy(out=xhi[:, s_, :], in_=xnf[:, s_, :]), [('xnf', s_)], [('xhi', s_)])
                vop(lambda e, s_=s_: e.tensor_tensor(out=xlo[:, s_, :], in0=xnf[:, s_, :], in1=xhi[:, s_, :], op=ALU.subtract),
                    [('xnf', s_), ('xhi', s_)], [('xlo', s_)])
            if KB == "b":
                pb.dma('sync', out_d[t0:t0 + 512, :].rearrange("(s p) d -> p s d", p=128), xnf, 'out_st',
                       reads=[('xnf', s_) for s_ in range(4)] + [('xlo', s_) for s_ in range(4)], writes=[])
                continue
            KEV = os.environ.get("KEV", "d")
            for kc in range(8):
                for hl, (src, sk) in enumerate(((xhi, 'xhi'), (xlo, 'xlo'))):
                    bank = 6 + hl
                    pst = psb[bank][:].bitcast(BF16)
                    for s_ in range(4):
                        pb.op('tensor', lambda e, s_=s_, kc=kc, pst=pst, src=src: e.transpose(
                            out=pst[:, s_ * 128:(s_ + 1) * 128], in_=src[:, s_, kc * 128:(kc + 1) * 128], identity=identb),
                            [(sk, s_), 'identb'], [PS(bank)])
                    if hl == 0:
                        vop(lambda e, kc=kc, pst=pst: e.tensor_scalar(out=n2b[:, kc, :], in0=pst[:, 0:512], scalar1=vec[:, 8 + kc:9 + kc],
                                                                     scalar2=None, op0=ALU.mult), [PS(bank), 'vec'], [('n2b', kc)])
                        if KEV == "d":
                            vop(lambda e, kc=kc, pst=pst: e.tensor_scalar(out=hiT[:, kc, :], in0=pst[:, 0:512], scalar1=1.0,
                                                                         scalar2=None, op0=ALU.mult), [PS(bank)], [('hiT', kc)])
                        else:
                            pb.op('scalar', lambda e, kc=kc, pst=pst: e.activation(out=hiT[:, kc, :], in_=pst[:, 0:512], func=AF.Copy),
                                  [PS(bank)], [('hiT', kc)])
                    else:
                        if KEV == "d":
                            vop(lambda e, kc=kc, pst=pst: e.tensor_scalar(out=loT[:, kc, :], in0=pst[:, 0:512], scalar1=1.0,
                                                                         scalar2=None, op0=ALU.mult), [PS(bank)], [('loT', kc)])
                        else:
                            vop(lambda e, kc=kc, pst=pst: e.tensor_copy(out=loT[:, kc, :], in_=pst[:, 0:512]), [PS(bank)], [('loT', kc)])
            if KB == "c":
                pb.dma('sync', out_d[t0:t0 + 512, :].rearrange("(s p) d -> p s d", p=128), xnf, 'out_st',
                       reads=[('xnf', s_) for s_ in range(4)] + [('n2b', kc) for kc in range(8)] + [('hiT', kc) for kc in range(8)] + [('loT', kc) for kc in range(8)], writes=[])
                continue
            KD = int(os.environ.get("KD", "9"))
            for s_ in range(4 if KD >= 3 else 0):
                bank = 5
                passes = [(hiT, 'hiT', Wrh, 'Wrh'), (loT, 'loT', Wrh, 'Wrh'), (hiT, 'hiT', Wrl, 'Wrl')]
                for pi_, (xa, xk, wa, wk) in enumerate(passes):
                    for kc in range(8):
                        mm(psb[bank][:, 0:20], xa[:, kc, s_ * 128:(s_ + 1) * 128], wa[:, kc, :], pi_ == 0 and kc == 0,
                           pi_ == 2 and kc == 7, [(xk, kc), wk], bank)
                if KD < 4:
                    vop(lambda e, s_=s_: e.memset(comb[:, s_, :], 0.0), [], [('comb', s_)])
                    continue
                L = lg[:, s_, :]
                gmax, ngm, gsum, m1n, e2, coef = (sm[:, i_, s_:s_ + 1] for i_ in range(6))
                ohg = sm[:, 6 + s_ // 2, (s_ % 2) * 4:(s_ % 2) * 4 + 4]
                eg = sm[:, 8 + s_ // 2, (s_ % 2) * 4:(s_ % 2) * 4 + 4]
                vop(lambda e, L=L: e.tensor_tensor(out=L, in0=psb[5][:, 0:20], in1=rbias, op=ALU.add), [PS(5), 'rbias'], ['L'])
                vop(lambda e, L=L, gmax=gmax: e.reduce_max(out=gmax, in_=L[:, 0:4], axis=AX.X), ['L'], ['gmax'])
                vop(lambda e, gmax=gmax, ngm=ngm: e.tensor_scalar(out=ngm, in0=gmax, scalar1=-1.0, scalar2=None, op0=ALU.mult), ['gmax'], ['ngm'])
                pb.op('scalar', lambda e, L=L, eg=eg, ngm=ngm: e.activation(out=eg, in_=L[:, 0:4], func=AF.Exp, bias=ngm), ['L', 'ngm'], ['eg'])
                vop(lambda e, eg=eg, gsum=gsum: e.reduce_sum(out=gsum, in_=eg, axis=AX.X), ['eg'], ['gsum'])
                vop(lambda e, L=L, ohg=ohg, gmax=gmax: e.tensor_scalar(out=ohg, in0=L[:, 0:4], scalar1=gmax, scalar2=None, op0=ALU.is_ge),
                    ['L', 'gmax'], ['ohg'])
                vop(lambda e, ohg=ohg: e.tensor_scalar(out=ohg, in0=ohg, scalar1=1.0, scalar2=1e9, op0=ALU.subtract, op1=ALU.mult), ['ohg'], ['ohg'])
                for g_ in range(4):
                    vop(lambda e, g_=g_, L=L, ohg=ohg: e.tensor_scalar(out=m16[:, g_ * 4:(g_ + 1) * 4], in0=L[:, 4 + g_ * 4:8 + g_ * 4],
                                                                     scalar1=ohg[:, g_:g_ + 1], scalar2=None, op0=ALU.add), ['L', 'ohg'], ['m16'])
                vop(lambda e: e.max(out=mx8, in_=m16), ['m16'], ['mx8'])
                vop(lambda e, m1n=m1n: e.tensor_scalar(out=m1n, in0=mx8[:, 0:1], scalar1=-1.0, scalar2=None, op0=ALU.mult), ['mx8'], ['m1n'])
                pb.op('scalar', lambda e, m1n=m1n: e.activation(out=e16, in_=m16, func=AF.Exp, bias=m1n), ['m16', 'm1n'], ['e16'])
                pb.op('scalar', lambda e, m1n=m1n, e2=e2: e.activation(out=e2, in_=mx8[:, 1:2], func=AF.Exp, bias=m1n), ['mx8', 'm1n'], ['e2'])
                vop(lambda e, e2=e2: e.tensor_scalar(out=e2, in0=e2, scalar1=1.0, scalar2=None, op0=ALU.add), ['e2'], ['e2'])
                vop(lambda e, e2=e2, gsum=gsum, coef=coef: e.tensor_tensor(out=coef, in0=e2, in1=gsum, op=ALU.mult), ['e2', 'gsum'], ['coef'])
                vop(lambda e, coef=coef: e.reciprocal(out=coef, in_=coef), ['coef'], ['coef'])
                vop(lambda e: e.tensor_scalar(out=m16, in0=m16, scalar1=mx8[:, 1:2], scalar2=None, op0=ALU.is_ge), ['m16', 'mx8'], ['m16'])
                vop(lambda e: e.tensor_tensor(out=e16, in0=e16, in1=m16, op=ALU.mult), ['e16', 'm16'], ['e16'])
                vop(lambda e, s_=s_, coef=coef: e.tensor_scalar(out=comb[:, s_, :], in0=e16, scalar1=coef, scalar2=None, op0=ALU.mult),
                    ['e16', 'coef'], [('comb', s_)])
            def stage_gu(ex):
                sl = ex % 2
                hd = hid[sl]
                pb.dma_split('sync', Wg[sl], moe_g_bf[l, ex].rearrange("(k p) f -> p k f", p=128), ('Wg', sl), 2, reads=wkeys[('moe', l, ex)], writes=[('Wg', sl)])
                pb.dma_split('sync', Wu[sl], moe_u_bf[l, ex].rearrange("(k p) f -> p k f", p=128), ('Wu', sl), 2, reads=wkeys[('moe', l, ex)], writes=[('Wu', sl)])
                pb.dma('sync', Wd[sl], moe_d_bf[l, ex].rearrange("(k p) d -> p k d", p=128), ('Wd', sl), reads=wkeys[('moe', l, ex)], writes=[('Wd', sl)])
                for fc in range(4):
                    bg, bu = (fc % 2) * 2, (fc % 2) * 2 + 1
                    for kc in range(8):
                        mm(psb[bg][:], Wg[sl][:, kc, fc * 128:(fc + 1) * 128], n2b[:, kc, :], kc == 0, kc == 7, [('Wg', sl), ('n2b', kc)], bg)
                    for kc in range(8):
                        mm(psb[bu][:], Wu[sl][:, kc, fc * 128:(fc + 1) * 128], n2b[:, kc, :], kc == 0, kc == 7, [('Wu', sl), ('n2b', kc)], bu)
                    sgt = sg[fc % 2]
                    pb.op('scalar', lambda e, sgt=sgt, bg=bg: e.activation(out=sgt, in_=psb[bg][:], func=AF.Silu), [PS(bg)], [('sg', fc % 2)])
                    vop(lambda e, sgt=sgt, bu=bu, fc=fc, hd=hd: e.tensor_tensor(out=hd[:, fc, :], in0=psb[bu][:], in1=sgt, op=ALU.mult),
                        [PS(bu), ('sg', fc % 2)], [('hid', sl, fc)])

            def stage_down(ex):
                sl = ex % 2
                hd = hid[sl]
                for s_ in range(4):
                    for hf in range(2):
                        idx = s_ * 2 + hf
                        bank = 4 + idx % 2
                        for fc in range(4):
                            mm(psb[bank][:], hd[:, fc, s_ * 128:(s_ + 1) * 128], Wd[sl][:, fc, hf * 512:(hf + 1) * 512], fc == 0, fc == 3,
                               [('hid', sl, fc), ('Wd', sl)], bank)
                        dt_ = dtmp[idx % 2]
                        vop(lambda e, dt_=dt_, bank=bank, s_=s_, ex=ex: e.tensor_scalar(out=dt_, in0=psb[bank][:], scalar1=comb[:, s_, ex:ex + 1],
                                                                                       scalar2=None, op0=ALU.mult),
                            [PS(bank), ('comb', s_)], [('dtmp', idx % 2)])
                        vop(lambda e, dt_=dt_, s_=s_, hf=hf: e.tensor_tensor(out=xt[:, s_, hf * 512:(hf + 1) * 512], in0=xt[:, s_, hf * 512:(hf + 1) * 512],
                                                                            in1=dt_, op=ALU.add), [('dtmp', idx % 2), 'xtD', ('xacc', s_, hf)], [('xacc', s_, hf)],
                            eng='gpsimd')

            if NE > 0:
                stage_gu(0)
            for ex in range(NE):
                if ex + 1 < NE:
                    stage_gu(ex + 1)
                stage_down(ex)
            allx = [('xacc', s_, hf) for s_ in range(4) for hf in range(2)]
            if not last:
                pb.dma('sync', xres[t0:t0 + 512, :].rearrange("(s p) d -> p s d", p=128), xt, 'xtD_out', reads=allx, writes=['xtD'])
            else:
                for s_ in range(4):
                    rmsnorm_rstd((xt[:, s_, :], allx), junk, ss[:, 4 + s_:5 + s_], rs[:, 4 + s_:5 + s_], D, 'F%d' % s_)
                    vop(lambda e, s_=s_: e.tensor_scalar(out=xnf[:, s_, :], in0=xt[:, s_, :], scalar1=rs[:, 4 + s_:5 + s_], scalar2=None,
                                                         op0=ALU.mult), allx + ['F%drs' % s_], [('xnf', s_)])
                    vop(lambda e, s_=s_: e.tensor_tensor(out=xnf[:, s_, :], in0=xnf[:, s_, :], in1=fng, op=ALU.mult), [('xnf', s_), 'fng'], [('xnf', s_)])
                pb.dma('sync', out_d[t0:t0 + 512, :].rearrange("(s p) d -> p s d", p=128), xnf, 'out_st',
                       reads=[('xnf', s_) for s_ in range(4)], writes=['xtD'])
        pb.barrier()
        ar.reset(base)

    for l in range(nlayers):
        if stop_after == ('conv',):
            break
        phase_A(l)
        if stop_after == ('A', l):
            break
        phase_BA(l)
        phase_BB(l)
        phase_BD(l)
        if stop_after == ('B1', l):
            break
        if os.environ.get("KSKIPBC") != "1":
            phase_BC(l)
        if stop_after == ('B2', l):
            break
        phase_C(l)
        if stop_after == ('C', l):
            break
        if l == 0 and nlayers > 1:
            conv_w_in(1)
            conv_rest(1)
        phase_D(l, l == nlayers - 1)
        if stop_after == ('D', l):
            break

    pb.barrier()
    pb.emit()


def _consts():
    bf = ml_dtypes.bfloat16
    k = np.arange(128)[:, None]
    q = np.arange(128)[None, :]
    tril = (k <= q).astype(np.float32)
    triu = (k > q).astype(np.float32)
    c = {}
    c['c_ident'] = np.eye(128, dtype=np.float32)
    c['c_tril4'] = np.tile(tril, (1, 4)).astype(bf)
    c['c_triu4'] = np.tile(triu, (1, 4)).astype(bf)
    n = np.arange(256)
    t = np.arange(S)
    vis = ((n[:, None] * 16 + 31) <= t[None, :]) & (n[:, None] < 255)
    m = vis.reshape(2, 128, NQT, 128).transpose(2, 1, 0, 3)
    c['c_cmpmask'] = np.ascontiguousarray(np.tile(m, (1, 1, 1, 4))).astype(bf)
    c_start = np.arange(256) * 16
    s_start = np.arange(64) * 64
    ov = ((c_start[:, None] <= s_start[None, :] + 63) & (c_start[:, None] + 31 >= s_start[None, :])).astype(np.float32)
    ov[255] = 0
    ovc = np.concatenate([ov, np.ones((256, 1), np.float32)], axis=1)
    ovc[255] = 0
    c['c_ovc'] = np.ascontiguousarray(ovc.reshape(2, 128, 65).transpose(1, 0, 2)).astype(bf)
    kk = np.arange(S)
    c['c_eblk'] = (kk[None, :] // 64 == np.arange(64)[:, None]).astype(bf)
    blk = np.arange(64)
    cur = t // 64
    causal = blk[None, :] * 64 <= t[:, None]
    forced = (blk[None, :] == 0) | (blk[None, :] == cur[:, None]) | (blk[None, :] == cur[:, None] - 1)
    c['c_selC'] = (causal & ~forced).astype(np.float32)
    c['c_selB'] = np.where(forced, 1e6, np.where(causal, 0.0, -1.0)).astype(np.float32)
    pc = np.zeros((128, 4, 16), np.float32)
    for gi, w in enumerate((2, 4, 8, 16)):
        pc[:, gi, :] = 1.0 / np.minimum(np.arange(16) + 1, w)
    c['c_pcorr'] = pc
    return c


def _prep(inp):
    f = np.float32
    L = DEPTH
    shared = {}
    shared['w_in'] = np.ascontiguousarray(inp['w_in'], dtype=f)
    shared['w_branch'] = np.ascontiguousarray(inp['w_branch'], dtype=f).reshape(L, 4 * MIX, D)
    shared['w_out'] = np.ascontiguousarray(inp['w_out'], dtype=f)
    shared['moe_g'] = np.ascontiguousarray(inp['moe_w_gate'], dtype=f)
    shared['moe_u'] = np.ascontiguousarray(inp['moe_w_up'], dtype=f)
    shared['moe_d'] = np.ascontiguousarray(inp['moe_w_down'], dtype=f)
    shared['gm_wsT'] = np.ascontiguousarray(np.transpose(inp['gm_ws'], (0, 1, 3, 2)), dtype=f)
    shared['lru_wa'] = np.ascontiguousarray(inp['lru_wa'], dtype=f)
    shared['lru_wx'] = np.ascontiguousarray(inp['lru_wx'], dtype=f)
    shared['cmp_w1'] = np.ascontiguousarray(inp['cmp_w1'], dtype=f)
    shared['cmp_w2'] = np.ascontiguousarray(inp['cmp_w2'], dtype=f)
    shared['cmp_peT'] = np.ascontiguousarray(np.transpose(inp['cmp_pe'], (0, 1, 3, 2)), dtype=f)
    shared['pool_w'] = np.ascontiguousarray(inp['pool_w'], dtype=f)
    shared['router_w'] = np.ascontiguousarray(np.concatenate([inp['router_w_group'], inp['router_w_expert']], axis=2), dtype=f)
    vecs = np.zeros((L, 128, NV), f)
    rows = np.zeros((L, 128, NR), f)
    for l in range(L):
        vecs[l, :, 0:8] = inp['norm1_g'][l].reshape(8, 128).T
        vecs[l, :, 8:16] = inp['norm2_g'][l].reshape(8, 128).T
        for k in range(4):
            vecs[l, :, 16 + 4 * k:20 + 4 * k] = inp['conv_w'][l, k].reshape(4, 128).T
        vecs[l, :, 32:36] = inp['conv_b'][l].reshape(4, 128).T
        vecs[l, :, 36:40] = inp['lru_ba'][l].reshape(4, 128).T
        vecs[l, :, 40:44] = inp['lru_bx'][l].reshape(4, 128).T
        vecs[l, :, 44:48] = inp['lru_lambda'][l].reshape(4, 128).T
        vecs[l, :, 48:52] = inp['pool_scale'][l].reshape(4, 128).T
        rows[l, :, 0:512] = inp['gm_norm_g'][l][None, :]
        rows[l, :, 512:1024] = inp['gm_b'][l].reshape(1, 512)
        rows[l, :, 1024:1028] = inp['router_b_group'][l][None, :]
        rows[l, :, 1028:1044] = inp['router_b_expert'][l][None, :]
        rows[l, :, 1044:2068] = inp['final_norm_g'][None, :]
    shared['vecs'] = vecs
    shared['rows'] = rows
    shared.update(_consts())
    return shared


def kernel(**inputs):
    shared = _prep(inputs)
    x = np.ascontiguousarray(inputs['x'], dtype=np.float32)
    nc = build()
    in_maps = []
    for c in range(8):
        m = dict(shared)
        m['x'] = x[c % 4]
        in_maps.append(m)
    res = run_bass_kernel_spmd(nc, in_maps, core_ids=list(range(8)))
    out = np.stack([res.results[c]['out'] for c in range(4)], axis=0)
    return out.astype(np.float32)
```

```python
import numpy as np
import ml_dtypes
from contextlib import ExitStack
import concourse.bass as bass
import concourse.mybir as mybir
from concourse.bass_utils import run_bass_kernel_spmd

F32 = mybir.dt.float32
BF16 = mybir.dt.bfloat16
AF = mybir.ActivationFunctionType
ALU = mybir.AluOpType
AX = mybir.AxisListType

S = 4096
D = 1024
MIX = 512
NIN = 7960
DEPTH = 2
EPS = 1e-6
NQT = 32
OFF = dict(u=0, v=512, gb=1024, rb=1536, q=2048, kcmp=2560, vcmp=2688, kslc=2816, vslc=2944,
           kwin=3072, vwin=3200, ng=3328, xd=3352, mg=3864)
NV = 64
NR = 2068
BIG = 30000.0
SAME_ENG_WAIT = True
ENGS = ['sync', 'scalar', 'vector', 'gpsimd', 'tensor']


class PB:
    def __init__(self, nc, es):
        self.nc, self.es = nc, es
        self.q = {e: [] for e in ENGS}
        self.cnt = {e: 0 for e in ENGS}
        self.seen = {e: {} for e in ENGS}
        self.buf = {}
        self.sems = {}
        self.dcnt = {}

    def _need(self, eng, ev, waits):
        if ev is None:
            return
        sk, val = ev
        if sk == ('e', eng) and (eng == 'tensor' or not SAME_ENG_WAIT):
            return
        if self.seen[eng].get(sk, 0) >= val:
            return
        self.seen[eng][sk] = val
        waits[sk] = max(waits.get(sk, 0), val)

    def _deps(self, eng, reads, writes):
        waits = {}
        for k in reads:
            b = self.buf.get(k)
            if b:
                self._need(eng, b[0], waits)
        for k in writes:
            b = self.buf.get(k)
            if b:
                self._need(eng, b[0], waits)
                for sk, val in b[1].items():
                    self._need(eng, (sk, val), waits)
        return list(waits.items())

    def _commit(self, ev, reads, writes):
        for k in reads:
            b = self.buf.setdefault(k, [None, {}])
            b[1][ev[0]] = max(b[1].get(ev[0], 0), ev[1])
        for k in writes:
            self.buf[k] = [ev, {}]

    def op(self, eng, fn, reads=(), writes=()):
        waits = self._deps(eng, reads, writes)
        self.cnt[eng] += 1
        ev = (('e', eng), self.cnt[eng])
        self._commit(ev, reads, writes)
        self.q[eng].append((waits, fn, (('e', eng), 1)))

    def dma(self, eng, out, in_, dkey, reads=(), writes=()):
        waits = self._deps(eng, reads, writes)
        self.dcnt[dkey] = self.dcnt.get(dkey, 0) + 16
        ev = (('d', dkey), self.dcnt[dkey])
        self._commit(ev, reads, writes)
        self.q[eng].append((waits, (lambda e: e.dma_start(out=out, in_=in_)), (('d', dkey), 16)))

    def dma_batch(self, eng, pairs, dkey, reads=(), writes=()):
        waits = self._deps(eng, reads, writes)
        self.dcnt[dkey] = self.dcnt.get(dkey, 0) + 16 * len(pairs)
        ev = (('d', dkey), self.dcnt[dkey])
        self._commit(ev, reads, writes)
        for i, (out, in_) in enumerate(pairs):
            self.q[eng].append((waits if i == 0 else [], (lambda e, out=out, in_=in_: e.dma_start(out=out, in_=in_)),
                                (('d', dkey), 16)))

    def dma_split(self, eng, out, in_, dkey, n, reads=(), writes=()):
        A = out.shape[1]
        assert A % n == 0 and in_.shape[1] == A, (out.shape, in_.shape, n)
        c = A // n
        pairs = [(out[:, i * c:(i + 1) * c], in_[:, i * c:(i + 1) * c]) for i in range(n)]
        self.dma_batch(eng, pairs, dkey, reads=reads, writes=writes)

    def group_done(self, dkey, key):
        self.buf[key] = [(('d', dkey), self.dcnt[dkey]), {}]

    def wait_event(self, eng, ev):
        waits = {}
        self._need(eng, ev, waits)
        if waits:
            self.q[eng].append((list(waits.items()), None, None))

    def barrier(self):
        evs = [(('e', e), self.cnt[e]) for e in ENGS if self.cnt[e] > 0]
        evs += [(('d', k), v) for k, v in self.dcnt.items()]
        for eng in ENGS:
            waits = {}
            for ev in evs:
                self._need(eng, ev, waits)
            if waits:
                self.q[eng].append((list(waits.items()), None, None))

    def emit(self):
        allsk = set()
        for e in ENGS:
            for waits, fn, inc in self.q[e]:
                for sk, _ in waits:
                    allsk.add(sk)
                if inc is not None:
                    allsk.add(inc[0])
        for i, sk in enumerate(sorted(allsk, key=repr)):
            self.sems[sk] = self.es.enter_context(self.nc.semaphore("s%d" % i))
        block = self.es.enter_context(self.nc.Block())
        for e in ENGS:
            def body(engine, e=e):
                for waits, fn, inc in self.q[e]:
                    for sk, val in waits:
                        engine.wait_ge(self.sems[sk], val)
                    if fn is not None:
                        ins = fn(engine)
                        ins.then_inc(self.sems[inc[0]], inc[1])
            getattr(block, e)(body)


class Arena:
    def __init__(self, ap_f32, nwords):
        self.ap, self.n, self.off = ap_f32, nwords, 0

    def mark(self):
        return self.off

    def reset(self, off):
        self.off = off

    def alloc(self, free_shape, dtype):
        n = int(np.prod(free_shape))
        nw = n if dtype == F32 else (n + 1) // 2
        nw = (nw + 7) // 8 * 8
        w0 = self.off
        self.off += nw
        assert self.off <= self.n, ("SBUF arena overflow", self.off, self.n)
        v = self.ap[:, w0:w0 + nw]
        if dtype != F32:
            v = v.bitcast(dtype)
        v = v[:, 0:n]
        if len(free_shape) == 2:
            v = v.rearrange("p (a b) -> p a b", a=free_shape[0], b=free_shape[1])
        elif len(free_shape) == 3:
            v = v.rearrange("p (a b c) -> p a b c", a=free_shape[0], b=free_shape[1], c=free_shape[2])
        return v


def flat2d(ap, cols):
    nd = len(ap.shape)
    names = " ".join("a%d" % i for i in range(nd))
    f = ap.rearrange("%s -> (%s)" % (names, names))
    return f.rearrange("(r c) -> r c", c=cols)


def build(stop_after=None, debug=False, nlayers=DEPTH):
    nc = bass.Bass("TRN2", target_bir_lowering=False)
    with ExitStack() as es:
        _build(nc, es, stop_after, debug, nlayers)
    return nc


def _build(nc, es, stop_after, debug, nlayers):
    pb = PB(nc, es)
    dbg_kind = "ExternalOutput" if debug else "Internal"

    def din(name, shape, dt=F32):
        return nc.dram_tensor(name, list(shape), dt, kind="ExternalInput").ap()

    def dscr(name, shape, dt, kind=None):
        return nc.dram_tensor(name, list(shape), dt, kind=kind or dbg_kind).ap()

    x_in = din("x", [S, D])
    w_in = din("w_in", [DEPTH, D, NIN])
    w_branch = din("w_branch", [DEPTH, 4 * MIX, D])
    w_out = din("w_out", [DEPTH, D, D])
    moe_g = din("moe_g", [DEPTH, 16, D, 512])
    moe_u = din("moe_u", [DEPTH, 16, D, 512])
    moe_d = din("moe_d", [DEPTH, 16, 512, D])
    gm_wsT = din("gm_wsT", [DEPTH, 4, 128, 128])
    lru_wa = din("lru_wa", [DEPTH, 8, 64, 64])
    lru_wx = din("lru_wx", [DEPTH, 8, 64, 64])
    cmp_w1 = din("cmp_w1", [DEPTH, 2, 2, 2048, 64])
    cmp_w2 = din("cmp_w2", [DEPTH, 2, 2, 64, 64])
    cmp_peT = din("cmp_peT", [DEPTH, 2, 64, 32])
    pool_w = din("pool_w", [DEPTH, 4, 128, 128])
    router_w = din("router_w", [DEPTH, D, 20])
    vecs = din("vecs", [DEPTH, 128, NV])
    rows = din("rows", [DEPTH, 128, NR])
    c_ident = din("c_ident", [128, 128])
    c_tril4 = din("c_tril4", [128, 512], BF16)
    c_triu4 = din("c_triu4", [128, 512], BF16)
    c_cmpmask = din("c_cmpmask", [NQT, 128, 2, 512], BF16)
    c_ovc = din("c_ovc", [128, 2, 65], BF16)
    c_eblk = din("c_eblk", [64, S], BF16)
    c_selC = din("c_selC", [S, 64])
    c_selB = din("c_selB", [S, 64])
    c_pcorr = din("c_pcorr", [128, 4, 16])
    out_d = nc.dram_tensor("out", [S, D], F32, kind="ExternalOutput").ap()

    w_in_bf = dscr("w_in_bf", [DEPTH, D, NIN], BF16, "Internal")
    w_branch_bf = dscr("w_branch_bf", [DEPTH, 4 * MIX, D], BF16, "Internal")
    w_out_bf = dscr("w_out_bf", [DEPTH, D, D], BF16, "Internal")
    moe_g_bf = dscr("moe_g_bf", [DEPTH, 16, D, 512], BF16, "Internal")
    moe_u_bf = dscr("moe_u_bf", [DEPTH, 16, D, 512], BF16, "Internal")
    moe_d_bf = dscr("moe_d_bf", [DEPTH, 16, 512, D], BF16, "Internal")
    cmp_w1_bf = dscr("cmp_w1_bf", [DEPTH, 2, 2, 2048, 64], BF16, "Internal")
    uT = dscr("uT", [MIX, S], BF16)
    vn = dscr("vn", [S, MIX], BF16)
    gbT = dscr("gbT", [MIX, S], BF16)
    rbT = dscr("rbT", [MIX, S], BF16)
    qT = dscr("qT", [MIX, S], BF16)
    kvT = dscr("kvT", [4, 128, S], BF16)
    vtok = dscr("vtok", [S, 256], BF16)
    gC = dscr("gC", [S, 24], F32)
    xdT = dscr("xdT", [MIX, S], BF16)
    mgT = dscr("mgT", [4 * D, S], BF16)
    yT = dscr("yT", [4 * MIX, S], BF16)
    xres = dscr("xres", [S, D], F32)

    ARW = 48 * 1024
    arena_t = es.enter_context(nc.sbuf_tensor("arena", [128, ARW], F32))
    ar = Arena(arena_t, ARW)
    psb = [es.enter_context(nc.psum_tensor("ps%d" % i, [128, 512], F32)) for i in range(8)]

    def PS(i):
        return ('ps', i)

    identb = ar.alloc([128], BF16)
    identf = ar.alloc([128], F32)
    tril4 = ar.alloc([512], BF16)
    triu4 = ar.alloc([512], BF16)
    vec = ar.alloc([NV], F32)
    pb.dma('gpsimd', identb, c_ident[:, :], 'identb', writes=['identb'])
    pb.dma('sync', identf, c_ident[:, :], 'identf', writes=['identf'])
    pb.dma('sync', tril4, c_tril4[:, :], 'tril4', writes=['tril4'])
    pb.dma('sync', triu4, c_triu4[:, :], 'triu4', writes=['triu4'])
    PERSIST = ar.mark()

    import os
    STAGE = int(os.environ.get("KSTAGE", "9"))

    cv_n = [0]

    def cv_dma(dst, src, key):
        slot = cv_n[0] % 4
        cv_n[0] += 1
        dk = ('cv', slot)
        if dk in pb.dcnt:
            pb.wait_event('gpsimd', (('d', dk), pb.dcnt[dk]))
        pb.dma('gpsimd', dst, src, dk, writes=[key])

    wkeys = {}

    def conv_w_in(l):
        ks = []
        for kc in range(8):
            ks.append(('w_in_bf', l, kc))
            cv_dma(w_in_bf[l, kc * 128:(kc + 1) * 128, :], w_in[l, kc * 128:(kc + 1) * 128, :], ks[-1])
        wkeys[('w_in', l)] = ks

    def conv_flat(dst, src, key, cols=4096, rows_per=128):
        d2, s2 = flat2d(dst, cols), flat2d(src, cols)
        nr = d2.shape[0]
        ks = []
        for r0 in range(0, nr, rows_per):
            r1 = min(nr, r0 + rows_per)
            ks.append((key, r0))
            cv_dma(d2[r0:r1, :], s2[r0:r1, :], ks[-1])
        return ks

    def conv_rest(l):
        wkeys[('w1', l)] = conv_flat(cmp_w1_bf[l], cmp_w1[l], ('cmp_w1_bf', l))
        wkeys[('wb', l)] = conv_flat(w_branch_bf[l], w_branch[l], ('w_branch_bf', l))
        wkeys[('wo', l)] = conv_flat(w_out_bf[l], w_out[l], ('w_out_bf', l))
        if STAGE < 4:
            return
        for e in range(int(os.environ.get("KNE", "16"))):
            wkeys[('moe', l, e)] = (conv_flat(moe_g_bf[l, e], moe_g[l, e], ('moe_g_bf', l, e))
                                    + conv_flat(moe_u_bf[l, e], moe_u[l, e], ('moe_u_bf', l, e))
                                    + conv_flat(moe_d_bf[l, e], moe_d[l, e], ('moe_d_bf', l, e)))

    if STAGE >= 2:
        conv_w_in(0)
    if STAGE >= 3:
        conv_rest(0)

    def rmsnorm_rstd(xt_s, junk, ss, rs, nfeat, tag):
        pb.op('scalar', lambda e: e.activation(out=junk, in_=xt_s[0], func=AF.Square),
              reads=xt_s[1], writes=[tag + 'junk'])
        pb.op('vector', lambda e: e.reduce_sum(out=ss, in_=junk, axis=AX.X), reads=[tag + 'junk'], writes=[tag + 'ss'])
        pb.op('vector', lambda e: e.tensor_scalar(out=rs, in0=ss, scalar1=1.0 / nfeat, scalar2=EPS,
                                                  op0=ALU.mult, op1=ALU.add), reads=[tag + 'ss'], writes=[tag + 'rs'])
        pb.op('scalar', lambda e: e.activation(out=rs, in_=rs, func=AF.Sqrt), reads=[tag + 'rs'], writes=[tag + 'rs'])
        pb.op('vector', lambda e: e.reciprocal(out=rs, in_=rs), reads=[tag + 'rs'], writes=[tag + 'rs'])

    def phase_A(l):
        base = ar.mark()
        Wsb = ar.alloc([8, NIN], BF16)
        xt = ar.alloc([4, D], F32)
        xn = ar.alloc([4, D], BF16)
        nT = ar.alloc([8, 512], BF16)
        junk = ar.alloc([D], F32)
        stg = [ar.alloc([4, 512], BF16) for _ in range(2)]
        vg = ar.alloc([512], F32)
        vstg = ar.alloc([4, 512], BF16)
        kvstg = ar.alloc([4, 256], BF16)
        gstg = ar.alloc([4, 24], F32)
        rowg = ar.alloc([512], F32)
        ss = ar.alloc([8], F32)
        rs = ar.alloc([8], F32)
        pb.dma('sync', vec, vecs[l], 'vec', writes=['vec'])
        pb.dma('sync', rowg, rows[l, :, 0:512], 'rowg', writes=['rowg'])
        for kc in range(8):
            pb.dma('sync', Wsb[:, kc, :], w_in_bf[l, kc * 128:(kc + 1) * 128, :], ('Wsb', kc),
                   reads=[('w_in_bf', l, kc)], writes=[('Wsb', kc)])
        Wk = [('Wsb', kc) for kc in range(8)]
        xsrc = x_in if l == 0 else xres
        psi = [0]

        def nextps():
            psi[0] = (psi[0] + 1) % 6
            return psi[0]

        groups = []
        for c4 in range(1):
            groups.append((OFF['u'], 4, AF.Gelu_apprx_tanh, uT, 0))
            groups.append((OFF['gb'], 4, AF.Gelu_apprx_tanh, gbT, 0))
            groups.append((OFF['rb'], 4, None, rbT, 0))
            groups.append((OFF['q'], 4, None, qT, 0))
            groups.append((OFF['xd'], 4, None, xdT, 0))
        for b in range(8):
            groups.append((OFF['mg'] + b * 512, 4, AF.Sigmoid, mgT, b * 512))
        kvT2 = kvT.rearrange("k p t -> (k p) t")
        kv_cols = [OFF['kcmp'], OFF['vcmp'], OFF['kslc'], OFF['kwin']]

        KA = int(os.environ.get("KA", "9"))
        for jt in range(int(os.environ.get("KJT", "8"))):
            t0 = jt * 512
            pb.dma('sync', xt, xsrc[t0:t0 + 512, :].rearrange("(s p) d -> p s d", p=128), 'xtA',
                   reads=[('xres', jt)] if l > 0 else [], writes=['xtA'])
            if KA < 2:
                continue
            for s in range(4):
                rmsnorm_rstd((xt[:, s, :], ['xtA']), junk, ss[:, s:s + 1], rs[:, s:s + 1], D, 'A%d' % s)
                pb.op('vector', lambda e, s=s: e.tensor_scalar(out=xn[:, s, :], in0=xt[:, s, :], scalar1=rs[:, s:s + 1],
                                                              scalar2=None, op0=ALU.mult),
                      reads=['xtA', 'A%drs' % s], writes=[('xn', s)])
            if KA < 3:
                continue
            for kc in range(8):
                bank = 6 + (kc % 2)
                pst = psb[bank][:].bitcast(BF16)
                for s in range(4):
                    pb.op('tensor', lambda e, s=s, kc=kc, pst=pst: e.transpose(
                        out=pst[:, s * 128:(s + 1) * 128], in_=xn[:, s, kc * 128:(kc + 1) * 128], identity=identb),
                        reads=[('xn', s), 'identb'], writes=[PS(bank)])
                pb.op('vector', lambda e, kc=kc, pst=pst: e.tensor_scalar(
                    out=nT[:, kc, :], in0=pst[:, 0:512], scalar1=vec[:, kc:kc + 1], scalar2=None, op0=ALU.mult),
                    reads=[PS(bank), 'vec'], writes=[('nT', kc)])
            nTk = [('nT', kc) for kc in range(8)]
            gi = 0

            def fm_group(col0, nch, func, dest, row0, cols_list=None):
                nonlocal gi
                slot = gi % 2
                gi += 1
                st = stg[slot]
                for c in range(nch):
                    cc0 = cols_list[c] if cols_list else col0 + c * 128
                    bank = nextps()
                    for kc in range(8):
                        pb.op('tensor', lambda e, kc=kc, cc0=cc0, bank=bank: e.matmul(
                            psb[bank][:], lhsT=Wsb[:, kc, cc0:cc0 + 128], rhs=nT[:, kc, :],
                            start=(kc == 0), stop=(kc == 7)),
                            reads=[Wk[kc], nTk[kc]], writes=[PS(bank)])
                    if func is None:
                        pb.op('vector', lambda e, c=c, bank=bank, st=st: e.tensor_copy(out=st[:, c, :], in_=psb[bank][:]),
                              reads=[PS(bank)], writes=[('stg', slot, c)])
                    else:
                        pb.op('scalar', lambda e, c=c, bank=bank, st=st, func=func: e.activation(
                            out=st[:, c, :], in_=psb[bank][:], func=func),
                            reads=[PS(bank)], writes=[('stg', slot, c)])
                pb.dma('sync', dest[row0:row0 + nch * 128, t0:t0 + 512].rearrange("(c p) t -> p c t", p=128),
                       st[:, 0:nch, :], ('stg', slot), reads=[('stg', slot, c) for c in range(nch)],
                       writes=[(dest.name, row0, jt)])

            if KA < 4:
                continue
            for (col0, nch, func, dest, row0) in (groups if KA >= 5 else groups[:1]):
                fm_group(col0, nch, func, dest, row0)
            if KA < 6:
                continue
            fm_group(0, 4, None, kvT2, 0, cols_list=kv_cols)
            if KA < 7:
                continue
            for s in range(4):
                bank = nextps()
                for kc in range(8):
                    pb.op('tensor', lambda e, kc=kc, s=s, bank=bank: e.matmul(
                        psb[bank][:], lhsT=nT[:, kc, s * 128:(s + 1) * 128], rhs=Wsb[:, kc, OFF['v']:OFF['v'] + 512],
                        start=(kc == 0), stop=(kc == 7)), reads=[Wk[kc], nTk[kc]], writes=[PS(bank)])
                pb.op('scalar', lambda e, bank=bank: e.activation(out=vg, in_=psb[bank][:], func=AF.Gelu_apprx_tanh),
                      reads=[PS(bank)], writes=['vg'])
                rmsnorm_rstd((vg, ['vg']), junk[:, 0:512], ss[:, 4:5], rs[:, 4:5], MIX, 'Av')
                pb.op('vector', lambda e: e.tensor_scalar(out=vg, in0=vg, scalar1=rs[:, 4:5], scalar2=None, op0=ALU.mult),
                      reads=['vg', 'Avrs'], writes=['vg'])
                pb.op('vector', lambda e, s=s: e.tensor_tensor(out=vstg[:, s, :], in0=vg, in1=rowg, op=ALU.mult),
                      reads=['vg', 'rowg'], writes=[('vstg', s)])
                if KA < 8:
                    continue
                bank = nextps()
                for (c0, n, o0) in ((OFF['vslc'], 128, 0), (OFF['vwin'], 128, 128), (OFF['ng'], 24, 256)):
                    for kc in range(8):
                        pb.op('tensor', lambda e, kc=kc, s=s, bank=bank, c0=c0, n=n, o0=o0: e.matmul(
                            psb[bank][:, o0:o0 + n], lhsT=nT[:, kc, s * 128:(s + 1) * 128], rhs=Wsb[:, kc, c0:c0 + n],
                            start=(kc == 0), stop=(kc == 7)), reads=[Wk[kc], nTk[kc]], writes=[PS(bank)])
                pb.op('vector', lambda e, s=s, bank=bank: e.tensor_copy(out=kvstg[:, s, :], in_=psb[bank][:, 0:256]),
                      reads=[PS(bank)], writes=[('kvstg', s)])
                pb.op('scalar', lambda e, s=s, bank=bank: e.activation(out=gstg[:, s, :], in_=psb[bank][:, 256:280],
                                                                      func=AF.Sigmoid),
                      reads=[PS(bank)], writes=[('gstg', s)])
            if KA < 9:
                continue
            pb.dma('sync', vn[t0:t0 + 512, :].rearrange("(s p) c -> p s c", p=128), vstg, 'vstg',
                   reads=[('vstg', s) for s in range(4)], writes=[('vn', jt)])
            pb.dma('sync', vtok[t0:t0 + 512, :].rearrange("(s p) c -> p s c", p=128), kvstg, 'kvstg',
                   reads=[('kvstg', s) for s in range(4)], writes=[('vtok', jt)])
            pb.dma('sync', gC[t0:t0 + 512, :].rearrange("(s p) c -> p s c", p=128), gstg, 'gstg',
                   reads=[('gstg', s) for s in range(4)], writes=[('gC', jt)])
        pb.barrier()
        ar.reset(base)


    def mm(out, lhsT, rhs, start, stop, reads, bank):
        pb.op('tensor', lambda e: e.matmul(out, lhsT=lhsT, rhs=rhs, start=start, stop=stop),
              reads=reads, writes=[PS(bank)])

    def vop(fn, reads, writes, eng='vector'):
        pb.op(eng, fn, reads=reads, writes=writes)

    def phase_BA(l):
        base = ar.mark()
        vnsb = ar.alloc([32, 512], BF16)
        usb = ar.alloc([4, S], BF16)
        ya = ar.alloc([4, S], BF16)
        wTf = ar.alloc([4, 128], F32)
        wTm = ar.alloc([4, 128], BF16)
        rowb = ar.alloc([512], F32)
        bsb = ar.alloc([512], BF16)
        ones1 = ar.alloc([128], BF16)
        pb.dma_split('sync', vnsb, vn.rearrange("(ch s) c -> s ch c", s=128), 'vnsb', 8, writes=['vnsb'])
        pb.dma('sync', usb, uT.rearrange("(g c) t -> c g t", c=128), 'usb', writes=['usb'])
        pb.dma('sync', wTf, gm_wsT[l].rearrange("g s t -> s g t"), 'wTf', writes=['wTf'])
        pb.dma('sync', rowb[0:1, :], rows[l, 0:1, 512:1024], 'rowb', writes=['rowb'])
        for g in range(4):
            vop(lambda e, g=g: e.tensor_tensor(out=wTm[:, g, :], in0=wTf[:, g, :], in1=tril4[:, 0:128], op=ALU.mult),
                ['wTf', 'tril4'], [('wTm', g)])
        vop(lambda e: e.tensor_copy(out=bsb[0:1, :], in_=rowb[0:1, :]), ['rowb'], ['bsb'])
        vop(lambda e: e.memset(ones1[0:1, :], 1.0), [], ['ones1'])
        for g in range(4):
            for ch4 in range(8):
                bank = (g * 8 + ch4) % 4
                for cq in range(4):
                    ch = ch4 * 4 + cq
                    mm(psb[bank][:, cq * 128:(cq + 1) * 128], vnsb[:, ch, g * 128:(g + 1) * 128], wTm[:, g, :],
                       True, False, ['vnsb', ('wTm', g)], bank)
                    mm(psb[bank][:, cq * 128:(cq + 1) * 128], ones1[0:1, :], bsb[0:1, g * 128:(g + 1) * 128],
                       False, True, ['ones1', 'bsb'], bank)
                vop(lambda e, g=g, ch4=ch4, bank=bank: e.tensor_tensor(
                    out=ya[:, g, ch4 * 512:(ch4 + 1) * 512], in0=psb[bank][:], in1=usb[:, g, ch4 * 512:(ch4 + 1) * 512],
                    op=ALU.mult), [PS(bank), 'usb'], [('ya', g, ch4)])
        pb.dma('sync', yT[0:512, :].rearrange("(g c) t -> c g t", c=128), ya, 'ya_out',
               reads=[('ya', g, c) for g in range(4) for c in range(8)], writes=[])
        pb.barrier()
        ar.reset(base)

    def phase_BB(l):
        base = ar.mark()
        xpad = ar.alloc([S + 8], BF16)
        xc = ar.alloc([S], F32)
        xcb = ar.alloc([S], BF16)
        rr = ar.alloc([S], F32)
        ii = ar.alloc([S], F32)
        tt = ar.alloc([S], F32)
        gb = ar.alloc([S], BF16)
        yb = ar.alloc([S], BF16)
        BDf = ar.alloc([2, 128], F32)
        BDb = ar.alloc([2, 128], BF16)
        sm = ar.alloc([8, 4], F32)
        vop(lambda e: e.memset(xpad[:, 0:8], 0.0), [], ['xpad0'])
        lam = vec[:, 44:48]
        z, az, ez, mz, negc = sm[:, 0, :], sm[:, 1, :], sm[:, 2, :], sm[:, 3, :], sm[:, 4, :]
        vop(lambda e: e.tensor_scalar(out=z, in0=lam, scalar1=-1.0, scalar2=None, op0=ALU.mult), ['vec'], ['z'])
        vop(lambda e: e.tensor_tensor(out=az, in0=z, in1=lam, op=ALU.max), ['z', 'vec'], ['az'])
        pb.op('scalar', lambda e: e.activation(out=ez, in_=az, func=AF.Exp, scale=-1.0), ['az'], ['ez'])
        vop(lambda e: e.tensor_scalar(out=ez, in0=ez, scalar1=1.0, scalar2=None, op0=ALU.add), ['ez'], ['ez'])
        pb.op('scalar', lambda e: e.activation(out=ez, in_=ez, func=AF.Ln), ['ez'], ['ez'])
        vop(lambda e: e.tensor_scalar(out=mz, in0=z, scalar1=0.0, scalar2=None, op0=ALU.max), ['z'], ['mz'])
        vop(lambda e: e.tensor_tensor(out=mz, in0=mz, in1=ez, op=ALU.add), ['mz', 'ez'], ['mz'])
        vop(lambda e: e.tensor_scalar(out=negc, in0=mz, scalar1=-8.0, scalar2=None, op0=ALU.mult), ['mz'], ['negc'])
        for cc in range(4):
            r0 = cc * 128
            pb.dma('sync', xpad[:, 8:], rbT[r0:r0 + 128, :], 'xpad', writes=['xpad'])
            pb.dma('sync', gb, gbT[r0:r0 + 128, :], 'gbsb', writes=['gbsb'])
            vop(lambda e: e.memset(BDf, 0.0), [], ['BDf'])
            pairs = []
            for h in range(2):
                p0 = h * 64
                pairs.append((BDf[p0:p0 + 64, 0, p0:p0 + 64], lru_wa[l, 2 * cc + h]))
                pairs.append((BDf[p0:p0 + 64, 1, p0:p0 + 64], lru_wx[l, 2 * cc + h]))
            pb.dma_batch('sync', pairs, 'BDf', reads=['BDf'], writes=['BDfd'])
            vop(lambda e: e.tensor_copy(out=BDb, in_=BDf), ['BDf', 'BDfd'], ['BDb'])
            vop(lambda e, cc=cc: e.tensor_scalar(out=xc, in0=xpad[:, 5:5 + S], scalar1=vec[:, 16 + cc:17 + cc],
                                                 scalar2=vec[:, 32 + cc:33 + cc], op0=ALU.mult, op1=ALU.add),
                ['xpad', 'xpad0', 'vec'], ['xc'])
            for k in range(1, 4):
                vop(lambda e, cc=cc, k=k: e.tensor_scalar(out=tt, in0=xpad[:, 5 + k:5 + k + S],
                                                          scalar1=vec[:, 16 + 4 * k + cc:17 + 4 * k + cc], scalar2=None, op0=ALU.mult),
                    ['xpad', 'xpad0', 'vec'], ['tt'], eng='gpsimd')
                vop(lambda e: e.tensor_tensor(out=xc, in0=xc, in1=tt, op=ALU.add), ['xc', 'tt'], ['xc'])
            pb.op('scalar', lambda e: e.activation(out=xcb, in_=xc, func=AF.Copy), ['xc'], ['xcb'])
            for tq in range(8):
                ts_ = slice(tq * 512, (tq + 1) * 512)
                for k, (dst, bcol) in enumerate(((rr, 36 + cc), (ii, 40 + cc))):
                    bank = (tq * 2 + k) % 6
                    mm(psb[bank][:], BDb[:, k, :], xcb[:, ts_], True, True, ['BDb', 'xcb'], bank)
                    pb.op('scalar', lambda e, dst=dst, bcol=bcol, bank=bank, ts_=ts_: e.activation(
                        out=dst[:, ts_], in_=psb[bank][:], func=AF.Sigmoid, bias=vec[:, bcol:bcol + 1]),
                        [PS(bank), 'vec'], [('ri', k, tq)])
            allri = [('ri', k, tq) for k in range(2) for tq in range(8)]
            pb.op('scalar', lambda e, cc=cc: e.activation(out=rr, in_=rr, func=AF.Exp, scale=negc[:, cc:cc + 1]),
                  allri + ['negc'], ['rr'])
            vop(lambda e: e.tensor_tensor(out=tt, in0=rr, in1=rr, op=ALU.mult), ['rr'], ['tt'])
            vop(lambda e: e.tensor_scalar(out=tt, in0=tt, scalar1=-1.0, scalar2=1.0, op0=ALU.mult, op1=ALU.add), ['tt'], ['tt'])
            pb.op('scalar', lambda e: e.activation(out=tt, in_=tt, func=AF.Sqrt), ['tt'], ['tt'])
            vop(lambda e: e.tensor_tensor(out=ii, in0=ii, in1=xc, op=ALU.mult), allri + ['xc'], ['ii'])
            vop(lambda e: e.tensor_tensor(out=ii, in0=ii, in1=tt, op=ALU.mult), ['ii', 'tt'], ['ii'])
            vop(lambda e: e.tensor_tensor_scan(out=tt, data0=rr, data1=ii, initial=0.0, op0=ALU.mult, op1=ALU.add),
                ['rr', 'ii', 'tt'], ['tt'])
            vop(lambda e: e.tensor_tensor(out=yb, in0=tt, in1=gb, op=ALU.mult), ['tt', 'gbsb'], ['yb'])
            pb.dma('sync', yT[512 + r0:512 + r0 + 128, :], yb, 'yb_out', reads=['yb'], writes=[])
        pb.barrier()
        ar.reset(base)

    def phase_BD(l):
        base = ar.mark()
        xp = ar.alloc([S + 16], BF16)
        A = ar.alloc([S + 16], F32)
        B = ar.alloc([S + 16], F32)
        plb = ar.alloc([S], BF16)
        yd = ar.alloc([S], BF16)
        pw = ar.alloc([4, 128], BF16)
        pcor = ar.alloc([4, 16], F32)
        pb.dma('gpsimd', pw, pool_w[l].rearrange("g i j -> i g j"), 'pw', writes=['pw'])
        pb.dma('sync', pcor, c_pcorr[:, :, :], 'pcor', writes=['pcor'])
        vop(lambda e: e.memset(A[:, 0:16], 0.0), [], ['A0'])
        vop(lambda e: e.memset(B[:, 0:16], 0.0), [], ['B0'])
        for gi in range(4):
            r0 = gi * 128
            w = 2 ** (gi + 1)
            pb.dma('sync', xp[:, 16:], xdT[r0:r0 + 128, :], 'xp', writes=['xp'])
            vop(lambda e: e.tensor_copy(out=A[:, 16:], in_=xp[:, 16:]), ['xp'], ['A'])
            cur, nxt, ck, nk = A, B, 'A', 'B'
            for k in range(gi + 1):
                sh = 2 ** k
                vop(lambda e, cur=cur, nxt=nxt, sh=sh: e.tensor_tensor(
                    out=nxt[:, 16:], in0=cur[:, 16:], in1=cur[:, 16 - sh:16 - sh + S], op=ALU.add),
                    [ck, 'A0', 'B0'], [nk])
                cur, nxt, ck, nk = nxt, cur, nk, ck
            vop(lambda e, cur=cur, nxt=nxt, w=w: e.tensor_scalar(out=nxt[:, 16:], in0=cur[:, 16:], scalar1=1.0 / w,
                                                               scalar2=None, op0=ALU.mult), [ck], [nk])
            vop(lambda e, cur=cur, nxt=nxt, gi=gi: e.tensor_tensor(out=nxt[:, 16:32], in0=cur[:, 16:32], in1=pcor[:, gi, :],
                                                                 op=ALU.mult), [ck, nk, 'pcor'], [nk])
            vop(lambda e, nxt=nxt: e.tensor_tensor(out=plb, in0=nxt[:, 16:], in1=xp[:, 16:], op=ALU.subtract),
                [nk, 'xp'], ['plb'])
            vop(lambda e, nxt=nxt: e.memset(nxt[:, 0:16], 0.0), [nk], [nk, 'A0', 'B0'])
            for tq in range(8):
                ts_ = slice(tq * 512, (tq + 1) * 512)
                bank = tq % 6
                mm(psb[bank][:], pw[:, gi, :], plb[:, ts_], True, True, ['pw', 'plb'], bank)
                vop(lambda e, bank=bank, ts_=ts_, gi=gi: e.tensor_scalar(out=yd[:, ts_], in0=psb[bank][:],
                                                                        scalar1=vec[:, 48 + gi:49 + gi], scalar2=None, op0=ALU.mult),
                    [PS(bank), 'vec'], [('yd', tq)])
            pb.dma('sync', yT[1536 + r0:1536 + r0 + 128, :], yd, 'yd_out', reads=[('yd', tq) for tq in range(8)], writes=[])
        pb.barrier()
        ar.reset(base)


    def phase_BC(l):
        base = ar.mark()
        NQ = int(os.environ.get("KNQ", str(NQT)))
        pe_sb = ar.alloc([2, 32], BF16)
        gates = ar.alloc([NQT, 24], F32)
        XT = ar.alloc([S], BF16)
        w1sb = ar.alloc([32, 64], BF16)
        w2sb = ar.alloc([2, 64], BF16)
        hidT = ar.alloc([256], BF16)
        KcT = ar.alloc([256], BF16)
        Vca_full = ar.alloc([2, 130], BF16)
        Vca = Vca_full[:, :, 0:129]
        biasb = ar.alloc([8], F32)
        Qa = ar.alloc([4, S], BF16)
        Ka = ar.alloc([S], BF16)
        Kw = ar.alloc([S], BF16)
        Vs_full = ar.alloc([NQT, 66], BF16)
        Vw_full = ar.alloc([NQT, 66], BF16)
        Vs = Vs_full[:, :, 0:65]
        Vw = Vw_full[:, :, 0:65]
        ycT = ar.alloc([2, S], BF16)
        cmk = [ar.alloc([2, 512], BF16) for _ in range(2)]
        sCB = [ar.alloc([2, 64], F32) for _ in range(2)]
        pbuf = [ar.alloc([512], BF16) for _ in range(3)]
        sm = ar.alloc([16, 4], F32)
        imp = ar.alloc([64], F32)
        tmp64 = ar.alloc([64], F32)
        m8 = ar.alloc([8], F32)
        Mpad = ar.alloc([128], BF16)
        oacc = ar.alloc([256], F32)
        otmp = ar.alloc([256], F32)
        ycb = ar.alloc([256], BF16)
        pb.dma('gpsimd', pe_sb[0:64], cmp_peT[l].rearrange("c d l -> d c l"), 'pe_sb', writes=['pe_sb'])
        pb.dma_split('sync', gates, gC.rearrange("(qt q) c -> q qt c", q=128), 'gates', 8, writes=['gates'])
        vop(lambda e: e.memset(Mpad, 0.0), [], ['Mpad'])
        vop(lambda e: e.memset(hidT, 0.0), [], ['hidT'])
        XT16 = XT.rearrange("p (n s) -> p n s", s=16)
        for g in range(2):
            pb.dma('gpsimd', w2sb[0:64], cmp_w2[l, :, g].rearrange("c e d -> e c d"), 'w2sb', writes=['w2sb'])
            pb.dma('sync', Vca[:, :, 64:129], c_ovc[:, :, :], 'Vca_c', writes=['Vca_c', ('Vca', 0), ('Vca', 1)])
            for c in range(2):
                pb.dma('sync', XT[0:64, :], kvT[c, g * 64:(g + 1) * 64, :], 'XT', writes=['XT'])
                pb.dma_split('sync', w1sb[0:64], cmp_w1_bf[l, c, g].rearrange("(l d) e -> d l e", d=64), 'w1sb', 4,
                             reads=wkeys[('w1', l)], writes=['w1sb'])
                for li in range(32):
                    mm(psb[7][0:64, 0:1], w1sb[0:64, li, :], pe_sb[0:64, c, li:li + 1], li == 0, li == 31, ['w1sb', 'pe_sb'], 7)
                vop(lambda e: e.tensor_copy(out=biasb[0:64, 0:1], in_=psb[7][0:64, 0:1]), [PS(7)], ['biasb'])
                for li in range(32):
                    rhs = XT16[0:64, 0:255, li] if li < 16 else XT16[0:64, 1:256, li - 16]
                    mm(psb[6][0:64, 0:255], w1sb[0:64, li, :], rhs, li == 0, li == 31, ['w1sb', 'XT'], 6)
                pb.op('scalar', lambda e: e.activation(out=hidT[0:64, 0:255], in_=psb[6][0:64, 0:255],
                                                       func=AF.Gelu_apprx_tanh, bias=biasb[0:64, 0:1]),
                      [PS(6), 'biasb'], ['hidT'])
                if c == 0:
                    mm(psb[5][0:64, 0:256], w2sb[0:64, 0, :], hidT[0:64, 0:256], True, True, ['w2sb', 'hidT'], 5)
                    vop(lambda e: e.tensor_copy(out=KcT[0:64, :], in_=psb[5][0:64, 0:256]), [PS(5)], ['KcT'])
                else:
                    for kt in range(2):
                        mm(psb[5][:, kt * 64:(kt + 1) * 64], hidT[0:64, kt * 128:(kt + 1) * 128], w2sb[0:64, 1, :],
                           True, True, ['w2sb', 'hidT'], 5)
                    for kt in range(2):
                        vop(lambda e, kt=kt: e.tensor_copy(out=Vca[:, kt, 0:64], in_=psb[5][:, kt * 64:(kt + 1) * 64]),
                            [PS(5), 'Vca_c'], [('Vca', kt)])
            pb.dma('sync', Qa[0:64], qT[g * 256:(g + 1) * 256, :].rearrange("(j d) t -> d j t", d=64), 'Qa', writes=['Qa'])
            pb.dma_batch('sync', [(Ka[0:64, :], kvT[2, g * 64:(g + 1) * 64, :]), (Ka[64:128, :], c_eblk[:, :])], 'Ka', writes=['Ka'])
            pb.dma('sync', Kw[0:64, :], kvT[3, g * 64:(g + 1) * 64, :], 'Kw', writes=['Kw'])
            pb.dma_split('sync', Vs[:, :, 0:64], vtok[:, g * 64:(g + 1) * 64].rearrange("(kt k) d -> k kt d", k=128), 'Vs', 8, writes=['Vs', 'Vs1'])
            pb.dma_split('sync', Vw[:, :, 0:64], vtok[:, 128 + g * 64:128 + (g + 1) * 64].rearrange("(kt k) d -> k kt d", k=128),
                         'Vw', 8, writes=['Vw', 'Vw1'])
            vop(lambda e: e.memset(Vs_full[:, :, 64:66], 1.0), ['Vs'], ['Vs1'])
            vop(lambda e: e.memset(Vw_full[:, :, 64:66], 1.0), ['Vw'], ['Vw1'])
            if debug and g == 0 and l == 0 and os.environ.get("KDBGBC") == "1":
                for nm, t_, shp, rk in (('dbgKa', Ka, [128, S], ['Ka']), ('dbgKw', Kw, [128, S], ['Kw']),
                                        ('dbgVs', Vs_full, [128, NQT, 66], ['Vs', 'Vs1']), ('dbgVw', Vw_full, [128, NQT, 66], ['Vw', 'Vw1']),
                                        ('dbgKcT', KcT, [128, 256], ['KcT']), ('dbgVca', Vca_full, [128, 2, 130], [('Vca', 0), ('Vca', 1), 'Vca_c'])):
                    dd = dscr(nm, shp, BF16)
                    pb.dma('sync', dd, t_, nm, reads=rk, writes=[])
                dd = dscr('dbgQa', [64, 4, S], BF16)
                pb.dma('sync', dd, Qa[0:64], 'dbgQa', reads=['Qa'], writes=[])
            sci = [0]

            def nextsc():
                sci[0] = (sci[0] + 1) % 3
                return sci[0]
            pbi = [0]

            def nextpb():
                pbi[0] = (pbi[0] + 1) % 3
                return pbi[0]

            def poc(j):
                return psb[3 + j // 2][:, (j % 2) * 129:(j % 2) * 129 + 129]

            for i in range(NQ):
                qs = slice(i * 128, (i + 1) * 128)
                sl = i % 2
                pb.dma('sync', cmk[sl], c_cmpmask[i], ('cmk', sl), writes=[('cmk', sl)])
                pb.dma_batch('sync', [(sCB[sl][:, 0, :], c_selC[qs, :]), (sCB[sl][:, 1, :], c_selB[qs, :])], ('sCB', sl),
                             writes=[('sCB', sl)])
                gt = gates[:, i, g * 12:(g + 1) * 12].rearrange("p (j b) -> p j b", b=3)
                nkt = 2 if i >= 16 else 1
                for kt in range(nkt):
                    b = nextsc()
                    mm(psb[b][:], KcT[0:64, kt * 128:(kt + 1) * 128], Qa[0:64, :, qs], True, True, ['KcT', 'Qa'], b)
                    pi = nextpb()
                    P = pbuf[pi]
                    pb.op('scalar', lambda e, P=P, b=b: e.activation(out=P, in_=psb[b][:], func=AF.Exp, scale=0.125),
                          [PS(b)], [('P', pi)])
                    vop(lambda e, P=P, kt=kt, sl=sl: e.tensor_tensor(out=P, in0=P, in1=cmk[sl][:, kt, :], op=ALU.mult),
                        [('P', pi), ('cmk', sl)], [('P', pi)])
                    for j in range(4):
                        mm(poc(j), P[:, j * 128:(j + 1) * 128], Vca[:, kt, :], kt == 0 and j % 2 == 0,
                           kt == nkt - 1 and j % 2 == 1, [('P', pi), ('Vca', kt), 'Vca_c'], 3 + j // 2)
                Zc, rZc, fC, rZs, fS = sm[:, 0, :], sm[:, 1, :], sm[:, 2, :], sm[:, 3, :], sm[:, 4, :]
                thr = sm[:, 5, 0:1]
                for h2 in range(2):
                    zv = psb[3 + h2][:, 0:258].rearrange("p (j c) -> p j c", c=129)[:, :, 128]
                    vop(lambda e, h2=h2, zv=zv: e.tensor_scalar(out=Zc[:, 2 * h2:2 * h2 + 2], in0=zv, scalar1=1e-30, scalar2=None,
                                                               op0=ALU.max), [PS(3 + h2)], [('Zc', h2)])
                vop(lambda e: e.reciprocal(out=rZc, in_=Zc), [('Zc', 0), ('Zc', 1)], ['rZc'])
                for j in range(4):
                    dst = imp if j == 0 else tmp64
                    vop(lambda e, j=j, dst=dst: e.tensor_scalar(out=dst, in0=poc(j)[:, 64:128], scalar1=rZc[:, j:j + 1],
                                                                scalar2=None, op0=ALU.mult),
                        [PS(3 + j // 2), 'rZc'], ['imp' if j == 0 else 'tmp64'])
                    if j > 0:
                        vop(lambda e: e.tensor_tensor(out=imp, in0=imp, in1=tmp64, op=ALU.add), ['imp', 'tmp64'], ['imp'])
                vop(lambda e, sl=sl: e.tensor_tensor(out=imp, in0=imp, in1=sCB[sl][:, 0, :], op=ALU.mult), ['imp', ('sCB', sl)], ['imp'])
                vop(lambda e, sl=sl: e.tensor_tensor(out=imp, in0=imp, in1=sCB[sl][:, 1, :], op=ALU.add), ['imp', ('sCB', sl)], ['imp'])
                vop(lambda e: e.max(out=m8, in_=imp), ['imp'], ['m8'])
                vop(lambda e: e.tensor_scalar(out=thr, in0=m8[:, 7:8], scalar1=0.0, scalar2=None, op0=ALU.max), ['m8'], ['thr'])
                vop(lambda e: e.tensor_scalar(out=Mpad[:, 64:128], in0=imp, scalar1=thr, scalar2=-BIG, op0=ALU.is_lt, op1=ALU.mult),
                    ['imp', 'thr'], ['Mpad'])
                pst = psb[7][:].bitcast(BF16)
                pb.op('tensor', lambda e, pst=pst: e.transpose(out=pst[:, 0:128], in_=Mpad, identity=identb),
                      ['Mpad', 'identb'], [PS(7)])
                for j in range(4):
                    if j % 2 == 0:
                        pb.op('scalar', lambda e, j=j, pst=pst, qs=qs: e.activation(out=Qa[64:128, j, qs], in_=pst[64:128, 0:128], func=AF.Copy),
                              [PS(7)], ['Qa'])
                    else:
                        vop(lambda e, j=j, pst=pst, qs=qs: e.tensor_copy(out=Qa[64:128, j, qs], in_=pst[64:128, 0:128]), [PS(7)], ['Qa'])
                vop(lambda e, gt=gt: e.tensor_tensor(out=fC, in0=rZc, in1=gt[:, :, 0], op=ALU.mult), ['rZc', 'gates'], ['fC'])
                for j in range(4):
                    pb.op('scalar', lambda e, j=j: e.activation(out=oacc[:, j * 64:(j + 1) * 64], in_=poc(j)[:, 0:64], func=AF.Copy,
                                                                scale=fC[:, j:j + 1]), [PS(3 + j // 2), 'fC'], [('oacc', j)])
                for br, (bank, Kt, kparts, Vt, vkey, kt0) in enumerate(((5, Ka, 128, Vs, 'Vs', 0), (6, Kw, 64, Vw, 'Vw', max(0, i - 4)))):
                    kts = list(range(kt0, i + 1))
                    for kt in kts:
                        b = nextsc()
                        mm(psb[b][:], Kt[0:kparts, kt * 128:(kt + 1) * 128], Qa[0:kparts, :, qs], True, True,
                           ['Ka' if br == 0 else 'Kw', 'Qa'], b)
                        pi = nextpb()
                        P = pbuf[pi]
                        pb.op('scalar', lambda e, P=P, b=b: e.activation(out=P, in_=psb[b][:], func=AF.Exp, scale=0.125),
                              [PS(b)], [('P', pi)])
                        msk = None
                        if kt == i:
                            msk, mk = tril4, 'tril4'
                        elif br == 1 and kt == i - 4:
                            msk, mk = triu4, 'triu4'
                        if msk is not None:
                            vop(lambda e, P=P, msk=msk: e.tensor_tensor(out=P, in0=P, in1=msk, op=ALU.mult), [('P', pi), mk], [('P', pi)])
                        for j in range(4):
                            mm(psb[bank][:, j * 65:(j + 1) * 65], P[:, j * 128:(j + 1) * 128], Vt[:, kt, :],
                               kt == kts[0] and j == 0, kt == i and j == 3, [('P', pi), vkey, vkey + '1'], bank)
                    zv = psb[bank][:, 0:260].rearrange("p (j c) -> p j c", c=65)[:, :, 64]
                    vop(lambda e, zv=zv: e.reciprocal(out=rZs, in_=zv), [PS(bank)], ['rZs'])
                    vop(lambda e, gt=gt, br=br: e.tensor_tensor(out=fS, in0=rZs, in1=gt[:, :, 1 + br], op=ALU.mult), ['rZs', 'gates'], ['fS'])
                    for j in range(4):
                        pb.op('scalar', lambda e, j=j, bank=bank: e.activation(
                            out=otmp[:, j * 64:(j + 1) * 64], in_=psb[bank][:, j * 65:j * 65 + 64], func=AF.Copy, scale=fS[:, j:j + 1]),
                            [PS(bank), 'fS'], [('otmp', j)])
                    dst, dk = (oacc, 'oaccs') if br == 0 else (ycb, 'ycb')
                    vop(lambda e, dst=dst: e.tensor_tensor(out=dst, in0=oacc, in1=otmp, op=ALU.add),
                        [('oacc', j) for j in range(4)] + [('otmp', j) for j in range(4)] + ['oaccs'],
                        [dk] + ([('oacc', j) for j in range(4)] if br == 0 else []))
                for c2 in range(2):
                    pb.op('tensor', lambda e, c2=c2, pst=pst: e.transpose(out=pst[:, 256 + c2 * 128:256 + (c2 + 1) * 128],
                                                                          in_=ycb[:, c2 * 128:(c2 + 1) * 128], identity=identb),
                          ['ycb', 'identb'], [PS(7)])
                vop(lambda e, pst=pst, qs=qs: e.tensor_copy(out=ycT[:, :, qs], in_=pst[:, 256:512].rearrange("p (c q) -> p c q", q=128)),
                    [PS(7)], ['ycT'])
            pb.dma('sync', yT[1024 + g * 256:1024 + (g + 1) * 256, :].rearrange("(c p) t -> p c t", p=128), ycT, 'yc_out',
                   reads=['ycT'], writes=[])
        pb.barrier()
        ar.reset(base)


    def phase_C(l):
        base = ar.mark()
        NJ = int(os.environ.get("KJC", "8"))
        Wb = ar.alloc([16, D], BF16)
        Wo = ar.alloc([8, D], BF16)
        ysb2 = [ar.alloc([16, 512], BF16) for _ in range(2)]
        gsb2 = [ar.alloc([32, 512], BF16) for _ in range(2)]
        xt2 = [ar.alloc([4, D], F32) for _ in range(2)]
        mT = ar.alloc([8, 512], BF16)
        macc = ar.alloc([512], F32)
        mtmp = ar.alloc([512], F32)
        pb.dma_split('sync', Wb, w_branch_bf[l].rearrange("(k p) d -> p k d", p=128), 'Wb', 4, reads=wkeys[('wb', l)], writes=['Wb'])
        pb.dma_split('sync', Wo, w_out_bf[l].rearrange("(k p) d -> p k d", p=128), 'Wo', 2, reads=wkeys[('wo', l)], writes=['Wo'])
        xsrc = x_in if l == 0 else xres

        def loads(jt):
            t0 = jt * 512
            sl = jt % 2
            pb.dma_split('sync', ysb2[sl], yT[:, t0:t0 + 512].rearrange("(k p) t -> p k t", p=128), ('ysb', sl), 4, writes=[('ysb', sl)])
            pb.dma_split('sync', gsb2[sl], mgT[:, t0:t0 + 512].rearrange("(k p) t -> p k t", p=128), ('gsb', sl), 8, writes=[('gsb', sl)])
            pb.dma('sync', xt2[sl], xsrc[t0:t0 + 512, :].rearrange("(s p) d -> p s d", p=128), ('xtC', sl), writes=[('xtC', sl)])

        loads(0)
        for jt in range(NJ):
            t0 = jt * 512
            sl = jt % 2
            ysb, gsb, xt = ysb2[sl], gsb2[sl], xt2[sl]
            if jt + 1 < NJ:
                loads(jt + 1)
            for dmc in range(8):
                for b in range(4):
                    bank = b
                    for cc in range(4):
                        mm(psb[bank][:], Wb[:, b * 4 + cc, dmc * 128:(dmc + 1) * 128], ysb[:, b * 4 + cc, :], cc == 0, cc == 3,
                           ['Wb', ('ysb', sl)], bank)
                    dst, dk = (macc, 'macc') if b == 0 else (mtmp, 'mtmp')
                    vop(lambda e, dst=dst, bank=bank, b=b, dmc=dmc, gsb=gsb: e.tensor_tensor(out=dst, in0=psb[bank][:], in1=gsb[:, b * 8 + dmc, :],
                                                                                             op=ALU.mult), [PS(bank), ('gsb', sl)], [dk])
                    if b > 0:
                        o = mT[:, dmc, :] if b == 3 else macc
                        vop(lambda e, o=o: e.tensor_tensor(out=o, in0=macc, in1=mtmp, op=ALU.add), ['macc', 'mtmp'],
                            [('mT', dmc)] if b == 3 else ['macc'], eng='gpsimd')
            for s_ in range(4):
                for hf in range(2):
                    bank = 4 + (s_ * 2 + hf) % 4
                    for dmc in range(8):
                        mm(psb[bank][:], mT[:, dmc, s_ * 128:(s_ + 1) * 128], Wo[:, dmc, hf * 512:(hf + 1) * 512], dmc == 0, dmc == 7,
                           [('mT', dmc), 'Wo'], bank)
                    vop(lambda e, s_=s_, hf=hf, bank=bank, xt=xt: e.tensor_tensor(out=xt[:, s_, hf * 512:(hf + 1) * 512], in0=psb[bank][:],
                                                                                 in1=xt[:, s_, hf * 512:(hf + 1) * 512], op=ALU.add),
                        [PS(bank), ('xtC', sl)], [('xtCo', sl, s_, hf)])
            pb.dma('sync', xres[t0:t0 + 512, :].rearrange("(s p) d -> p s d", p=128), xt, ('xtC_out', sl),
                   reads=[('xtCo', sl, s_, hf) for s_ in range(4) for hf in range(2)], writes=[('xtC', sl)])
        pb.barrier()
        ar.reset(base)

    def phase_D(l, last):
        base = ar.mark()
        NJ = int(os.environ.get("KJD", "8"))
        NE = int(os.environ.get("KNE", "16"))
        xt = ar.alloc([4, D], F32)
        xnf = ar.alloc([4, D], F32)
        n2b = ar.alloc([8, 512], BF16)
        xhi = ar.alloc([4, D], BF16)
        xlo = ar.alloc([4, D], BF16)
        hiT = ar.alloc([8, 512], BF16)
        loT = ar.alloc([8, 512], BF16)
        Wrh = ar.alloc([8, 20], BF16)
        Wrl = ar.alloc([8, 20], BF16)
        Wrt = ar.alloc([8, 20], F32)
        junk = ar.alloc([D], F32)
        Wr = ar.alloc([8, 20], F32)
        rbias = ar.alloc([20], F32)
        fng = ar.alloc([D], F32)
        Wg = [ar.alloc([8, 512], BF16) for _ in range(2)]
        Wu = [ar.alloc([8, 512], BF16) for _ in range(2)]
        Wd = [ar.alloc([4, D], BF16) for _ in range(2)]
        hid = [ar.alloc([4, 512], BF16) for _ in range(2)]
        sg = [ar.alloc([512], F32) for _ in range(2)]
        dtmp = [ar.alloc([512], F32) for _ in range(2)]
        ss = ar.alloc([8], F32)
        rs = ar.alloc([8], F32)
        lg = ar.alloc([4, 20], F32)
        comb = ar.alloc([4, 16], F32)
        sm = ar.alloc([16, 8], F32)
        m16 = ar.alloc([16], F32)
        e16 = ar.alloc([16], F32)
        mx8 = ar.alloc([8], F32)
        pb.dma_split('sync', Wr, router_w[l].rearrange("(k p) c -> p k c", p=128), 'Wr', 2, writes=['Wr'])
        for kc in range(8):
            vop(lambda e, kc=kc: e.tensor_scalar(out=Wr[:, kc, :], in0=Wr[:, kc, :], scalar1=vec[:, 8 + kc:9 + kc], scalar2=None,
                                                 op0=ALU.mult), ['Wr', 'vec'], ['Wr'])
        vop(lambda e: e.tensor_copy(out=Wrh, in_=Wr), ['Wr'], ['Wrh'])
        vop(lambda e: e.tensor_tensor(out=Wrt, in0=Wr, in1=Wrh, op=ALU.subtract), ['Wr', 'Wrh'], ['Wrt'])
        vop(lambda e: e.tensor_copy(out=Wrl, in_=Wrt), ['Wrt'], ['Wrl'])
        pb.dma('sync', rbias, rows[l, :, 1024:1044], 'rbias', writes=['rbias'])
        pb.dma('sync', fng, rows[l, :, 1044:2068], 'fng', writes=['fng'])
        for jt in range(NJ):
            t0 = jt * 512
            pb.dma('sync', xt, xres[t0:t0 + 512, :].rearrange("(s p) d -> p s d", p=128), 'xtD', writes=['xtD'])
            if int(os.environ.get("KD", "9")) <= 1:
                pb.dma('sync', out_d[t0:t0 + 512, :].rearrange("(s p) d -> p s d", p=128), xt, 'out_st', reads=['xtD'], writes=[])
                continue
            for s_ in range(4):
                rmsnorm_rstd((xt[:, s_, :], ['xtD']), junk, ss[:, s_:s_ + 1], rs[:, s_:s_ + 1], D, 'D%d' % s_)
                vop(lambda e, s_=s_: e.tensor_scalar(out=xnf[:, s_, :], in0=xt[:, s_, :], scalar1=rs[:, s_:s_ + 1], scalar2=None,
                                                     op0=ALU.mult), ['xtD', 'D%drs' % s_], [('xnf', s_)])
            KB = os.environ.get("KB", "z")
            if KB == "a":
                pb.dma('sync', out_d[t0:t0 + 512, :].rearrange("(s p) d -> p s d", p=128), xnf, 'out_st',
                       reads=[('xnf', s_) for s_ in range(4)], writes=[])
                continue
            for s_ in range(4):
                vop(lambda e, s_=s_: e.tensor_copy(out=xhi[:, s_, :], in_=xnf[:, s_, :]), [('xnf', s_)], [('xhi', s_)])
                vop(lambda e, s_=s_: e.tensor_tensor(out=xlo[:, s_, :], in0=xnf[:, s_, :], in1=xhi[:, s_, :], op=ALU.subtract),
                    [('xnf', s_), ('xhi', s_)], [('xlo', s_)])
            if KB == "b":
                pb.dma('sync', out_d[t0:t0 + 512, :].rearrange("(s p) d -> p s d", p=128), xnf, 'out_st',
                       reads=[('xnf', s_) for s_ in range(4)] + [('xlo', s_) for s_ in range(4)], writes=[])
                continue
            KEV = os.environ.get("KEV", "d")
            for kc in range(8):
                for hl, (src, sk) in enumerate(((xhi, 'xhi'), (xlo, 'xlo'))):
                    bank = 6 + hl
                    pst = psb[bank][:].bitcast(BF16)
                    for s_ in range(4):
                        pb.op('tensor', lambda e, s_=s_, kc=kc, pst=pst, src=src: e.transpose(
                            out=pst[:, s_ * 128:(s_ + 1) * 128], in_=src[:, s_, kc * 128:(kc + 1) * 128], identity=identb),
                            [(sk, s_), 'identb'], [PS(bank)])
                    if hl == 0:
                        vop(lambda e, kc=kc, pst=pst: e.tensor_scalar(out=n2b[:, kc, :], in0=pst[:, 0:512], scalar1=vec[:, 8 + kc:9 + kc],
                                                                     scalar2=None, op0=ALU.mult), [PS(bank), 'vec'], [('n2b', kc)])
                        if KEV == "d":
                            vop(lambda e, kc=kc, pst=pst: e.tensor_scalar(out=hiT[:, kc, :], in0=pst[:, 0:512], scalar1=1.0,
                                                                         scalar2=None, op0=ALU.mult), [PS(bank)], [('hiT', kc)])
                        else:
                            pb.op('scalar', lambda e, kc=kc, pst=pst: e.activation(out=hiT[:, kc, :], in_=pst[:, 0:512], func=AF.Copy),
                                  [PS(bank)], [('hiT', kc)])
                    else:
                        if KEV == "d":
                            vop(lambda e, kc=kc, pst=pst: e.tensor_scalar(out=loT[:, kc, :], in0=pst[:, 0:512], scalar1=1.0,
                                                                         scalar2=None, op0=ALU.mult), [PS(bank)], [('loT', kc)])
                        else:
                            vop(lambda e, kc=kc, pst=pst: e.tensor_copy(out=loT[:, kc, :], in_=pst[:, 0:512]), [PS(bank)], [('loT', kc)])
            if KB == "c":
                pb.dma('sync', out_d[t0:t0 + 512, :].rearrange("(s p) d -> p s d", p=128), xnf, 'out_st',
                       reads=[('xnf', s_) for s_ in range(4)] + [('n2b', kc) for kc in range(8)] + [('hiT', kc) for kc in range(8)] + [('loT', kc) for kc in range(8)], writes=[])
                continue
            KD = int(os.environ.get("KD", "9"))
            for s_ in range(4 if KD >= 3 else 0):
                bank = 5
                passes = [(hiT, 'hiT', Wrh, 'Wrh'), (loT, 'loT', Wrh, 'Wrh'), (hiT, 'hiT', Wrl, 'Wrl')]
                for pi_, (xa, xk, wa, wk) in enumerate(passes):
                    for kc in range(8):
                        mm(psb[bank][:, 0:20], xa[:, kc, s_ * 128:(s_ + 1) * 128], wa[:, kc, :], pi_ == 0 and kc == 0,
                           pi_ == 2 and kc == 7, [(xk, kc), wk], bank)
                if KD < 4:
                    vop(lambda e, s_=s_: e.memset(comb[:, s_, :], 0.0), [], [('comb', s_)])
                    continue
                L = lg[:, s_, :]
                gmax, ngm, gsum, m1n, e2, coef = (sm[:, i_, s_:s_ + 1] for i_ in range(6))
                ohg = sm[:, 6 + s_ // 2, (s_ % 2) * 4:(s_ % 2) * 4 + 4]
                eg = sm[:, 8 + s_ // 2, (s_ % 2) * 4:(s_ % 2) * 4 + 4]
                vop(lambda e, L=L: e.tensor_tensor(out=L, in0=psb[5][:, 0:20], in1=rbias, op=ALU.add), [PS(5), 'rbias'], ['L'])
                vop(lambda e, L=L, gmax=gmax: e.reduce_max(out=gmax, in_=L[:, 0:4], axis=AX.X), ['L'], ['gmax'])
                vop(lambda e, gmax=gmax, ngm=ngm: e.tensor_scalar(out=ngm, in0=gmax, scalar1=-1.0, scalar2=None, op0=ALU.mult), ['gmax'], ['ngm'])
                pb.op('scalar', lambda e, L=L, eg=eg, ngm=ngm: e.activation(out=eg, in_=L[:, 0:4], func=AF.Exp, bias=ngm), ['L', 'ngm'], ['eg'])
                vop(lambda e, eg=eg, gsum=gsum: e.reduce_sum(out=gsum, in_=eg, axis=AX.X), ['eg'], ['gsum'])
                vop(lambda e, L=L, ohg=ohg, gmax=gmax: e.tensor_scalar(out=ohg, in0=L[:, 0:4], scalar1=gmax, scalar2=None, op0=ALU.is_ge),
                    ['L', 'gmax'], ['ohg'])
                vop(lambda e, ohg=ohg: e.tensor_scalar(out=ohg, in0=ohg, scalar1=1.0, scalar2=1e9, op0=ALU.subtract, op1=ALU.mult), ['ohg'], ['ohg'])
                for g_ in range(4):
                    vop(lambda e, g_=g_, L=L, ohg=ohg: e.tensor_scalar(out=m16[:, g_ * 4:(g_ + 1) * 4], in0=L[:, 4 + g_ * 4:8 + g_ * 4],
                                                                     scalar1=ohg[:, g_:g_ + 1], scalar2=None, op0=ALU.add), ['L', 'ohg'], ['m16'])
                vop(lambda e: e.max(out=mx8, in_=m16), ['m16'], ['mx8'])
                vop(lambda e, m1n=m1n: e.tensor_scalar(out=m1n, in0=mx8[:, 0:1], scalar1=-1.0, scalar2=None, op0=ALU.mult), ['mx8'], ['m1n'])
                pb.op('scalar', lambda e, m1n=m1n: e.activation(out=e16, in_=m16, func=AF.Exp, bias=m1n), ['m16', 'm1n'], ['e16'])
                pb.op('scalar', lambda e, m1n=m1n, e2=e2: e.activation(out=e2, in_=mx8[:, 1:2], func=AF.Exp, bias=m1n), ['mx8', 'm1n'], ['e2'])
                vop(lambda e, e2=e2: e.tensor_scalar(out=e2, in0=e2, scalar1=1.0, scalar2=None, op0=ALU.add), ['e2'], ['e2'])
                vop(lambda e, e2=e2, gsum=gsum, coef=coef: e.tensor_tensor(out=coef, in0=e2, in1=gsum, op=ALU.mult), ['e2', 'gsum'], ['coef'])
                vop(lambda e, coef=coef: e.reciprocal(out=coef, in_=coef), ['coef'], ['coef'])
                vop(lambda e: e.tensor_scalar(out=m16, in0=m16, scalar1=mx8[:, 1:2], scalar2=None, op0=ALU.is_ge), ['m16', 'mx8'], ['m16'])
                vop(lambda e: e.tensor_tensor(out=e16, in0=e16, in1=m16, op=ALU.mult), ['e16', 'm16'], ['e16'])
                vop(lambda e, s_=s_, coef=coef: e.tensor_scalar(out=comb[:, s_, :], in0=e16, scalar1=coef, scalar2=None, op0=ALU.mult),
                    ['e16', 'coef'], [('comb', s_)])
            def stage_gu(ex):
                sl = ex % 2
                hd = hid[sl]
                pb.dma_split('sync', Wg[sl], moe_g_bf[l, ex].rearrange("(k p) f -> p k f", p=128), ('Wg', sl), 2, reads=wkeys[('moe', l, ex)], writes=[('Wg', sl)])
                pb.dma_split('sync', Wu[sl], moe_u_bf[l, ex].rearrange("(k p) f -> p k f", p=128), ('Wu', sl), 2, reads=wkeys[('moe', l, ex)], writes=[('Wu', sl)])
                pb.dma('sync', Wd[sl], moe_d_bf[l, ex].rearrange("(k p) d -> p k d", p=128), ('Wd', sl), reads=wkeys[('moe', l, ex)], writes=[('Wd', sl)])
                for fc in range(4):
                    bg, bu = (fc % 2) * 2, (fc % 2) * 2 + 1
                    for kc in range(8):
                        mm(psb[bg][:], Wg[sl][:, kc, fc * 128:(fc + 1) * 128], n2b[:, kc, :], kc == 0, kc == 7, [('Wg', sl), ('n2b', kc)], bg)
                    for kc in range(8):
                        mm(psb[bu][:], Wu[sl][:, kc, fc * 128:(fc + 1) * 128], n2b[:, kc, :], kc == 0, kc == 7, [('Wu', sl), ('n2b', kc)], bu)
                    sgt = sg[fc % 2]
                    pb.op('scalar', lambda e, sgt=sgt, bg=bg: e.activation(out=sgt, in_=psb[bg][:], func=AF.Silu), [PS(bg)], [('sg', fc % 2)])
                    vop(lambda e, sgt=sgt, bu=bu, fc=fc, hd=hd: e.tensor_tensor(out=hd[:, fc, :], in0=psb[bu][:], in1=sgt, op=ALU.mult),
                        [PS(bu), ('sg', fc % 2)], [('hid', sl, fc)])

            def stage_down(ex):
                sl = ex % 2
                hd = hid[sl]
                for s_ in range(4):
                    for hf in range(2):
                        idx = s_ * 2 + hf
                        bank = 4 + idx % 2
                        for fc in range(4):
                            mm(psb[bank][:], hd[:, fc, s_ * 128:(s_ + 1) * 128], Wd[sl][:, fc, hf * 512:(hf + 1) * 512], fc == 0, fc == 3,
                               [('hid', sl, fc), ('Wd', sl)], bank)
                        dt_ = dtmp[idx % 2]
                        vop(lambda e, dt_=dt_, bank=bank, s_=s_, ex=ex: e.tensor_scalar(out=dt_, in0=psb[bank][:], scalar1=comb[:, s_, ex:ex + 1],
                                                                                       scalar2=None, op0=ALU.mult),
                            [PS(bank), ('comb', s_)], [('dtmp', idx % 2)])
                        vop(lambda e, dt_=dt_, s_=s_, hf=hf: e.tensor_tensor(out=xt[:, s_, hf * 512:(hf + 1) * 512], in0=xt[:, s_, hf * 512:(hf + 1) * 512],
                                                                            in1=dt_, op=ALU.add), [('dtmp', idx % 2), 'xtD', ('xacc', s_, hf)], [('xacc', s_, hf)],
                            eng='gpsimd')

            if NE > 0:
                stage_gu(0)
            for ex in range(NE):
                if ex + 1 < NE:
                    stage_gu(ex + 1)
                stage_down(ex)
            allx = [('xacc', s_, hf) for s_ in range(4) for hf in range(2)]
            if not last:
                pb.dma('sync', xres[t0:t0 + 512, :].rearrange("(s p) d -> p s d", p=128), xt, 'xtD_out', reads=allx, writes=['xtD'])
            else:
                for s_ in range(4):
                    rmsnorm_rstd((xt[:, s_, :], allx), junk, ss[:, 4 + s_:5 + s_], rs[:, 4 + s_:5 + s_], D, 'F%d' % s_)
                    vop(lambda e, s_=s_: e.tensor_scalar(out=xnf[:, s_, :], in0=xt[:, s_, :], scalar1=rs[:, 4 + s_:5 + s_], scalar2=None,
                                                         op0=ALU.mult), allx + ['F%drs' % s_], [('xnf', s_)])
                    vop(lambda e, s_=s_: e.tensor_tensor(out=xnf[:, s_, :], in0=xnf[:, s_, :], in1=fng, op=ALU.mult), [('xnf', s_), 'fng'], [('xnf', s_)])
                pb.dma('sync', out_d[t0:t0 + 512, :].rearrange("(s p) d -> p s d", p=128), xnf, 'out_st',
                       reads=[('xnf', s_) for s_ in range(4)], writes=['xtD'])
        pb.barrier()
        ar.reset(base)

    for l in range(nlayers):
        if stop_after == ('conv',):
            break
        phase_A(l)
        if stop_after == ('A', l):
            break
        phase_BA(l)
        phase_BB(l)
        phase_BD(l)
        if stop_after == ('B1', l):
            break
        if os.environ.get("KSKIPBC") != "1":
            phase_BC(l)
        if stop_after == ('B2', l):
            break
        phase_C(l)
        if stop_after == ('C', l):
            break
        if l == 0 and nlayers > 1:
            conv_w_in(1)
            conv_rest(1)
        phase_D(l, l == nlayers - 1)
        if stop_after == ('D', l):
            break

    pb.barrier()
    pb.emit()


def _consts():
    bf = ml_dtypes.bfloat16
    k = np.arange(128)[:, None]
    q = np.arange(128)[None, :]
    tril = (k <= q).astype(np.float32)
    triu = (k > q).astype(np.float32)
    c = {}
    c['c_ident'] = np.eye(128, dtype=np.float32)
    c['c_tril4'] = np.tile(tril, (1, 4)).astype(bf)
    c['c_triu4'] = np.tile(triu, (1, 4)).astype(bf)
    n = np.arange(256)
    t = np.arange(S)
    vis = ((n[:, None] * 16 + 31) <= t[None, :]) & (n[:, None] < 255)
    m = vis.reshape(2, 128, NQT, 128).transpose(2, 1, 0, 3)
    c['c_cmpmask'] = np.ascontiguousarray(np.tile(m, (1, 1, 1, 4))).astype(bf)
    c_start = np.arange(256) * 16
    s_start = np.arange(64) * 64
    ov = ((c_start[:, None] <= s_start[None, :] + 63) & (c_start[:, None] + 31 >= s_start[None, :])).astype(np.float32)
    ov[255] = 0
    ovc = np.concatenate([ov, np.ones((256, 1), np.float32)], axis=1)
    ovc[255] = 0
    c['c_ovc'] = np.ascontiguousarray(ovc.reshape(2, 128, 65).transpose(1, 0, 2)).astype(bf)
    kk = np.arange(S)
    c['c_eblk'] = (kk[None, :] // 64 == np.arange(64)[:, None]).astype(bf)
    blk = np.arange(64)
    cur = t // 64
    causal = blk[None, :] * 64 <= t[:, None]
    forced = (blk[None, :] == 0) | (blk[None, :] == cur[:, None]) | (blk[None, :] == cur[:, None] - 1)
    c['c_selC'] = (causal & ~forced).astype(np.float32)
    c['c_selB'] = np.where(forced, 1e6, np.where(causal, 0.0, -1.0)).astype(np.float32)
    pc = np.zeros((128, 4, 16), np.float32)
    for gi, w in enumerate((2, 4, 8, 16)):
        pc[:, gi, :] = 1.0 / np.minimum(np.arange(16) + 1, w)
    c['c_pcorr'] = pc
    return c


def _prep(inp):
    f = np.float32
    L = DEPTH
    shared = {}
    shared['w_in'] = np.ascontiguousarray(inp['w_in'], dtype=f)
    shared['w_branch'] = np.ascontiguousarray(inp['w_branch'], dtype=f).reshape(L, 4 * MIX, D)
    shared['w_out'] = np.ascontiguousarray(inp['w_out'], dtype=f)
    shared['moe_g'] = np.ascontiguousarray(inp['moe_w_gate'], dtype=f)
    shared['moe_u'] = np.ascontiguousarray(inp['moe_w_up'], dtype=f)
    shared['moe_d'] = np.ascontiguousarray(inp['moe_w_down'], dtype=f)
    shared['gm_wsT'] = np.ascontiguousarray(np.transpose(inp['gm_ws'], (0, 1, 3, 2)), dtype=f)
    shared['lru_wa'] = np.ascontiguousarray(inp['lru_wa'], dtype=f)
    shared['lru_wx'] = np.ascontiguousarray(inp['lru_wx'], dtype=f)
    shared['cmp_w1'] = np.ascontiguousarray(inp['cmp_w1'], dtype=f)
    shared['cmp_w2'] = np.ascontiguousarray(inp['cmp_w2'], dtype=f)
    shared['cmp_peT'] = np.ascontiguousarray(np.transpose(inp['cmp_pe'], (0, 1, 3, 2)), dtype=f)
    shared['pool_w'] = np.ascontiguousarray(inp['pool_w'], dtype=f)
    shared['router_w'] = np.ascontiguousarray(np.concatenate([inp['router_w_group'], inp['router_w_expert']], axis=2), dtype=f)
    vecs = np.zeros((L, 128, NV), f)
    rows = np.zeros((L, 128, NR), f)
    for l in range(L):
        vecs[l, :, 0:8] = inp['norm1_g'][l].reshape(8, 128).T
        vecs[l, :, 8:16] = inp['norm2_g'][l].reshape(8, 128).T
        for k in range(4):
            vecs[l, :, 16 + 4 * k:20 + 4 * k] = inp['conv_w'][l, k].reshape(4, 128).T
        vecs[l, :, 32:36] = inp['conv_b'][l].reshape(4, 128).T
        vecs[l, :, 36:40] = inp['lru_ba'][l].reshape(4, 128).T
        vecs[l, :, 40:44] = inp['lru_bx'][l].reshape(4, 128).T
        vecs[l, :, 44:48] = inp['lru_lambda'][l].reshape(4, 128).T
        vecs[l, :, 48:52] = inp['pool_scale'][l].reshape(4, 128).T
        rows[l, :, 0:512] = inp['gm_norm_g'][l][None, :]
        rows[l, :, 512:1024] = inp['gm_b'][l].reshape(1, 512)
        rows[l, :, 1024:1028] = inp['router_b_group'][l][None, :]
        rows[l, :, 1028:1044] = inp['router_b_expert'][l][None, :]
        rows[l, :, 1044:2068] = inp['final_norm_g'][None, :]
    shared['vecs'] = vecs
    shared['rows'] = rows
    shared.update(_consts())
    return shared


def kernel(**inputs):
    shared = _prep(inputs)
    x = np.ascontiguousarray(inputs['x'], dtype=np.float32)
    nc = build()
    in_maps = []
    for c in range(8):
        m = dict(shared)
        m['x'] = x[c % 4]
        in_maps.append(m)
    res = run_bass_kernel_spmd(nc, in_maps, core_ids=list(range(8)))
    out = np.stack([res.results[c]['out'] for c in range(4)], axis=0)
    return out.astype(np.float32)
```

```python
import numpy as np
import ml_dtypes
from contextlib import ExitStack
import concourse.bass as bass
import concourse.mybir as mybir
from concourse.bass_utils import run_bass_kernel_spmd

F32 = mybir.dt.float32
BF16 = mybir.dt.bfloat16
AF = mybir.ActivationFunctionType
ALU = mybir.AluOpType
AX = mybir.AxisListType

S = 4096
D = 1024
MIX = 512
NIN = 7960
DEPTH = 2
EPS = 1e-6
NQT = 32
OFF = dict(u=0, v=512, gb=1024, rb=1536, q=2048, kcmp=2560, vcmp=2688, kslc=2816, vslc=2944,
           kwin=3072, vwin=3200, ng=3328, xd=3352, mg=3864)
NV = 64
NR = 2068
BIG = 30000.0
SAME_ENG_WAIT = True
ENGS = ['sync', 'scalar', 'vector', 'gpsimd', 'tensor']


class PB:
    def __init__(self, nc, es):
        self.nc, self.es = nc, es
        self.q = {e: [] for e in ENGS}
        self.cnt = {e: 0 for e in ENGS}
        self.seen = {e: {} for e in ENGS}
        self.buf = {}
        self.sems = {}
        self.dcnt = {}

    def _need(self, eng, ev, waits):
        if ev is None:
            return
        sk, val = ev
        if sk == ('e', eng) and (eng == 'tensor' or not SAME_ENG_WAIT):
            return
        if self.seen[eng].get(sk, 0) >= val:
            return
        self.seen[eng][sk] = val
        waits[sk] = max(waits.get(sk, 0), val)

    def _deps(self, eng, reads, writes):
        waits = {}
        for k in reads:
            b = self.buf.get(k)
            if b:
                self._need(eng, b[0], waits)
        for k in writes:
            b = self.buf.get(k)
            if b:
                self._need(eng, b[0], waits)
                for sk, val in b[1].items():
                    self._need(eng, (sk, val), waits)
        return list(waits.items())

    def _commit(self, ev, reads, writes):
        for k in reads:
            b = self.buf.setdefault(k, [None, {}])
            b[1][ev[0]] = max(b[1].get(ev[0], 0), ev[1])
        for k in writes:
            self.buf[k] = [ev, {}]

    def op(self, eng, fn, reads=(), writes=()):
        waits = self._deps(eng, reads, writes)
        self.cnt[eng] += 1
        ev = (('e', eng), self.cnt[eng])
        self._commit(ev, reads, writes)
        self.q[eng].append((waits, fn, (('e', eng), 1)))

    def dma(self, eng, out, in_, dkey, reads=(), writes=()):
        waits = self._deps(eng, reads, writes)
        self.dcnt[dkey] = self.dcnt.get(dkey, 0) + 16
        ev = (('d', dkey), self.dcnt[dkey])
        self._commit(ev, reads, writes)
        self.q[eng].append((waits, (lambda e: e.dma_start(out=out, in_=in_)), (('d', dkey), 16)))

    def dma_batch(self, eng, pairs, dkey, reads=(), writes=()):
        waits = self._deps(eng, reads, writes)
        self.dcnt[dkey] = self.dcnt.get(dkey, 0) + 16 * len(pairs)
        ev = (('d', dkey), self.dcnt[dkey])
        self._commit(ev, reads, writes)
        for i, (out, in_) in enumerate(pairs):
            self.q[eng].append((waits if i == 0 else [], (lambda e, out=out, in_=in_: e.dma_start(out=out, in_=in_)),
                                (('d', dkey), 16)))

    def dma_split(self, eng, out, in_, dkey, n, reads=(), writes=()):
        A = out.shape[1]
        assert A % n == 0 and in_.shape[1] == A, (out.shape, in_.shape, n)
        c = A // n
        pairs = [(out[:, i * c:(i + 1) * c], in_[:, i * c:(i + 1) * c]) for i in range(n)]
        self.dma_batch(eng, pairs, dkey, reads=reads, writes=writes)

    def group_done(self, dkey, key):
        self.buf[key] = [(('d', dkey), self.dcnt[dkey]), {}]

    def wait_event(self, eng, ev):
        waits = {}
        self._need(eng, ev, waits)
        if waits:
            self.q[eng].append((list(waits.items()), None, None))

    def barrier(self):
        evs = [(('e', e), self.cnt[e]) for e in ENGS if self.cnt[e] > 0]
        evs += [(('d', k), v) for k, v in self.dcnt.items()]
        for eng in ENGS:
            waits = {}
            for ev in evs:
                self._need(eng, ev, waits)
            if waits:
                self.q[eng].append((list(waits.items()), None, None))

    def emit(self):
        allsk = set()
        for e in ENGS:
            for waits, fn, inc in self.q[e]:
                for sk, _ in waits:
                    allsk.add(sk)
                if inc is not None:
                    allsk.add(inc[0])
        for i, sk in enumerate(sorted(allsk, key=repr)):
            self.sems[sk] = self.es.enter_context(self.nc.semaphore("s%d" % i))
        block = self.es.enter_context(self.nc.Block())
        for e in ENGS:
            def body(engine, e=e):
                for waits, fn, inc in self.q[e]:
                    for sk, val in waits:
                        engine.wait_ge(self.sems[sk], val)
                    if fn is not None:
                        ins = fn(engine)
                        ins.then_inc(self.sems[inc[0]], inc[1])
            getattr(block, e)(body)


class Arena:
    def __init__(self, ap_f32, nwords):
        self.ap, self.n, self.off = ap_f32, nwords, 0

    def mark(self):
        return self.off

    def reset(self, off):
        self.off = off

    def alloc(self, free_shape, dtype):
        n = int(np.prod(free_shape))
        nw = n if dtype == F32 else (n + 1) // 2
        nw = (nw + 7) // 8 * 8
        w0 = self.off
        self.off += nw
        assert self.off <= self.n, ("SBUF arena overflow", self.off, self.n)
        v = self.ap[:, w0:w0 + nw]
        if dtype != F32:
            v = v.bitcast(dtype)
        v = v[:, 0:n]
        if len(free_shape) == 2:
            v = v.rearrange("p (a b) -> p a b", a=free_shape[0], b=free_shape[1])
        elif len(free_shape) == 3:
            v = v.rearrange("p (a b c) -> p a b c", a=free_shape[0], b=free_shape[1], c=free_shape[2])
        return v


def flat2d(ap, cols):
    nd = len(ap.shape)
    names = " ".join("a%d" % i for i in range(nd))
    f = ap.rearrange("%s -> (%s)" % (names, names))
    return f.rearrange("(r c) -> r c", c=cols)


def build(stop_after=None, debug=False, nlayers=DEPTH):
    nc = bass.Bass("TRN2", target_bir_lowering=False)
    with ExitStack() as es:
        _build(nc, es, stop_after, debug, nlayers)
    return nc


def _build(nc, es, stop_after, debug, nlayers):
    pb = PB(nc, es)
    dbg_kind = "ExternalOutput" if debug else "Internal"

    def din(name, shape, dt=F32):
        return nc.dram_tensor(name, list(shape), dt, kind="ExternalInput").ap()

    def dscr(name, shape, dt, kind=None):
        return nc.dram_tensor(name, list(shape), dt, kind=kind or dbg_kind).ap()

    x_in = din("x", [S, D])
    w_in = din("w_in", [DEPTH, D, NIN])
    w_branch = din("w_branch", [DEPTH, 4 * MIX, D])
    w_out = din("w_out", [DEPTH, D, D])
    moe_g = din("moe_g", [DEPTH, 16, D, 512])
    moe_u = din("moe_u", [DEPTH, 16, D, 512])
    moe_d = din("moe_d", [DEPTH, 16, 512, D])
    gm_wsT = din("gm_wsT", [DEPTH, 4, 128, 128])
    lru_wa = din("lru_wa", [DEPTH, 8, 64, 64])
    lru_wx = din("lru_wx", [DEPTH, 8, 64, 64])
    cmp_w1 = din("cmp_w1", [DEPTH, 2, 2, 2048, 64])
    cmp_w2 = din("cmp_w2", [DEPTH, 2, 2, 64, 64])
    cmp_peT = din("cmp_peT", [DEPTH, 2, 64, 32])
    pool_w = din("pool_w", [DEPTH, 4, 128, 128])
    router_w = din("router_w", [DEPTH, D, 20])
    vecs = din("vecs", [DEPTH, 128, NV])
    rows = din("rows", [DEPTH, 128, NR])
    c_ident = din("c_ident", [128, 128])
    c_tril4 = din("c_tril4", [128, 512], BF16)
    c_triu4 = din("c_triu4", [128, 512], BF16)
    c_cmpmask = din("c_cmpmask", [NQT, 128, 2, 512], BF16)
    c_ovc = din("c_ovc", [128, 2, 65], BF16)
    c_eblk = din("c_eblk", [64, S], BF16)
    c_selC = din("c_selC", [S, 64])
    c_selB = din("c_selB", [S, 64])
    c_pcorr = din("c_pcorr", [128, 4, 16])
    out_d = nc.dram_tensor("out", [S, D], F32, kind="ExternalOutput").ap()

    w_in_bf = dscr("w_in_bf", [DEPTH, D, NIN], BF16, "Internal")
    w_branch_bf = dscr("w_branch_bf", [DEPTH, 4 * MIX, D], BF16, "Internal")
    w_out_bf = dscr("w_out_bf", [DEPTH, D, D], BF16, "Internal")
    moe_g_bf = dscr("moe_g_bf", [DEPTH, 16, D, 512], BF16, "Internal")
    moe_u_bf = dscr("moe_u_bf", [DEPTH, 16, D, 512], BF16, "Internal")
    moe_d_bf = dscr("moe_d_bf", [DEPTH, 16, 512, D], BF16, "Internal")
    cmp_w1_bf = dscr("cmp_w1_bf", [DEPTH, 2, 2, 2048, 64], BF16, "Internal")
    uT = dscr("uT", [MIX, S], BF16)
    vn = dscr("vn", [S, MIX], BF16)
    gbT = dscr("gbT", [MIX, S], BF16)
    rbT = dscr("rbT", [MIX, S], BF16)
    qT = dscr("qT", [MIX, S], BF16)
    kvT = dscr("kvT", [4, 128, S], BF16)
    vtok = dscr("vtok", [S, 256], BF16)
    gC = dscr("gC", [S, 24], F32)
    xdT = dscr("xdT", [MIX, S], BF16)
    mgT = dscr("mgT", [4 * D, S], BF16)
    yT = dscr("yT", [4 * MIX, S], BF16)
    xres = dscr("xres", [S, D], F32)

    ARW = 48 * 1024
    arena_t = es.enter_context(nc.sbuf_tensor("arena", [128, ARW], F32))
    ar = Arena(arena_t, ARW)
    psb = [es.enter_context(nc.psum_tensor("ps%d" % i, [128, 512], F32)) for i in range(8)]

    def PS(i):
        return ('ps', i)

    identb = ar.alloc([128], BF16)
    identf = ar.alloc([128], F32)
    tril4 = ar.alloc([512], BF16)
    triu4 = ar.alloc([512], BF16)
    vec = ar.alloc([NV], F32)
    pb.dma('gpsimd', identb, c_ident[:, :], 'identb', writes=['identb'])
    pb.dma('sync', identf, c_ident[:, :], 'identf', writes=['identf'])
    pb.dma('sync', tril4, c_tril4[:, :], 'tril4', writes=['tril4'])
    pb.dma('sync', triu4, c_triu4[:, :], 'triu4', writes=['triu4'])
    PERSIST = ar.mark()

    import os
    STAGE = int(os.environ.get("KSTAGE", "9"))

    cv_n = [0]

    def cv_dma(dst, src, key):
        slot = cv_n[0] % 4
        cv_n[0] += 1
        dk = ('cv', slot)
        if dk in pb.dcnt:
            pb.wait_event('gpsimd', (('d', dk), pb.dcnt[dk]))
        pb.dma('gpsimd', dst, src, dk, writes=[key])

    wkeys = {}

    def conv_w_in(l):
        ks = []
        for kc in range(8):
            ks.append(('w_in_bf', l, kc))
            cv_dma(w_in_bf[l, kc * 128:(kc + 1) * 128, :], w_in[l, kc * 128:(kc + 1) * 128, :], ks[-1])
        wkeys[('w_in', l)] = ks

    def conv_flat(dst, src, key, cols=4096, rows_per=128):
        d2, s2 = flat2d(dst, cols), flat2d(src, cols)
        nr = d2.shape[0]
        ks = []
        for r0 in range(0, nr, rows_per):
            r1 = min(nr, r0 + rows_per)
            ks.append((key, r0))
            cv_dma(d2[r0:r1, :], s2[r0:r1, :], ks[-1])
        return ks

    def conv_rest(l):
        wkeys[('w1', l)] = conv_flat(cmp_w1_bf[l], cmp_w1[l], ('cmp_w1_bf', l))
        wkeys[('wb', l)] = conv_flat(w_branch_bf[l], w_branch[l], ('w_branch_bf', l))
        wkeys[('wo', l)] = conv_flat(w_out_bf[l], w_out[l], ('w_out_bf', l))
        if STAGE < 4:
            return
        for e in range(int(os.environ.get("KNE", "16"))):
            wkeys[('moe', l, e)] = (conv_flat(moe_g_bf[l, e], moe_g[l, e], ('moe_g_bf', l, e))
                                    + conv_flat(moe_u_bf[l, e], moe_u[l, e], ('moe_u_bf', l, e))
                                    + conv_flat(moe_d_bf[l, e], moe_d[l, e], ('moe_d_bf', l, e)))

    if STAGE >= 2:
        conv_w_in(0)
    if STAGE >= 3:
        conv_rest(0)

    def rmsnorm_rstd(xt_s, junk, ss, rs, nfeat, tag):
        pb.op('scalar', lambda e: e.activation(out=junk, in_=xt_s[0], func=AF.Square),
              reads=xt_s[1], writes=[tag + 'junk'])
        pb.op('vector', lambda e: e.reduce_sum(out=ss, in_=junk, axis=AX.X), reads=[tag + 'junk'], writes=[tag + 'ss'])
        pb.op('vector', lambda e: e.tensor_scalar(out=rs, in0=ss, scalar1=1.0 / nfeat, scalar2=EPS,
                                                  op0=ALU.mult, op1=ALU.add), reads=[tag + 'ss'], writes=[tag + 'rs'])
        pb.op('scalar', lambda e: e.activation(out=rs, in_=rs, func=AF.Sqrt), reads=[tag + 'rs'], writes=[tag + 'rs'])
        pb.op('vector', lambda e: e.reciprocal(out=rs, in_=rs), reads=[tag + 'rs'], writes=[tag + 'rs'])

    def phase_A(l):
        base = ar.mark()
        Wsb = ar.alloc([8, NIN], BF16)
        xt = ar.alloc([4, D], F32)
        xn = ar.alloc([4, D], BF16)
        nT = ar.alloc([8, 512], BF16)
        junk = ar.alloc([D], F32)
        stg = [ar.alloc([4, 512], BF16) for _ in range(2)]
        vg = ar.alloc([512], F32)
        vstg = ar.alloc([4, 512], BF16)
        kvstg = ar.alloc([4, 256], BF16)
        gstg = ar.alloc([4, 24], F32)
        rowg = ar.alloc([512], F32)
        ss = ar.alloc([8], F32)
        rs = ar.alloc([8], F32)
        pb.dma('sync', vec, vecs[l], 'vec', writes=['vec'])
        pb.dma('sync', rowg, rows[l, :, 0:512], 'rowg', writes=['rowg'])
        for kc in range(8):
            pb.dma('sync', Wsb[:, kc, :], w_in_bf[l, kc * 128:(kc + 1) * 128, :], ('Wsb', kc),
                   reads=[('w_in_bf', l, kc)], writes=[('Wsb', kc)])
        Wk = [('Wsb', kc) for kc in range(8)]
        xsrc = x_in if l == 0 else xres
        psi = [0]

        def nextps():
            psi[0] = (psi[0] + 1) % 6
            return psi[0]

        groups = []
        for c4 in range(1):
            groups.append((OFF['u'], 4, AF.Gelu_apprx_tanh, uT, 0))
            groups.append((OFF['gb'], 4, AF.Gelu_apprx_tanh, gbT, 0))
            groups.append((OFF['rb'], 4, None, rbT, 0))
            groups.append((OFF['q'], 4, None, qT, 0))
            groups.append((OFF['xd'], 4, None, xdT, 0))
        for b in range(8):
            groups.append((OFF['mg'] + b * 512, 4, AF.Sigmoid, mgT, b * 512))
        kvT2 = kvT.rearrange("k p t -> (k p) t")
        kv_cols = [OFF['kcmp'], OFF['vcmp'], OFF['kslc'], OFF['kwin']]

        KA = int(os.environ.get("KA", "9"))
        for jt in range(int(os.environ.get("KJT", "8"))):
            t0 = jt * 512
            pb.dma('sync', xt, xsrc[t0:t0 + 512, :].rearrange("(s p) d -> p s d", p=128), 'xtA',
                   reads=[('xres', jt)] if l > 0 else [], writes=['xtA'])
            if KA < 2:
                continue
            for s in range(4):
                rmsnorm_rstd((xt[:, s, :], ['xtA']), junk, ss[:, s:s + 1], rs[:, s:s + 1], D, 'A%d' % s)
                pb.op('vector', lambda e, s=s: e.tensor_scalar(out=xn[:, s, :], in0=xt[:, s, :], scalar1=rs[:, s:s + 1],
                                                              scalar2=None, op0=ALU.mult),
                      reads=['xtA', 'A%drs' % s], writes=[('xn', s)])
            if KA < 3:
                continue
            for kc in range(8):
                bank = 6 + (kc % 2)
                pst = psb[bank][:].bitcast(BF16)
                for s in range(4):
                    pb.op('tensor', lambda e, s=s, kc=kc, pst=pst: e.transpose(
                        out=pst[:, s * 128:(s + 1) * 128], in_=xn[:, s, kc * 128:(kc + 1) * 128], identity=identb),
                        reads=[('xn', s), 'identb'], writes=[PS(bank)])
                pb.op('vector', lambda e, kc=kc, pst=pst: e.tensor_scalar(
                    out=nT[:, kc, :], in0=pst[:, 0:512], scalar1=vec[:, kc:kc + 1], scalar2=None, op0=ALU.mult),
                    reads=[PS(bank), 'vec'], writes=[('nT', kc)])
            nTk = [('nT', kc) for kc in range(8)]
            gi = 0

            def fm_group(col0, nch, func, dest, row0, cols_list=None):
                nonlocal gi
                slot = gi % 2
                gi += 1
                st = stg[slot]
                for c in range(nch):
                    cc0 = cols_list[c] if cols_list else col0 + c * 128
                    bank = nextps()
                    for kc in range(8):
                        pb.op('tensor', lambda e, kc=kc, cc0=cc0, bank=bank: e.matmul(
                            psb[bank][:], lhsT=Wsb[:, kc, cc0:cc0 + 128], rhs=nT[:, kc, :],
                            start=(kc == 0), stop=(kc == 7)),
                            reads=[Wk[kc], nTk[kc]], writes=[PS(bank)])
                    if func is None:
                        pb.op('vector', lambda e, c=c, bank=bank, st=st: e.tensor_copy(out=st[:, c, :], in_=psb[bank][:]),
                              reads=[PS(bank)], writes=[('stg', slot, c)])
                    else:
                        pb.op('scalar', lambda e, c=c, bank=bank, st=st, func=func: e.activation(
                            out=st[:, c, :], in_=psb[bank][:], func=func),
                            reads=[PS(bank)], writes=[('stg', slot, c)])
                pb.dma('sync', dest[row0:row0 + nch * 128, t0:t0 + 512].rearrange("(c p) t -> p c t", p=128),
                       st[:, 0:nch, :], ('stg', slot), reads=[('stg', slot, c) for c in range(nch)],
                       writes=[(dest.name, row0, jt)])

            if KA < 4:
                continue
            for (col0, nch, func, dest, row0) in (groups if KA >= 5 else groups[:1]):
                fm_group(col0, nch, func, dest, row0)
            if KA < 6:
                continue
            fm_group(0, 4, None, kvT2, 0, cols_list=kv_cols)
            if KA < 7:
                continue
            for s in range(4):
                bank = nextps()
                for kc in range(8):
                    pb.op('tensor', lambda e, kc=kc, s=s, bank=bank: e.matmul(
                        psb[bank][:], lhsT=nT[:, kc, s * 128:(s + 1) * 128], rhs=Wsb[:, kc, OFF['v']:OFF['v'] + 512],
                        start=(kc == 0), stop=(kc == 7)), reads=[Wk[kc], nTk[kc]], writes=[PS(bank)])
                pb.op('scalar', lambda e, bank=bank: e.activation(out=vg, in_=psb[bank][:], func=AF.Gelu_apprx_tanh),
                      reads=[PS(bank)], writes=['vg'])
                rmsnorm_rstd((vg, ['vg']), junk[:, 0:512], ss[:, 4:5], rs[:, 4:5], MIX, 'Av')
                pb.op('vector', lambda e: e.tensor_scalar(out=vg, in0=vg, scalar1=rs[:, 4:5], scalar2=None, op0=ALU.mult),
                      reads=['vg', 'Avrs'], writes=['vg'])
                pb.op('vector', lambda e, s=s: e.tensor_tensor(out=vstg[:, s, :], in0=vg, in1=rowg, op=ALU.mult),
                      reads=['vg', 'rowg'], writes=[('vstg', s)])
                if KA < 8:
                    continue
                bank = nextps()
                for (c0, n, o0) in ((OFF['vslc'], 128, 0), (OFF['vwin'], 128, 128), (OFF['ng'], 24, 256)):
                    for kc in range(8):
                        pb.op('tensor', lambda e, kc=kc, s=s, bank=bank, c0=c0, n=n, o0=o0: e.matmul(
                            psb[bank][:, o0:o0 + n], lhsT=nT[:, kc, s * 128:(s + 1) * 128], rhs=Wsb[:, kc, c0:c0 + n],
                            start=(kc == 0), stop=(kc == 7)), reads=[Wk[kc], nTk[kc]], writes=[PS(bank)])
                pb.op('vector', lambda e, s=s, bank=bank: e.tensor_copy(out=kvstg[:, s, :], in_=psb[bank][:, 0:256]),
                      reads=[PS(bank)], writes=[('kvstg', s)])
                pb.op('scalar', lambda e, s=s, bank=bank: e.activation(out=gstg[:, s, :], in_=psb[bank][:, 256:280],
                                                                      func=AF.Sigmoid),
                      reads=[PS(bank)], writes=[('gstg', s)])
            if KA < 9:
                continue
            pb.dma('sync', vn[t0:t0 + 512, :].rearrange("(s p) c -> p s c", p=128), vstg, 'vstg',
                   reads=[('vstg', s) for s in range(4)], writes=[('vn', jt)])
            pb.dma('sync', vtok[t0:t0 + 512, :].rearrange("(s p) c -> p s c", p=128), kvstg, 'kvstg',
                   reads=[('kvstg', s) for s in range(4)], writes=[('vtok', jt)])
            pb.dma('sync', gC[t0:t0 + 512, :].rearrange("(s p) c -> p s c", p=128), gstg, 'gstg',
                   reads=[('gstg', s) for s in range(4)], writes=[('gC', jt)])
        pb.barrier()
        ar.reset(base)


    def mm(out, lhsT, rhs, start, stop, reads, bank):
        pb.op('tensor', lambda e: e.matmul(out, lhsT=lhsT, rhs=rhs, start=start, stop=stop),
              reads=reads, writes=[PS(bank)])

    def vop(fn, reads, writes, eng='vector'):
        pb.op(eng, fn, reads=reads, writes=writes)

    def phase_BA(l):
        base = ar.mark()
        vnsb = ar.alloc([32, 512], BF16)
        usb = ar.alloc([4, S], BF16)
        ya = ar.alloc([4, S], BF16)
        wTf = ar.alloc([4, 128], F32)
        wTm = ar.alloc([4, 128], BF16)
        rowb = ar.alloc([512], F32)
        bsb = ar.alloc([512], BF16)
        ones1 = ar.alloc([128], BF16)
        pb.dma_split('sync', vnsb, vn.rearrange("(ch s) c -> s ch c", s=128), 'vnsb', 8, writes=['vnsb'])
        pb.dma('sync', usb, uT.rearrange("(g c) t -> c g t", c=128), 'usb', writes=['usb'])
        pb.dma('sync', wTf, gm_wsT[l].rearrange("g s t -> s g t"), 'wTf', writes=['wTf'])
        pb.dma('sync', rowb[0:1, :], rows[l, 0:1, 512:1024], 'rowb', writes=['rowb'])
        for g in range(4):
            vop(lambda e, g=g: e.tensor_tensor(out=wTm[:, g, :], in0=wTf[:, g, :], in1=tril4[:, 0:128], op=ALU.mult),
                ['wTf', 'tril4'], [('wTm', g)])
        vop(lambda e: e.tensor_copy(out=bsb[0:1, :], in_=rowb[0:1, :]), ['rowb'], ['bsb'])
        vop(lambda e: e.memset(ones1[0:1, :], 1.0), [], ['ones1'])
        for g in range(4):
            for ch4 in range(8):
                bank = (g * 8 + ch4) % 4
                for cq in range(4):
                    ch = ch4 * 4 + cq
                    mm(psb[bank][:, cq * 128:(cq + 1) * 128], vnsb[:, ch, g * 128:(g + 1) * 128], wTm[:, g, :],
                       True, False, ['vnsb', ('wTm', g)], bank)
                    mm(psb[bank][:, cq * 128:(cq + 1) * 128], ones1[0:1, :], bsb[0:1, g * 128:(g + 1) * 128],
                       False, True, ['ones1', 'bsb'], bank)
                vop(lambda e, g=g, ch4=ch4, bank=bank: e.tensor_tensor(
                    out=ya[:, g, ch4 * 512:(ch4 + 1) * 512], in0=psb[bank][:], in1=usb[:, g, ch4 * 512:(ch4 + 1) * 512],
                    op=ALU.mult), [PS(bank), 'usb'], [('ya', g, ch4)])
        pb.dma('sync', yT[0:512, :].rearrange("(g c) t -> c g t", c=128), ya, 'ya_out',
               reads=[('ya', g, c) for g in range(4) for c in range(8)], writes=[])
        pb.barrier()
        ar.reset(base)

    def phase_BB(l):
        base = ar.mark()
        xpad = ar.alloc([S + 8], BF16)
        xc = ar.alloc([S], F32)
        xcb = ar.alloc([S], BF16)
        rr = ar.alloc([S], F32)
        ii = ar.alloc([S], F32)
        tt = ar.alloc([S], F32)
        gb = ar.alloc([S], BF16)
        yb = ar.alloc([S], BF16)
        BDf = ar.alloc([2, 128], F32)
        BDb = ar.alloc([2, 128], BF16)
        sm = ar.alloc([8, 4], F32)
        vop(lambda e: e.memset(xpad[:, 0:8], 0.0), [], ['xpad0'])
        lam = vec[:, 44:48]
        z, az, ez, mz, negc = sm[:, 0, :], sm[:, 1, :], sm[:, 2, :], sm[:, 3, :], sm[:, 4, :]
        vop(lambda e: e.tensor_scalar(out=z, in0=lam, scalar1=-1.0, scalar2=None, op0=ALU.mult), ['vec'], ['z'])
        vop(lambda e: e.tensor_tensor(out=az, in0=z, in1=lam, op=ALU.max), ['z', 'vec'], ['az'])
        pb.op('scalar', lambda e: e.activation(out=ez, in_=az, func=AF.Exp, scale=-1.0), ['az'], ['ez'])
        vop(lambda e: e.tensor_scalar(out=ez, in0=ez, scalar1=1.0, scalar2=None, op0=ALU.add), ['ez'], ['ez'])
        pb.op('scalar', lambda e: e.activation(out=ez, in_=ez, func=AF.Ln), ['ez'], ['ez'])
        vop(lambda e: e.tensor_scalar(out=mz, in0=z, scalar1=0.0, scalar2=None, op0=ALU.max), ['z'], ['mz'])
        vop(lambda e: e.tensor_tensor(out=mz, in0=mz, in1=ez, op=ALU.add), ['mz', 'ez'], ['mz'])
        vop(lambda e: e.tensor_scalar(out=negc, in0=mz, scalar1=-8.0, scalar2=None, op0=ALU.mult), ['mz'], ['negc'])
        for cc in range(4):
            r0 = cc * 128
            pb.dma('sync', xpad[:, 8:], rbT[r0:r0 + 128, :], 'xpad', writes=['xpad'])
            pb.dma('sync', gb, gbT[r0:r0 + 128, :], 'gbsb', writes=['gbsb'])
            vop(lambda e: e.memset(BDf, 0.0), [], ['BDf'])
            pairs = []
            for h in range(2):
                p0 = h * 64
                pairs.append((BDf[p0:p0 + 64, 0, p0:p0 + 64], lru_wa[l, 2 * cc + h]))
                pairs.append((BDf[p0:p0 + 64, 1, p0:p0 + 64], lru_wx[l, 2 * cc + h]))
            pb.dma_batch('sync', pairs, 'BDf', reads=['BDf'], writes=['BDfd'])
            vop(lambda e: e.tensor_copy(out=BDb, in_=BDf), ['BDf', 'BDfd'], ['BDb'])
            vop(lambda e, cc=cc: e.tensor_scalar(out=xc, in0=xpad[:, 5:5 + S], scalar1=vec[:, 16 + cc:17 + cc],
                                                 scalar2=vec[:, 32 + cc:33 + cc], op0=ALU.mult, op1=ALU.add),
                ['xpad', 'xpad0', 'vec'], ['xc'])
            for k in range(1, 4):
                vop(lambda e, cc=cc, k=k: e.tensor_scalar(out=tt, in0=xpad[:, 5 + k:5 + k + S],
                                                          scalar1=vec[:, 16 + 4 * k + cc:17 + 4 * k + cc], scalar2=None, op0=ALU.mult),
                    ['xpad', 'xpad0', 'vec'], ['tt'], eng='gpsimd')
                vop(lambda e: e.tensor_tensor(out=xc, in0=xc, in1=tt, op=ALU.add), ['xc', 'tt'], ['xc'])
            pb.op('scalar', lambda e: e.activation(out=xcb, in_=xc, func=AF.Copy), ['xc'], ['xcb'])
            for tq in range(8):
                ts_ = slice(tq * 512, (tq + 1) * 512)
                for k, (dst, bcol) in enumerate(((rr, 36 + cc), (ii, 40 + cc))):
                    bank = (tq * 2 + k) % 6
                    mm(psb[bank][:], BDb[:, k, :], xcb[:, ts_], True, True, ['BDb', 'xcb'], bank)
                    pb.op('scalar', lambda e, dst=dst, bcol=bcol, bank=bank, ts_=ts_: e.activation(
                        out=dst[:, ts_], in_=psb[bank][:], func=AF.Sigmoid, bias=vec[:, bcol:bcol + 1]),
                        [PS(bank), 'vec'], [('ri', k, tq)])
            allri = [('ri', k, tq) for k in range(2) for tq in range(8)]
            pb.op('scalar', lambda e, cc=cc: e.activation(out=rr, in_=rr, func=AF.Exp, scale=negc[:, cc:cc + 1]),
                  allri + ['negc'], ['rr'])
            vop(lambda e: e.tensor_tensor(out=tt, in0=rr, in1=rr, op=ALU.mult), ['rr'], ['tt'])
            vop(lambda e: e.tensor_scalar(out=tt, in0=tt, scalar1=-1.0, scalar2=1.0, op0=ALU.mult, op1=ALU.add), ['tt'], ['tt'])
            pb.op('scalar', lambda e: e.activation(out=tt, in_=tt, func=AF.Sqrt), ['tt'], ['tt'])
            vop(lambda e: e.tensor_tensor(out=ii, in0=ii, in1=xc, op=ALU.mult), allri + ['xc'], ['ii'])
            vop(lambda e: e.tensor_tensor(out=ii, in0=ii, in1=tt, op=ALU.mult), ['ii', 'tt'], ['ii'])
            vop(lambda e: e.tensor_tensor_scan(out=tt, data0=rr, data1=ii, initial=0.0, op0=ALU.mult, op1=ALU.add),
                ['rr', 'ii', 'tt'], ['tt'])
            vop(lambda e: e.tensor_tensor(out=yb, in0=tt, in1=gb, op=ALU.mult), ['tt', 'gbsb'], ['yb'])
            pb.dma('sync', yT[512 + r0:512 + r0 + 128, :], yb, 'yb_out', reads=['yb'], writes=[])
        pb.barrier()
        ar.reset(base)

    def phase_BD(l):
        base = ar.mark()
        xp = ar.alloc([S + 16], BF16)
        A = ar.alloc([S + 16], F32)
        B = ar.alloc([S + 16], F32)
        plb = ar.alloc([S], BF16)
        yd = ar.alloc([S], BF16)
        pw = ar.alloc([4, 128], BF16)
        pcor = ar.alloc([4, 16], F32)
        pb.dma('gpsimd', pw, pool_w[l].rearrange("g i j -> i g j"), 'pw', writes=['pw'])
        pb.dma('sync', pcor, c_pcorr[:, :, :], 'pcor', writes=['pcor'])
        vop(lambda e: e.memset(A[:, 0:16], 0.0), [], ['A0'])
        vop(lambda e: e.memset(B[:, 0:16], 0.0), [], ['B0'])
        for gi in range(4):
            r0 = gi * 128
            w = 2 ** (gi + 1)
            pb.dma('sync', xp[:, 16:], xdT[r0:r0 + 128, :], 'xp', writes=['xp'])
            vop(lambda e: e.tensor_copy(out=A[:, 16:], in_=xp[:, 16:]), ['xp'], ['A'])
            cur, nxt, ck, nk = A, B, 'A', 'B'
            for k in range(gi + 1):
                sh = 2 ** k
                vop(lambda e, cur=cur, nxt=nxt, sh=sh: e.tensor_tensor(
                    out=nxt[:, 16:], in0=cur[:, 16:], in1=cur[:, 16 - sh:16 - sh + S], op=ALU.add),
                    [ck, 'A0', 'B0'], [nk])
                cur, nxt, ck, nk = nxt, cur, nk, ck
            vop(lambda e, cur=cur, nxt=nxt, w=w: e.tensor_scalar(out=nxt[:, 16:], in0=cur[:, 16:], scalar1=1.0 / w,
                                                               scalar2=None, op0=ALU.mult), [ck], [nk])
            vop(lambda e, cur=cur, nxt=nxt, gi=gi: e.tensor_tensor(out=nxt[:, 16:32], in0=cur[:, 16:32], in1=pcor[:, gi, :],
                                                                 op=ALU.mult), [ck, nk, 'pcor'], [nk])
            vop(lambda e, nxt=nxt: e.tensor_tensor(out=plb, in0=nxt[:, 16:], in1=xp[:, 16:], op=ALU.subtract),
                [nk, 'xp'], ['plb'])
            vop(lambda e, nxt=nxt: e.memset(nxt[:, 0:16], 0.0), [nk], [nk, 'A0', 'B0'])
            for tq in range(8):
                ts_ = slice(tq * 512, (tq + 1) * 512)
                bank = tq % 6
                mm(psb[bank][:], pw[:, gi, :], plb[:, ts_], True, True, ['pw', 'plb'], bank)
                vop(lambda e, bank=bank, ts_=ts_, gi=gi: e.tensor_scalar(out=yd[:, ts_], in0=psb[bank][:],
                                                                        scalar1=vec[:, 48 + gi:49 + gi], scalar2=None, op0=ALU.mult),
                    [PS(bank), 'vec'], [('yd', tq)])
            pb.dma('sync', yT[1536 + r0:1536 + r0 + 128, :], yd, 'yd_out', reads=[('yd', tq) for tq in range(8)], writes=[])
        pb.barrier()
        ar.reset(base)


    def phase_BC(l):
        base = ar.mark()
        NQ = int(os.environ.get("KNQ", str(NQT)))
        pe_sb = ar.alloc([2, 32], BF16)
        gates = ar.alloc([NQT, 24], F32)
        XT = ar.alloc([S], BF16)
        w1sb = ar.alloc([32, 64], BF16)
        w2sb = ar.alloc([2, 64], BF16)
        hidT = ar.alloc([256], BF16)
        KcT = ar.alloc([256], BF16)
        Vca_full = ar.alloc([2, 130], BF16)
        Vca = Vca_full[:, :, 0:129]
        biasb = ar.alloc([8], F32)
        Qa = ar.alloc([4, S], BF16)
        Ka = ar.alloc([S], BF16)
        Kw = ar.alloc([S], BF16)
        Vs_full = ar.alloc([NQT, 66], BF16)
        Vw_full = ar.alloc([NQT, 66], BF16)
        Vs = Vs_full[:, :, 0:65]
        Vw = Vw_full[:, :, 0:65]
        ycT = ar.alloc([2, S], BF16)
        cmk = [ar.alloc([2, 512], BF16) for _ in range(2)]
        sCB = [ar.alloc([2, 64], F32) for _ in range(2)]
        pbuf = [ar.alloc([512], BF16) for _ in range(3)]
        sm = ar.alloc([16, 4], F32)
        imp = ar.alloc([64], F32)
        tmp64 = ar.alloc([64], F32)
        m8 = ar.alloc([8], F32)
        Mpad = ar.alloc([128], BF16)
        oacc = ar.alloc([256], F32)
        otmp = ar.alloc([256], F32)
        ycb = ar.alloc([256], BF16)
        pb.dma('gpsimd', pe_sb[0:64], cmp_peT[l].rearrange("c d l -> d c l"), 'pe_sb', writes=['pe_sb'])
        pb.dma_split('sync', gates, gC.rearrange("(qt q) c -> q qt c", q=128), 'gates', 8, writes=['gates'])
        vop(lambda e: e.memset(Mpad, 0.0), [], ['Mpad'])
        vop(lambda e: e.memset(hidT, 0.0), [], ['hidT'])
        XT16 = XT.rearrange("p (n s) -> p n s", s=16)
        for g in range(2):
            pb.dma('gpsimd', w2sb[0:64], cmp_w2[l, :, g].rearrange("c e d -> e c d"), 'w2sb', writes=['w2sb'])
            pb.dma('sync', Vca[:, :, 64:129], c_ovc[:, :, :], 'Vca_c', writes=['Vca_c', ('Vca', 0), ('Vca', 1)])
            for c in range(2):
                pb.dma('sync', XT[0:64, :], kvT[c, g * 64:(g + 1) * 64, :], 'XT', writes=['XT'])
                pb.dma_split('sync', w1sb[0:64], cmp_w1_bf[l, c, g].rearrange("(l d) e -> d l e", d=64), 'w1sb', 4,
                             reads=wkeys[('w1', l)], writes=['w1sb'])
                for li in range(32):
                    mm(psb[7][0:64, 0:1], w1sb[0:64, li, :], pe_sb[0:64, c, li:li + 1], li == 0, li == 31, ['w1sb', 'pe_sb'], 7)
                vop(lambda e: e.tensor_copy(out=biasb[0:64, 0:1], in_=psb[7][0:64, 0:1]), [PS(7)], ['biasb'])
                for li in range(32):
                    rhs = XT16[0:64, 0:255, li] if li < 16 else XT16[0:64, 1:256, li - 16]
                    mm(psb[6][0:64, 0:255], w1sb[0:64, li, :], rhs, li == 0, li == 31, ['w1sb', 'XT'], 6)
                pb.op('scalar', lambda e: e.activation(out=hidT[0:64, 0:255], in_=psb[6][0:64, 0:255],
                                                       func=AF.Gelu_apprx_tanh, bias=biasb[0:64, 0:1]),
                      [PS(6), 'biasb'], ['hidT'])
                if c == 0:
                    mm(psb[5][0:64, 0:256], w2sb[0:64, 0, :], hidT[0:64, 0:256], True, True, ['w2sb', 'hidT'], 5)
                    vop(lambda e: e.tensor_copy(out=KcT[0:64, :], in_=psb[5][0:64, 0:256]), [PS(5)], ['KcT'])
                else:
                    for kt in range(2):
                        mm(psb[5][:, kt * 64:(kt + 1) * 64], hidT[0:64, kt * 128:(kt + 1) * 128], w2sb[0:64, 1, :],
                           True, True, ['w2sb', 'hidT'], 5)
                    for kt in range(2):
                        vop(lambda e, kt=kt: e.tensor_copy(out=Vca[:, kt, 0:64], in_=psb[5][:, kt * 64:(kt + 1) * 64]),
                            [PS(5), 'Vca_c'], [('Vca', kt)])
            pb.dma('sync', Qa[0:64], qT[g * 256:(g + 1) * 256, :].rearrange("(j d) t -> d j t", d=64), 'Qa', writes=['Qa'])
            pb.dma_batch('sync', [(Ka[0:64, :], kvT[2, g * 64:(g + 1) * 64, :]), (Ka[64:128, :], c_eblk[:, :])], 'Ka', writes=['Ka'])
            pb.dma('sync', Kw[0:64, :], kvT[3, g * 64:(g + 1) * 64, :], 'Kw', writes=['Kw'])
            pb.dma_split('sync', Vs[:, :, 0:64], vtok[:, g * 64:(g + 1) * 64].rearrange("(kt k) d -> k kt d", k=128), 'Vs', 8, writes=['Vs', 'Vs1'])
            pb.dma_split('sync', Vw[:, :, 0:64], vtok[:, 128 + g * 64:128 + (g + 1) * 64].rearrange("(kt k) d -> k kt d", k=128),
                         'Vw', 8, writes=['Vw', 'Vw1'])
            vop(lambda e: e.memset(Vs_full[:, :, 64:66], 1.0), ['Vs'], ['Vs1'])
            vop(lambda e: e.memset(Vw_full[:, :, 64:66], 1.0), ['Vw'], ['Vw1'])
            if debug and g == 0 and l == 0 and os.environ.get("KDBGBC") == "1":
                for nm, t_, shp, rk in (('dbgKa', Ka, [128, S], ['Ka']), ('dbgKw', Kw, [128, S], ['Kw']),
                                        ('dbgVs', Vs_full, [128, NQT, 66], ['Vs', 'Vs1']), ('dbgVw', Vw_full, [128, NQT, 66], ['Vw', 'Vw1']),
                                        ('dbgKcT', KcT, [128, 256], ['KcT']), ('dbgVca', Vca_full, [128, 2, 130], [('Vca', 0), ('Vca', 1), 'Vca_c'])):
                    dd = dscr(nm, shp, BF16)
                    pb.dma('sync', dd, t_, nm, reads=rk, writes=[])
                dd = dscr('dbgQa', [64, 4, S], BF16)
                pb.dma('sync', dd, Qa[0:64], 'dbgQa', reads=['Qa'], writes=[])
            sci = [0]

            def nextsc():
                sci[0] = (sci[0] + 1) % 3
                return sci[0]
            pbi = [0]

            def nextpb():
                pbi[0] = (pbi[0] + 1) % 3
                return pbi[0]

            def poc(j):
                return psb[3 + j // 2][:, (j % 2) * 129:(j % 2) * 129 + 129]

            for i in range(NQ):
                qs = slice(i * 128, (i + 1) * 128)
                sl = i % 2
                pb.dma('sync', cmk[sl], c_cmpmask[i], ('cmk', sl), writes=[('cmk', sl)])
                pb.dma_batch('sync', [(sCB[sl][:, 0, :], c_selC[qs, :]), (sCB[sl][:, 1, :], c_selB[qs, :])], ('sCB', sl),
                             writes=[('sCB', sl)])
                gt = gates[:, i, g * 12:(g + 1) * 12].rearrange("p (j b) -> p j b", b=3)
                nkt = 2 if i >= 16 else 1
                for kt in range(nkt):
                    b = nextsc()
                    mm(psb[b][:], KcT[0:64, kt * 128:(kt + 1) * 128], Qa[0:64, :, qs], True, True, ['KcT', 'Qa'], b)
                    pi = nextpb()
                    P = pbuf[pi]
                    pb.op('scalar', lambda e, P=P, b=b: e.activation(out=P, in_=psb[b][:], func=AF.Exp, scale=0.125),
                          [PS(b)], [('P', pi)])
                    vop(lambda e, P=P, kt=kt, sl=sl: e.tensor_tensor(out=P, in0=P, in1=cmk[sl][:, kt, :], op=ALU.mult),
                        [('P', pi), ('cmk', sl)], [('P', pi)])
                    for j in range(4):
                        mm(poc(j), P[:, j * 128:(j + 1) * 128], Vca[:, kt, :], kt == 0 and j % 2 == 0,
                           kt == nkt - 1 and j % 2 == 1, [('P', pi), ('Vca', kt), 'Vca_c'], 3 + j // 2)
                Zc, rZc, fC, rZs, fS = sm[:, 0, :], sm[:, 1, :], sm[:, 2, :], sm[:, 3, :], sm[:, 4, :]
                thr = sm[:, 5, 0:1]
                for h2 in range(2):
                    zv = psb[3 + h2][:, 0:258].rearrange("p (j c) -> p j c", c=129)[:, :, 128]
                    vop(lambda e, h2=h2, zv=zv: e.tensor_scalar(out=Zc[:, 2 * h2:2 * h2 + 2], in0=zv, scalar1=1e-30, scalar2=None,
                                                               op0=ALU.max), [PS(3 + h2)], [('Zc', h2)])
                vop(lambda e: e.reciprocal(out=rZc, in_=Zc), [('Zc', 0), ('Zc', 1)], ['rZc'])
                for j in range(4):
                    dst = imp if j == 0 else tmp64
                    vop(lambda e, j=j, dst=dst: e.tensor_scalar(out=dst, in0=poc(j)[:, 64:128], scalar1=rZc[:, j:j + 1],
                                                                scalar2=None, op0=ALU.mult),
                        [PS(3 + j // 2), 'rZc'], ['imp' if j == 0 else 'tmp64'])
                    if j > 0:
                        vop(lambda e: e.tensor_tensor(out=imp, in0=imp, in1=tmp64, op=ALU.add), ['imp', 'tmp64'], ['imp'])
                vop(lambda e, sl=sl: e.tensor_tensor(out=imp, in0=imp, in1=sCB[sl][:, 0, :], op=ALU.mult), ['imp', ('sCB', sl)], ['imp'])
                vop(lambda e, sl=sl: e.tensor_tensor(out=imp, in0=imp, in1=sCB[sl][:, 1, :], op=ALU.add), ['imp', ('sCB', sl)], ['imp'])
                vop(lambda e: e.max(out=m8, in_=imp), ['imp'], ['m8'])
                vop(lambda e: e.tensor_scalar(out=thr, in0=m8[:, 7:8], scalar1=0.0, scalar2=None, op0=ALU.max), ['m8'], ['thr'])
                vop(lambda e: e.tensor_scalar(out=Mpad[:, 64:128], in0=imp, scalar1=thr, scalar2=-BIG, op0=ALU.is_lt, op1=ALU.mult),
                    ['imp', 'thr'], ['Mpad'])
                pst = psb[7][:].bitcast(BF16)
                pb.op('tensor', lambda e, pst=pst: e.transpose(out=pst[:, 0:128], in_=Mpad, identity=identb),
                      ['Mpad', 'identb'], [PS(7)])
                for j in range(4):
                    if j % 2 == 0:
                        pb.op('scalar', lambda e, j=j, pst=pst, qs=qs: e.activation(out=Qa[64:128, j, qs], in_=pst[64:128, 0:128], func=AF.Copy),
                              [PS(7)], ['Qa'])
                    else:
                        vop(lambda e, j=j, pst=pst, qs=qs: e.tensor_copy(out=Qa[64:128, j, qs], in_=pst[64:128, 0:128]), [PS(7)], ['Qa'])
                vop(lambda e, gt=gt: e.tensor_tensor(out=fC, in0=rZc, in1=gt[:, :, 0], op=ALU.mult), ['rZc', 'gates'], ['fC'])
                for j in range(4):
                    pb.op('scalar', lambda e, j=j: e.activation(out=oacc[:, j * 64:(j + 1) * 64], in_=poc(j)[:, 0:64], func=AF.Copy,
                                                                scale=fC[:, j:j + 1]), [PS(3 + j // 2), 'fC'], [('oacc', j)])
                for br, (bank, Kt, kparts, Vt, vkey, kt0) in enumerate(((5, Ka, 128, Vs, 'Vs', 0), (6, Kw, 64, Vw, 'Vw', max(0, i - 4)))):
                    kts = list(range(kt0, i + 1))
                    for kt in kts:
                        b = nextsc()
                        mm(psb[b][:], Kt[0:kparts, kt * 128:(kt + 1) * 128], Qa[0:kparts, :, qs], True, True,
                           ['Ka' if br == 0 else 'Kw', 'Qa'], b)
                        pi = nextpb()
                        P = pbuf[pi]
                        pb.op('scalar', lambda e, P=P, b=b: e.activation(out=P, in_=psb[b][:], func=AF.Exp, scale=0.125),
                              [PS(b)], [('P', pi)])
                        msk = None
                        if kt == i:
                            msk, mk = tril4, 'tril4'
                        elif br == 1 and kt == i - 4:
                            msk, mk = triu4, 'triu4'
                        if msk is not None:
                            vop(lambda e, P=P, msk=msk: e.tensor_tensor(out=P, in0=P, in1=msk, op=ALU.mult), [('P', pi), mk], [('P', pi)])
                        for j in range(4):
                            mm(psb[bank][:, j * 65:(j + 1) * 65], P[:, j * 128:(j + 1) * 128], Vt[:, kt, :],
                               kt == kts[0] and j == 0, kt == i and j == 3, [('P', pi), vkey, vkey + '1'], bank)
                    zv = psb[bank][:, 0:260].rearrange("p (j c) -> p j c", c=65)[:, :, 64]
                    vop(lambda e, zv=zv: e.reciprocal(out=rZs, in_=zv), [PS(bank)], ['rZs'])
                    vop(lambda e, gt=gt, br=br: e.tensor_tensor(out=fS, in0=rZs, in1=gt[:, :, 1 + br], op=ALU.mult), ['rZs', 'gates'], ['fS'])
                    for j in range(4):
                        pb.op('scalar', lambda e, j=j, bank=bank: e.activation(
                            out=otmp[:, j * 64:(j + 1) * 64], in_=psb[bank][:, j * 65:j * 65 + 64], func=AF.Copy, scale=fS[:, j:j + 1]),
                            [PS(bank), 'fS'], [('otmp', j)])
                    dst, dk = (oacc, 'oaccs') if br == 0 else (ycb, 'ycb')
                    vop(lambda e, dst=dst: e.tensor_tensor(out=dst, in0=oacc, in1=otmp, op=ALU.add),
                        [('oacc', j) for j in range(4)] + [('otmp', j) for j in range(4)] + ['oaccs'],
                        [dk] + ([('oacc', j) for j in range(4)] if br == 0 else []))
                for c2 in range(2):
                    pb.op('tensor', lambda e, c2=c2, pst=pst: e.transpose(out=pst[:, 256 + c2 * 128:256 + (c2 + 1) * 128],
                                                                          in_=ycb[:, c2 * 128:(c2 + 1) * 128], identity=identb),
                          ['ycb', 'identb'], [PS(7)])
                vop(lambda e, pst=pst, qs=qs: e.tensor_copy(out=ycT[:, :, qs], in_=pst[:, 256:512].rearrange("p (c q) -> p c q", q=128)),
                    [PS(7)], ['ycT'])
            pb.dma('sync', yT[1024 + g * 256:1024 + (g + 1) * 256, :].rearrange("(c p) t -> p c t", p=128), ycT, 'yc_out',
                   reads=['ycT'], writes=[])
        pb.barrier()
        ar.reset(base)


    def phase_C(l):
        base = ar.mark()
        NJ = int(os.environ.get("KJC", "8"))
        Wb = ar.alloc([16, D], BF16)
        Wo = ar.alloc([8, D], BF16)
        ysb2 = [ar.alloc([16, 512], BF16) for _ in range(2)]
        gsb2 = [ar.alloc([32, 512], BF16) for _ in range(2)]
        xt2 = [ar.alloc([4, D], F32) for _ in range(2)]
        mT = ar.alloc([8, 512], BF16)
        macc = ar.alloc([512], F32)
        mtmp = ar.alloc([512], F32)
        pb.dma_split('sync', Wb, w_branch_bf[l].rearrange("(k p) d -> p k d", p=128), 'Wb', 4, reads=wkeys[('wb', l)], writes=['Wb'])
        pb.dma_split('sync', Wo, w_out_bf[l].rearrange("(k p) d -> p k d", p=128), 'Wo', 2, reads=wkeys[('wo', l)], writes=['Wo'])
        xsrc = x_in if l == 0 else xres

        def loads(jt):
            t0 = jt * 512
            sl = jt % 2
            pb.dma_split('sync', ysb2[sl], yT[:, t0:t0 + 512].rearrange("(k p) t -> p k t", p=128), ('ysb', sl), 4, writes=[('ysb', sl)])
            pb.dma_split('sync', gsb2[sl], mgT[:, t0:t0 + 512].rearrange("(k p) t -> p k t", p=128), ('gsb', sl), 8, writes=[('gsb', sl)])
            pb.dma('sync', xt2[sl], xsrc[t0:t0 + 512, :].rearrange("(s p) d -> p s d", p=128), ('xtC', sl), writes=[('xtC', sl)])

        loads(0)
        for jt in range(NJ):
            t0 = jt * 512
            sl = jt % 2
            ysb, gsb, xt = ysb2[sl], gsb2[sl], xt2[sl]
            if jt + 1 < NJ:
                loads(jt + 1)
            for dmc in range(8):
                for b in range(4):
                    bank = b
                    for cc in range(4):
                        mm(psb[bank][:], Wb[:, b * 4 + cc, dmc * 128:(dmc + 1) * 128], ysb[:, b * 4 + cc, :], cc == 0, cc == 3,
                           ['Wb', ('ysb', sl)], bank)
                    dst, dk = (macc, 'macc') if b == 0 else (mtmp, 'mtmp')
                    vop(lambda e, dst=dst, bank=bank, b=b, dmc=dmc, gsb=gsb: e.tensor_tensor(out=dst, in0=psb[bank][:], in1=gsb[:, b * 8 + dmc, :],
                                                                                             op=ALU.mult), [PS(bank), ('gsb', sl)], [dk])
                    if b > 0:
                        o = mT[:, dmc, :] if b == 3 else macc
                        vop(lambda e, o=o: e.tensor_tensor(out=o, in0=macc, in1=mtmp, op=ALU.add), ['macc', 'mtmp'],
                            [('mT', dmc)] if b == 3 else ['macc'], eng='gpsimd')
            for s_ in range(4):
                for hf in range(2):
                    bank = 4 + (s_ * 2 + hf) % 4
                    for dmc in range(8):
                        mm(psb[bank][:], mT[:, dmc, s_ * 128:(s_ + 1) * 128], Wo[:, dmc, hf * 512:(hf + 1) * 512], dmc == 0, dmc == 7,
                           [('mT', dmc), 'Wo'], bank)
                    vop(lambda e, s_=s_, hf=hf, bank=bank, xt=xt: e.tensor_tensor(out=xt[:, s_, hf * 512:(hf + 1) * 512], in0=psb[bank][:],
                                                                                 in1=xt[:, s_, hf * 512:(hf + 1) * 512], op=ALU.add),
                        [PS(bank), ('xtC', sl)], [('xtCo', sl, s_, hf)])
            pb.dma('sync', xres[t0:t0 + 512, :].rearrange("(s p) d -> p s d", p=128), xt, ('xtC_out', sl),
                   reads=[('xtCo', sl, s_, hf) for s_ in range(4) for hf in range(2)], writes=[('xtC', sl)])
        pb.barrier()
        ar.reset(base)

    def phase_D(l, last):
        base = ar.mark()
        NJ = int(os.environ.get("KJD", "8"))
        NE = int(os.environ.get("KNE", "16"))
        xt2 = [ar.alloc([4, D], F32) for _ in range(2)]
        n2b2 = [ar.alloc([8, 512], BF16) for _ in range(2)]
        comb2 = [ar.alloc([4, 16], F32) for _ in range(2)]
        xnf = ar.alloc([4, D], F32)
        xhi = ar.alloc([4, D], BF16)
        xlo = ar.alloc([4, D], BF16)
        hiT = ar.alloc([8, 512], BF16)
        loT = ar.alloc([8, 512], BF16)
        Wrh = ar.alloc([8, 20], BF16)
        Wrl = ar.alloc([8, 20], BF16)
        Wrt = ar.alloc([8, 20], F32)
        junk = ar.alloc([D], F32)
        Wr = ar.alloc([8, 20], F32)
        rbias = ar.alloc([20], F32)
        fng = ar.alloc([D], F32)
        Wg = [ar.alloc([8, 512], BF16) for _ in range(2)]
        Wu = [ar.alloc([8, 512], BF16) for _ in range(2)]
        Wd = [ar.alloc([4, D], BF16) for _ in range(2)]
        hid = [ar.alloc([4, 512], BF16) for _ in range(2)]
        sg = [ar.alloc([512], F32) for _ in range(2)]
        dtmp = [ar.alloc([512], F32) for _ in range(2)]
        ss = ar.alloc([8], F32)
        rs = ar.alloc([8], F32)
        lg = ar.alloc([4, 20], F32)
        sm = ar.alloc([16, 8], F32)
        m16 = ar.alloc([16], F32)
        e16 = ar.alloc([16], F32)
        mx8 = ar.alloc([8], F32)
        pb.dma_split('sync', Wr, router_w[l].rearrange("(k p) c -> p k c", p=128), 'Wr', 2, writes=['Wr'])
        for kc in range(8):
            vop(lambda e, kc=kc: e.tensor_scalar(out=Wr[:, kc, :], in0=Wr[:, kc, :], scalar1=vec[:, 8 + kc:9 + kc], scalar2=None,
                                                 op0=ALU.mult), ['Wr', 'vec'], ['Wr'])
        vop(lambda e: e.tensor_copy(out=Wrh, in_=Wr), ['Wr'], ['Wrh'])
        vop(lambda e: e.tensor_tensor(out=Wrt, in0=Wr, in1=Wrh, op=ALU.subtract), ['Wr', 'Wrh'], ['Wrt'])
        vop(lambda e: e.tensor_copy(out=Wrl, in_=Wrt), ['Wrt'], ['Wrl'])
        pb.dma('sync', rbias, rows[l, :, 1024:1044], 'rbias', writes=['rbias'])
        pb.dma('sync', fng, rows[l, :, 1044:2068], 'fng', writes=['fng'])

        def front_load(jt):
            sl = jt % 2
            t0 = jt * 512
            pb.dma('sync', xt2[sl], xres[t0:t0 + 512, :].rearrange("(s p) d -> p s d", p=128), ('xtD', sl), writes=[('xtD', sl)])

        def front_compute(jt):
            sl = jt % 2
            xt, n2b, comb = xt2[sl], n2b2[sl], comb2[sl]
            xk = ('xtD', sl)
            for s_ in range(4):
                rmsnorm_rstd((xt[:, s_, :], [xk]), junk, ss[:, s_:s_ + 1], rs[:, s_:s_ + 1], D, 'D%d' % s_)
                vop(lambda e, s_=s_, xt=xt: e.tensor_scalar(out=xnf[:, s_, :], in0=xt[:, s_, :], scalar1=rs[:, s_:s_ + 1], scalar2=None,
                                                            op0=ALU.mult), [xk, 'D%drs' % s_], [('xnf', s_)])
            for s_ in range(4):
                vop(lambda e, s_=s_: e.tensor_copy(out=xhi[:, s_, :], in_=xnf[:, s_, :]), [('xnf', s_)], [('xhi', s_)])
                vop(lambda e, s_=s_: e.tensor_tensor(out=xlo[:, s_, :], in0=xnf[:, s_, :], in1=xhi[:, s_, :], op=ALU.subtract),
                    [('xnf', s_), ('xhi', s_)], [('xlo', s_)])
            for kc in range(8):
                for hl, (src, sk) in enumerate(((xhi, 'xhi'), (xlo, 'xlo'))):
                    bank = 6 + hl
                    pst = psb[bank][:].bitcast(BF16)
                    for s_ in range(4):
                        pb.op('tensor', lambda e, s_=s_, kc=kc, pst=pst, src=src: e.transpose(
                            out=pst[:, s_ * 128:(s_ + 1) * 128], in_=src[:, s_, kc * 128:(kc + 1) * 128], identity=identb),
                            [(sk, s_), 'identb'], [PS(bank)])
                    if hl == 0:
                        vop(lambda e, kc=kc, pst=pst, n2b=n2b: e.tensor_scalar(out=n2b[:, kc, :], in0=pst[:, 0:512], scalar1=vec[:, 8 + kc:9 + kc],
                                                                              scalar2=None, op0=ALU.mult), [PS(bank), 'vec'], [('n2b', sl, kc)])
                        vop(lambda e, kc=kc, pst=pst: e.tensor_scalar(out=hiT[:, kc, :], in0=pst[:, 0:512], scalar1=1.0,
                                                                     scalar2=None, op0=ALU.mult), [PS(bank)], [('hiT', kc)])
                    else:
                        vop(lambda e, kc=kc, pst=pst: e.tensor_scalar(out=loT[:, kc, :], in0=pst[:, 0:512], scalar1=1.0,
                                                                     scalar2=None, op0=ALU.mult), [PS(bank)], [('loT', kc)])
            for s_ in range(4):
                bank = 5
                passes = [(hiT, 'hiT', Wrh, 'Wrh'), (loT, 'loT', Wrh, 'Wrh'), (hiT, 'hiT', Wrl, 'Wrl')]
                for pi_, (xa, xk_, wa, wk) in enumerate(passes):
                    for kc in range(8):
                        mm(psb[bank][:, 0:20], xa[:, kc, s_ * 128:(s_ + 1) * 128], wa[:, kc, :], pi_ == 0 and kc == 0,
                           pi_ == 2 and kc == 7, [(xk_, kc), wk], bank)
                L = lg[:, s_, :]
                gmax, ngm, gsum, m1n, e2, coef = (sm[:, i_, s_:s_ + 1] for i_ in range(6))
                ohg = sm[:, 6 + s_ // 2, (s_ % 2) * 4:(s_ % 2) * 4 + 4]
                eg = sm[:, 8 + s_ // 2, (s_ % 2) * 4:(s_ % 2) * 4 + 4]
                vop(lambda e, L=L: e.tensor_tensor(out=L, in0=psb[5][:, 0:20], in1=rbias, op=ALU.add), [PS(5), 'rbias'], ['L'])
                vop(lambda e, L=L, gmax=gmax: e.reduce_max(out=gmax, in_=L[:, 0:4], axis=AX.X), ['L'], ['gmax'])
                vop(lambda e, gmax=gmax, ngm=ngm: e.tensor_scalar(out=ngm, in0=gmax, scalar1=-1.0, scalar2=None, op0=ALU.mult), ['gmax'], ['ngm'])
                pb.op('scalar', lambda e, L=L, eg=eg, ngm=ngm: e.activation(out=eg, in_=L[:, 0:4], func=AF.Exp, bias=ngm), ['L', 'ngm'], ['eg'])
                vop(lambda e, eg=eg, gsum=gsum: e.reduce_sum(out=gsum, in_=eg, axis=AX.X), ['eg'], ['gsum'])
                vop(lambda e, L=L, ohg=ohg, gmax=gmax: e.tensor_scalar(out=ohg, in0=L[:, 0:4], scalar1=gmax, scalar2=None, op0=ALU.is_ge),
                    ['L', 'gmax'], ['ohg'])
                vop(lambda e, ohg=ohg: e.tensor_scalar(out=ohg, in0=ohg, scalar1=1.0, scalar2=1e9, op0=ALU.subtract, op1=ALU.mult), ['ohg'], ['ohg'])
                for g_ in range(4):
                    vop(lambda e, g_=g_, L=L, ohg=ohg: e.tensor_scalar(out=m16[:, g_ * 4:(g_ + 1) * 4], in0=L[:, 4 + g_ * 4:8 + g_ * 4],
                                                                     scalar1=ohg[:, g_:g_ + 1], scalar2=None, op0=ALU.add), ['L', 'ohg'], ['m16'])
                vop(lambda e: e.max(out=mx8, in_=m16), ['m16'], ['mx8'])
                vop(lambda e, m1n=m1n: e.tensor_scalar(out=m1n, in0=mx8[:, 0:1], scalar1=-1.0, scalar2=None, op0=ALU.mult), ['mx8'], ['m1n'])
                pb.op('scalar', lambda e, m1n=m1n: e.activation(out=e16, in_=m16, func=AF.Exp, bias=m1n), ['m16', 'm1n'], ['e16'])
                pb.op('scalar', lambda e, m1n=m1n, e2=e2: e.activation(out=e2, in_=mx8[:, 1:2], func=AF.Exp, bias=m1n), ['mx8', 'm1n'], ['e2'])
                vop(lambda e, e2=e2: e.tensor_scalar(out=e2, in0=e2, scalar1=1.0, scalar2=None, op0=ALU.add), ['e2'], ['e2'])
                vop(lambda e, e2=e2, gsum=gsum, coef=coef: e.tensor_tensor(out=coef, in0=e2, in1=gsum, op=ALU.mult), ['e2', 'gsum'], ['coef'])
                vop(lambda e, coef=coef: e.reciprocal(out=coef, in_=coef), ['coef'], ['coef'])
                vop(lambda e: e.tensor_scalar(out=m16, in0=m16, scalar1=mx8[:, 1:2], scalar2=None, op0=ALU.is_ge), ['m16', 'mx8'], ['m16'])
                vop(lambda e: e.tensor_tensor(out=e16, in0=e16, in1=m16, op=ALU.mult), ['e16', 'm16'], ['e16'])
                vop(lambda e, s_=s_, coef=coef, comb=comb: e.tensor_scalar(out=comb[:, s_, :], in0=e16, scalar1=coef, scalar2=None, op0=ALU.mult),
                    ['e16', 'coef'], [('comb', sl, s_)])

        def experts(jt):
            sl_t = jt % 2
            xt, n2b, comb = xt2[sl_t], n2b2[sl_t], comb2[sl_t]
            xk = ('xtD', sl_t)

            def stage_gu(ex):
                sl = ex % 2
                hd = hid[sl]
                pb.dma_split('sync', Wg[sl], moe_g_bf[l, ex].rearrange("(k p) f -> p k f", p=128), ('Wg', sl), 2, reads=wkeys[('moe', l, ex)], writes=[('Wg', sl)])
                pb.dma_split('sync', Wu[sl], moe_u_bf[l, ex].rearrange("(k p) f -> p k f", p=128), ('Wu', sl), 2, reads=wkeys[('moe', l, ex)], writes=[('Wu', sl)])
                pb.dma('sync', Wd[sl], moe_d_bf[l, ex].rearrange("(k p) d -> p k d", p=128), ('Wd', sl), reads=wkeys[('moe', l, ex)], writes=[('Wd', sl)])
                for fc in range(4):
                    bg, bu = (fc % 2) * 2, (fc % 2) * 2 + 1
                    for kc in range(8):
                        mm(psb[bg][:], Wg[sl][:, kc, fc * 128:(fc + 1) * 128], n2b[:, kc, :], kc == 0, kc == 7, [('Wg', sl), ('n2b', sl_t, kc)], bg)
                    for kc in range(8):
                        mm(psb[bu][:], Wu[sl][:, kc, fc * 128:(fc + 1) * 128], n2b[:, kc, :], kc == 0, kc == 7, [('Wu', sl), ('n2b', sl_t, kc)], bu)
                    sgt = sg[fc % 2]
                    pb.op('scalar', lambda e, sgt=sgt, bg=bg: e.activation(out=sgt, in_=psb[bg][:], func=AF.Silu), [PS(bg)], [('sg', fc % 2)])
                    vop(lambda e, sgt=sgt, bu=bu, fc=fc, hd=hd: e.tensor_tensor(out=hd[:, fc, :], in0=psb[bu][:], in1=sgt, op=ALU.mult),
                        [PS(bu), ('sg', fc % 2)], [('hid', sl, fc)])

            def stage_down(ex):
                sl = ex % 2
                hd = hid[sl]
                for s_ in range(4):
                    for hf in range(2):
                        idx = s_ * 2 + hf
                        bank = 4 + idx % 2
                        for fc in range(4):
                            mm(psb[bank][:], hd[:, fc, s_ * 128:(s_ + 1) * 128], Wd[sl][:, fc, hf * 512:(hf + 1) * 512], fc == 0, fc == 3,
                               [('hid', sl, fc), ('Wd', sl)], bank)
                        dt_ = dtmp[idx % 2]
                        vop(lambda e, dt_=dt_, bank=bank, s_=s_, ex=ex: e.tensor_scalar(out=dt_, in0=psb[bank][:], scalar1=comb[:, s_, ex:ex + 1],
                                                                                       scalar2=None, op0=ALU.mult),
                            [PS(bank), ('comb', sl_t, s_)], [('dtmp', idx % 2)])
                        vop(lambda e, dt_=dt_, s_=s_, hf=hf: e.tensor_tensor(out=xt[:, s_, hf * 512:(hf + 1) * 512], in0=xt[:, s_, hf * 512:(hf + 1) * 512],
                                                                            in1=dt_, op=ALU.add), [('dtmp', idx % 2), xk, ('xacc', sl_t, s_, hf)],
                            [('xacc', sl_t, s_, hf)], eng='gpsimd')

            if NE > 0:
                stage_gu(0)
            for ex in range(NE):
                if ex + 1 < NE:
                    stage_gu(ex + 1)
                stage_down(ex)
                if ex == NE // 2 and jt + 1 < NJ:
                    front_compute(jt + 1)
            if NE <= 1 and jt + 1 < NJ:
                front_compute(jt + 1)

        def finish(jt):
            sl = jt % 2
            t0 = jt * 512
            xt = xt2[sl]
            allx = [('xacc', sl, s_, hf) for s_ in range(4) for hf in range(2)] + [('xtD', sl)]
            if not last:
                pb.dma('sync', xres[t0:t0 + 512, :].rearrange("(s p) d -> p s d", p=128), xt, ('xtD_out', sl), reads=allx, writes=[('xtD', sl)])
            else:
                for s_ in range(4):
                    rmsnorm_rstd((xt[:, s_, :], allx), junk, ss[:, 4 + s_:5 + s_], rs[:, 4 + s_:5 + s_], D, 'F%d' % s_)
                    vop(lambda e, s_=s_, xt=xt: e.tensor_scalar(out=xnf[:, s_, :], in0=xt[:, s_, :], scalar1=rs[:, 4 + s_:5 + s_], scalar2=None,
                                                                op0=ALU.mult), allx + ['F%drs' % s_], [('xnf', s_)])
                    vop(lambda e, s_=s_: e.tensor_tensor(out=xnf[:, s_, :], in0=xnf[:, s_, :], in1=fng, op=ALU.mult), [('xnf', s_), 'fng'], [('xnf', s_)])
                pb.dma('sync', out_d[t0:t0 + 512, :].rearrange("(s p) d -> p s d", p=128), xnf, 'out_st',
                       reads=[('xnf', s_) for s_ in range(4)], writes=[('xtD', sl)] + [('xnf', s_) for s_ in range(4)])

        front_load(0)
        front_compute(0)
        for jt in range(NJ):
            if jt + 1 < NJ:
                front_load(jt + 1)
            experts(jt)
            finish(jt)
        pb.barrier()
        ar.reset(base)

    for l in range(nlayers):
        if stop_after == ('conv',):
            break
        phase_A(l)
        if stop_after == ('A', l):
            break
        phase_BA(l)
        phase_BB(l)
        phase_BD(l)
        if stop_after == ('B1', l):
            break
        if os.environ.get("KSKIPBC") != "1":
            phase_BC(l)
        if stop_after == ('B2', l):
            break
        phase_C(l)
        if stop_after == ('C', l):
            break
        if l == 0 and nlayers > 1:
            conv_w_in(1)
            conv_rest(1)
        phase_D(l, l == nlayers - 1)
        if stop_after == ('D', l):
            break

    pb.barrier()
    pb.emit()


def _consts():
    bf = ml_dtypes.bfloat16
    k = np.arange(128)[:, None]
    q = np.arange(128)[None, :]
    tril = (k <= q).astype(np.float32)
    triu = (k > q).astype(np.float32)
    c = {}
    c['c_ident'] = np.eye(128, dtype=np.float32)
    c['c_tril4'] = np.tile(tril, (1, 4)).astype(bf)
    c['c_triu4'] = np.tile(triu, (1, 4)).astype(bf)
    n = np.arange(256)
    t = np.arange(S)
    vis = ((n[:, None] * 16 + 31) <= t[None, :]) & (n[:, None] < 255)
    m = vis.reshape(2, 128, NQT, 128).transpose(2, 1, 0, 3)
    c['c_cmpmask'] = np.ascontiguousarray(np.tile(m, (1, 1, 1, 4))).astype(bf)
    c_start = np.arange(256) * 16
    s_start = np.arange(64) * 64
    ov = ((c_start[:, None] <= s_start[None, :] + 63) & (c_start[:, None] + 31 >= s_start[None, :])).astype(np.float32)
    ov[255] = 0
    ovc = np.concatenate([ov, np.ones((256, 1), np.float32)], axis=1)
    ovc[255] = 0
    c['c_ovc'] = np.ascontiguousarray(ovc.reshape(2, 128, 65).transpose(1, 0, 2)).astype(bf)
    kk = np.arange(S)
    c['c_eblk'] = (kk[None, :] // 64 == np.arange(64)[:, None]).astype(bf)
    blk = np.arange(64)
    cur = t // 64
    causal = blk[None, :] * 64 <= t[:, None]
    forced = (blk[None, :] == 0) | (blk[None, :] == cur[:, None]) | (blk[None, :] == cur[:, None] - 1)
    c['c_selC'] = (causal & ~forced).astype(np.float32)
    c['c_selB'] = np.where(forced, 1e6, np.where(causal, 0.0, -1.0)).astype(np.float32)
    pc = np.zeros((128, 4, 16), np.float32)
    for gi, w in enumerate((2, 4, 8, 16)):
        pc[:, gi, :] = 1.0 / np.minimum(np.arange(16) + 1, w)
    c['c_pcorr'] = pc
    return c


def _prep(inp):
    f = np.float32
    L = DEPTH
    shared = {}
    shared['w_in'] = np.ascontiguousarray(inp['w_in'], dtype=f)
    shared['w_branch'] = np.ascontiguousarray(inp['w_branch'], dtype=f).reshape(L, 4 * MIX, D)
    shared['w_out'] = np.ascontiguousarray(inp['w_out'], dtype=f)
    shared['moe_g'] = np.ascontiguousarray(inp['moe_w_gate'], dtype=f)
    shared['moe_u'] = np.ascontiguousarray(inp['moe_w_up'], dtype=f)
    shared['moe_d'] = np.ascontiguousarray(inp['moe_w_down'], dtype=f)
    shared['gm_wsT'] = np.ascontiguousarray(np.transpose(inp['gm_ws'], (0, 1, 3, 2)), dtype=f)
    shared['lru_wa'] = np.ascontiguousarray(inp['lru_wa'], dtype=f)
    shared['lru_wx'] = np.ascontiguousarray(inp['lru_wx'], dtype=f)
    shared['cmp_w1'] = np.ascontiguousarray(inp['cmp_w1'], dtype=f)
    shared['cmp_w2'] = np.ascontiguousarray(inp['cmp_w2'], dtype=f)
    shared['cmp_peT'] = np.ascontiguousarray(np.transpose(inp['cmp_pe'], (0, 1, 3, 2)), dtype=f)
    shared['pool_w'] = np.ascontiguousarray(inp['pool_w'], dtype=f)
    shared['router_w'] = np.ascontiguousarray(np.concatenate([inp['router_w_group'], inp['router_w_expert']], axis=2), dtype=f)
    vecs = np.zeros((L, 128, NV), f)
    rows = np.zeros((L, 128, NR), f)
    for l in range(L):
        vecs[l, :, 0:8] = inp['norm1_g'][l].reshape(8, 128).T
        vecs[l, :, 8:16] = inp['norm2_g'][l].reshape(8, 128).T
        for k in range(4):
            vecs[l, :, 16 + 4 * k:20 + 4 * k] = inp['conv_w'][l, k].reshape(4, 128).T
        vecs[l, :, 32:36] = inp['conv_b'][l].reshape(4, 128).T
        vecs[l, :, 36:40] = inp['lru_ba'][l].reshape(4, 128).T
        vecs[l, :, 40:44] = inp['lru_bx'][l].reshape(4, 128).T
        vecs[l, :, 44:48] = inp['lru_lambda'][l].reshape(4, 128).T
        vecs[l, :, 48:52] = inp['pool_scale'][l].reshape(4, 128).T
        rows[l, :, 0:512] = inp['gm_norm_g'][l][None, :]
        rows[l, :, 512:1024] = inp['gm_b'][l].reshape(1, 512)
        rows[l, :, 1024:1028] = inp['router_b_group'][l][None, :]
        rows[l, :, 1028:1044] = inp['router_b_expert'][l][None, :]
        rows[l, :, 1044:2068] = inp['final_norm_g'][None, :]
    shared['vecs'] = vecs
    shared['rows'] = rows
    shared.update(_consts())
    return shared


def kernel(**inputs):
    shared = _prep(inputs)
    x = np.ascontiguousarray(inputs['x'], dtype=np.float32)
    nc = build()
    in_maps = []
    for c in range(8):
        m = dict(shared)
        m['x'] = x[c % 4]
        in_maps.append(m)
    res = run_bass_kernel_spmd(nc, in_maps, core_ids=list(range(8)))
    out = np.stack([res.results[c]['out'] for c in range(4)], axis=0)
    return out.astype(np.float32)
```
